# Optimizing a Trainium2 kernel written in Bass

```python
import jax, jax.numpy as jnp
from jax import lax
import numpy as np

D_MODEL = 1024
BATCH = 4
SEQ = 4096
DEPTH = 1

CHUNK = 64
ATT_HEADS = 8
HEAD_DIM = 64
ATT_WIDTH = ATT_HEADS * HEAD_DIM
LRU_WIDTH = D_MODEL - ATT_WIDTH
LRU_BLOCKS = 8
LRU_BLOCK_DIM = LRU_WIDTH // LRU_BLOCKS
CONV_WIDTH = 4
LRU_C = 8.0
Q_BLOCK = 128
N_GROUPS = 4
EXPERTS_PER_GROUP = 8
TOP_K_INNER = 2
D_EXPERT = 256
RMS_EPS = 1e-6
IN_SIZES = (ATT_WIDTH, ATT_WIDTH, ATT_WIDTH, ATT_HEADS, LRU_WIDTH, LRU_WIDTH)
IN_COLS = sum(IN_SIZES)

kernel_name = 'hymba_fox_rglru_hmoe_block'


def rmsnorm(x, g):
    xf = x.astype(jnp.float32)
    inv = lax.rsqrt(jnp.mean(xf * xf, axis=-1, keepdims=True) + RMS_EPS)
    return (xf * inv * g.astype(jnp.float32)).astype(x.dtype)


def forgetting_attention(q, k, v, log_f):
    b, s, h, dh = q.shape
    c = jnp.cumsum(log_f, axis=1).transpose(0, 2, 1)
    n_blk = s // Q_BLOCK
    q_blk = q.reshape(b, n_blk, Q_BLOCK, h, dh).transpose(1, 0, 2, 3, 4)
    c_blk = c.reshape(b, h, n_blk, Q_BLOCK).transpose(2, 0, 1, 3)
    starts = jnp.arange(n_blk, dtype=jnp.int32) * Q_BLOCK
    k_pos = jnp.arange(s, dtype=jnp.int32)
    scale = HEAD_DIM ** -0.5

    def one_block(args):
        qb, cb, start = args
        logits = jnp.einsum('bqhd,bkhd->bhqk', qb, k, preferred_element_type=jnp.float32) * scale
        decay = cb[..., :, None] - c[..., None, :]
        q_pos = start + jnp.arange(Q_BLOCK, dtype=jnp.int32)
        causal = k_pos[None, :] <= q_pos[:, None]
        logits = jnp.where(causal, logits + decay, -1e30)
        p = jax.nn.softmax(logits, axis=-1)
        return jnp.einsum('bhqk,bkhd->bqhd', p.astype(v.dtype), v)

    out = lax.map(one_block, (q_blk, c_blk, starts))
    return out.transpose(1, 0, 2, 3, 4).reshape(b, s, h * dh)


def causal_depthwise_conv(x, w, bias):
    s = x.shape[1]
    xp = jnp.pad(x, ((0, 0), (CONV_WIDTH - 1, 0), (0, 0)))
    out = bias + w[0] * xp[:, 0:s]
    for tap in range(1, CONV_WIDTH):
        out = out + w[tap] * xp[:, tap:tap + s]
    return out


def block_diag_linear(x, w, b):
    xb = x.reshape(x.shape[:-1] + (LRU_BLOCKS, LRU_BLOCK_DIM))
    y = jnp.einsum('bsni,nij->bsnj', xb, w) + b
    return y.reshape(x.shape)


def rg_lru(x, w_a, b_a, w_x, b_x, lam):
    r = jax.nn.sigmoid(block_diag_linear(x, w_a, b_a)).astype(jnp.float32)
    i = jax.nn.sigmoid(block_diag_linear(x, w_x, b_x))
    log_a = -LRU_C * r * jax.nn.softplus(-lam.astype(jnp.float32))
    a = jnp.exp(log_a)
    mult = jnp.sqrt(-jnp.expm1(2.0 * log_a))
    u = mult * (i * x).astype(jnp.float32)

    def combine(left, right):
        a_l, u_l = left
        a_r, u_r = right
        return a_r * a_l, a_r * u_l + u_r

    _, hseq = lax.associative_scan(combine, (a, u), axis=1)
    return hseq.astype(x.dtype)


def hierarchical_moe(t, w_group, b_group, w_inner, b_inner, w_gate, w_up, w_down):
    group_p = jax.nn.softmax((t @ w_group + b_group).astype(jnp.float32), axis=-1)
    g_w, g_idx = lax.top_k(group_p, 1)
    inner_all = jnp.einsum('nd,gde->nge', t, w_inner) + b_inner
    inner_logits = jnp.take_along_axis(inner_all, g_idx[:, :, None], axis=1)[:, 0]
    inner_p = jax.nn.softmax(inner_logits.astype(jnp.float32), axis=-1)
    e_w, e_idx = lax.top_k(inner_p, TOP_K_INNER)
    e_w = e_w / jnp.sum(e_w, axis=-1, keepdims=True)
    expert_w = jnp.sum(jax.nn.one_hot(e_idx, EXPERTS_PER_GROUP, dtype=jnp.float32) * e_w[..., None], axis=1)
    combine = (g_w[:, :, None]
               * jax.nn.one_hot(g_idx[:, 0], N_GROUPS, dtype=jnp.float32)[:, :, None]
               * expert_w[:, None, :]).astype(t.dtype)
    y = jnp.zeros_like(t)
    for g in range(N_GROUPS):
        hg = jax.nn.silu(jnp.einsum('nd,edf->nef', t, w_gate[g])) * jnp.einsum('nd,edf->nef', t, w_up[g])
        y = y + jnp.einsum('nef,efd->nd', hg * combine[:, g, :, None], w_down[g])
    return y


def setup_inputs(seed: int = 0) -> dict:
    key = jax.random.key(seed)
    ks = jax.random.split(key, 24)
    nrm = jax.random.normal
    L, D, G, E, F = DEPTH, D_MODEL, N_GROUPS, EXPERTS_PER_GROUP, D_EXPERT
    a0 = jax.random.uniform(ks[10], (L, LRU_WIDTH), minval=0.9, maxval=0.999)
    sig = a0 ** (1.0 / LRU_C)
    lam = jnp.log(sig) - jnp.log1p(-sig)
    return {
        'x': nrm(ks[0], (BATCH, SEQ, D), jnp.float32),
        'mix_norm': 1.0 + 0.05 * nrm(ks[1], (L, D), jnp.float32),
        'w_in': nrm(ks[2], (L, D, IN_COLS), jnp.float32) * D ** -0.5,
        'b_forget': jnp.linspace(1.0, 6.0, ATT_HEADS, dtype=jnp.float32) + 0.1 * nrm(ks[3], (L, ATT_HEADS), jnp.float32),
        'conv_w': nrm(ks[4], (L, CONV_WIDTH, LRU_WIDTH), jnp.float32) * CONV_WIDTH ** -0.5,
        'conv_b': 0.02 * nrm(ks[5], (L, LRU_WIDTH), jnp.float32),
        'w_a': nrm(ks[6], (L, LRU_BLOCKS, LRU_BLOCK_DIM, LRU_BLOCK_DIM), jnp.float32) * LRU_BLOCK_DIM ** -0.5,
        'b_a': 0.02 * nrm(ks[7], (L, LRU_BLOCKS, LRU_BLOCK_DIM), jnp.float32),
        'w_x': nrm(ks[8], (L, LRU_BLOCKS, LRU_BLOCK_DIM, LRU_BLOCK_DIM), jnp.float32) * LRU_BLOCK_DIM ** -0.5,
        'b_x': 0.02 * nrm(ks[9], (L, LRU_BLOCKS, LRU_BLOCK_DIM), jnp.float32),
        'lru_lambda': lam,
        'w_out': nrm(ks[11], (L, D, D), jnp.float32) * D ** -0.5,
        'ffn_norm': 1.0 + 0.05 * nrm(ks[12], (L, D), jnp.float32),
        'w_group': nrm(ks[13], (L, D, G), jnp.float32) * D ** -0.5,
        'b_group': 0.01 * nrm(ks[14], (L, G), jnp.float32),
        'w_inner': nrm(ks[15], (L, G, D, E), jnp.float32) * D ** -0.5,
        'b_inner': 0.01 * nrm(ks[16], (L, G, E), jnp.float32),
        'w_gate': nrm(ks[17], (L, G, E, D, F), jnp.float32) * D ** -0.5,
        'w_up': nrm(ks[18], (L, G, E, D, F), jnp.float32) * D ** -0.5,
        'w_down': nrm(ks[19], (L, G, E, F, D), jnp.float32) * F ** -0.5,
        'final_norm': 1.0 + 0.05 * nrm(ks[20], (D,), jnp.float32),
    }


def reference(x, mix_norm, w_in, b_forget, conv_w, conv_b, w_a, b_a, w_x, b_x, lru_lambda,
              w_out, ffn_norm, w_group, b_group, w_inner, b_inner, w_gate, w_up, w_down, final_norm):
    b, s, d = x.shape
    split_points = [int(v) for v in np.cumsum(IN_SIZES)[:-1]]
    for l in range(DEPTH):
        h = rmsnorm(x, mix_norm[l])
        z = h @ w_in[l]
        q, k, v, f_logit, gate_in, rec_in = jnp.split(z, split_points, axis=-1)
        log_f = jax.nn.log_sigmoid(f_logit.astype(jnp.float32) + b_forget[l].astype(jnp.float32))
        att = forgetting_attention(q.reshape(b, s, ATT_HEADS, HEAD_DIM),
                                   k.reshape(b, s, ATT_HEADS, HEAD_DIM),
                                   v.reshape(b, s, ATT_HEADS, HEAD_DIM), log_f)
        rec = causal_depthwise_conv(rec_in, conv_w[l], conv_b[l])
        rec = rg_lru(rec, w_a[l], b_a[l], w_x[l], b_x[l], lru_lambda[l])
        lru_out = rec * jax.nn.gelu(gate_in, approximate=True)
        x = x + jnp.concatenate([att, lru_out], axis=-1) @ w_out[l]
        h2 = rmsnorm(x, ffn_norm[l]).reshape(b * s, d)
        y = hierarchical_moe(h2, w_group[l], b_group[l], w_inner[l], b_inner[l],
                             w_gate[l], w_up[l], w_down[l])
        x = x + y.reshape(b, s, d)
    return rmsnorm(x, final_norm)
```

```python
import os
import numpy as np
import ml_dtypes
from contextlib import ExitStack
import concourse.bass as bass
import concourse.mybir as mybir
from concourse.bass_utils import run_bass_kernel_spmd

F32 = mybir.dt.float32
BF16 = mybir.dt.bfloat16
AF = mybir.ActivationFunctionType
ALU = mybir.AluOpType
AX = mybir.AxisListType

ENGS = ("sp", "act", "dve", "pool", "pe")

D = 1024
SEQ = 4096
NTOK = 4096
NOWN = 2048
NH = 8
HD = 64
KA = 71
NEG = -30000.0
NE = 32
FE = 256
CAP = 2048 + 128
I32 = mybir.dt.int32
STAGE = int(os.environ.get("MK_STAGE", "9"))


class Builder:
    def __init__(self, nc):
        self.nc = nc
        self.ops = []
        self.last_write = {}
        self.reads_since = {}
        self.same_engine_sync = {"act": True, "dve": True, "pool": True, "pe": False, "sp": False}
        self.barrier_deps = set()
        self.names = {}
        self.cur_cond = None
        self.flags_ap = None
        self.flags_key = None

    def op(self, eng, fn, reads=(), writes=(), dma=False, nobarrier=False, semgroup=None, semkey=None):
        deps = set()
        for b in reads:
            w = self.last_write.get(b)
            if w is not None:
                deps.add(w)
        for b in writes:
            w = self.last_write.get(b)
            if w is not None:
                deps.add(w)
            deps.update(self.reads_since.get(b, ()))
        if not nobarrier:
            deps.update(self.barrier_deps)
        idx = len(self.ops)
        self.ops.append(dict(eng=eng, fn=fn, deps=deps, dma=dma, wkey=(("grp", semgroup) if semgroup else (("sk", semkey) if semkey else (writes[0] if writes else None))),
                             grp=semgroup is not None, cond=self.cur_cond))
        for b in reads:
            self.reads_since.setdefault(b, []).append(idx)
        for b in writes:
            self.last_write[b] = idx
            self.reads_since[b] = []
        return idx

    def dma(self, eng, out, in_, reads=(), writes=(), nobarrier=False, semgroup=None, semkey=None, **kw):
        def fn(e):
            return e.dma_start(out=out, in_=in_, **kw)
        return self.op(eng, fn, reads, writes, dma=True, nobarrier=nobarrier, semgroup=semgroup, semkey=semkey)

    def begin_cond(self, c):
        self.cur_cond = (self.cur_cond or ()) + (c,)

    def end_cond(self):
        self.cur_cond = self.cur_cond[:-1] or None

    def barrier(self):
        last = {}
        deps = set()
        for i, o in enumerate(self.ops):
            if o["dma"]:
                deps.add(i)
            else:
                last[o["eng"]] = i
        latest = {}
        for i in deps:
            latest[self.ops[i]["wkey"]] = max(latest.get(self.ops[i]["wkey"], -1), i)
        self.barrier_deps = set(latest.values()) | set(last.values())

    def emit(self, stack):
        nc = self.nc
        ops = self.ops
        n = len(ops)
        needed = [False] * n
        for i, o in enumerate(ops):
            pruned = set()
            for d in o["deps"]:
                od = ops[d]
                if (not od["dma"]) and od["eng"] == o["eng"] and not self.same_engine_sync[o["eng"]]:
                    continue
                pruned.add(d)
            o["deps"] = pruned
            for d in pruned:
                needed[d] = True
        if self.flags_key is not None:
            needed[self.last_write[self.flags_key]] = True
        eng_sem = {e: stack.enter_context(nc.semaphore("S_" + e)) for e in ENGS}
        dma_sems = {}
        dma_count = {}
        eng_count = {e: 0 for e in ENGS}
        for i, o in enumerate(ops):
            if o["dma"]:
                k = o["wkey"]
                if k not in dma_sems:
                    dma_sems[k] = stack.enter_context(nc.semaphore("D%d" % len(dma_sems)))
                    dma_count[k] = 0
                dma_count[k] += 16
                o["sig"] = (dma_sems[k], dma_count[k], ("d", k))
            elif needed[i]:
                eng_count[o["eng"]] += 1
                o["sig"] = (eng_sem[o["eng"]], eng_count[o["eng"]], ("e", o["eng"]))
            else:
                o["sig"] = None
        for o in ops:
            if o["dma"] and o["grp"]:
                sem, val, key = o["sig"]
                o["sig"] = (sem, dma_count[o["wkey"]], key)
        self.n_sems = len(dma_sems) + len(ENGS)
        print("ops", n, "sems", self.n_sems)
        per_eng = {e: [] for e in ENGS}
        for i, o in enumerate(ops):
            per_eng[o["eng"]].append(i)
        final_waits = [(dma_sems[k], dma_count[k], ("d", k)) for k in dma_sems]

        flag_sig = None
        if self.flags_key is not None:
            fo = ops[self.last_write[self.flags_key]]
            flag_sig = fo["sig"]
            assert flag_sig is not None

        def run_engine(ename, e):
            known = {}
            reg = None

            def emit_op(i):
                o = ops[i]
                need = {}
                for d in o["deps"]:
                    sem, val, key = ops[d]["sig"]
                    if known.get(key, 0) >= val:
                        continue
                    if key not in need or need[key][1] < val:
                        need[key] = (sem, val)
                for key, (sem, val) in need.items():
                    e.wait_ge(sem, val)
                    known[key] = val
                ins = o["fn"](e)
                try:
                    self.names[ins.ins.name] = i
                except Exception:
                    pass
                if o["sig"] is not None:
                    ins.then_inc(o["sig"][0], 16 if o["dma"] else 1)

            def emit_list(idxs, depth):
                nonlocal reg
                p = 0
                while p < len(idxs):
                    cpath = ops[idxs[p]]["cond"] or ()
                    if len(cpath) <= depth:
                        emit_op(idxs[p])
                        p += 1
                        continue
                    c = cpath[depth]
                    blk = []
                    while p < len(idxs):
                        cp2 = ops[idxs[p]]["cond"] or ()
                        if len(cp2) > depth and cp2[depth] == c:
                            blk.append(idxs[p])
                            p += 1
                        else:
                            break
                    if reg is None:
                        reg = e.alloc_register("cr_" + ename)
                    sem, val, key = flag_sig
                    if known.get(key, 0) < val:
                        e.wait_ge(sem, val)
                        known[key] = val
                    e.reg_load(reg, self.flags_ap[0:1, c:c + 1])
                    snap = dict(known)
                    with e.If(reg):
                        emit_list(blk, depth + 1)
                    known.clear()
                    known.update(snap)
                    comp = {}
                    for i in blk:
                        sg_ = ops[i]["sig"]
                        if sg_ is None:
                            continue
                        sem, val, key = sg_
                        inc = 16 if ops[i]["dma"] else 1
                        if key not in comp:
                            comp[key] = [sem, val - inc, 0]
                        comp[key][2] += inc
                    if comp:
                        with e.Else():
                            for key, (sem, prev, tot) in comp.items():
                                if prev > 0:
                                    e.wait_ge(sem, prev)
                                e.sem_inc(sem, tot)

            emit_list(per_eng[ename], 0)
            if ename == "sp":
                for sem, val, key in final_waits:
                    if known.get(key, 0) < val:
                        e.wait_ge(sem, val)

        block = stack.enter_context(nc.Block())

        @block.sync
        def _(e):
            run_engine("sp", e)

        @block.scalar
        def _(e):
            run_engine("act", e)

        @block.vector
        def _(e):
            run_engine("dve", e)

        @block.gpsimd
        def _(e):
            run_engine("pool", e)

        @block.tensor
        def _(e):
            run_engine("pe", e)


class Deferred:
    def __init__(self):
        self.q = []
        self.n = 0

    def at(self, target, fn):
        self.q.append((target, self.n, fn))
        self.n += 1

    def run(self, now):
        ready = sorted([x for x in self.q if x[0] <= now])
        self.q = [x for x in self.q if x[0] > now]
        for _, _, fn in ready:
            fn()

    def flush(self):
        self.run(1 << 60)


class Arena:
    def __init__(self, nc, stack, nbytes):
        self.t = stack.enter_context(nc.sbuf_tensor("arena", [128, nbytes // 4], F32))
        self.nbytes = nbytes
        self.off = 0

    def seek(self, off):
        self.off = off

    def alloc(self, shape, dt):
        n = 1
        for s in shape[1:]:
            n *= s
        esz = 4 if dt == F32 else 2
        nb = (n * esz + 31) // 32 * 32
        assert self.off + nb <= self.nbytes, (self.off, nb, self.nbytes)
        v = self.t[:, self.off // 4:(self.off + nb) // 4]
        if dt != F32:
            v = v.bitcast(dt)
        v = v[:, 0:n]
        if len(shape) == 3:
            v = v.rearrange("p (a b) -> p a b", a=shape[1])
        elif len(shape) == 4:
            v = v.rearrange("p (a b c) -> p a b c", a=shape[1], b=shape[2])
        self.off += nb
        return v


def build_program():
    nc = bass.Bass("TRN2", target_bir_lowering=False)
    din = lambda name, shape, dt=F32: nc.dram_tensor(name, list(shape), dt, kind="ExternalInput").ap()
    xs = din("xs", [NTOK, D])
    ident_d = din("ident", [128, 128])
    maskT_d = din("maskT", [128, 128])
    flag_d = din("flag", [128, 1])
    kmrow_d = din("kmrow", [1, NTOK], BF16)
    ones_d = din("onesd", [NH, 3, 512], BF16)
    gcol_d = din("gcol", [128, 8])
    w_in_d = din("w_in", [D, 2568])
    bf_d = din("bfcol", [8, 1])
    cdiag_d = din("cdiag", [128, 16, 128])
    convb_d = din("convb", [128, 4])
    bda_d = din("bda", [128, 4, 128])
    bdx_d = din("bdx", [128, 4, 128])
    ba_d = din("bacol", [128, 4])
    bx_d = din("bxcol", [128, 4])
    lam_d = din("lamcol", [128, 4])
    w_out_d = din("w_out", [D, D])
    g2bc_d = din("g2bc", [128, D])
    g3bc_d = din("g3bc", [128, D])
    wr_d = din("wr", [D, 36])
    rb_d = din("rbbc", [128, 36])
    wg_d = din("w_gate", [NE, D, FE])
    wu_d = din("w_up", [NE, D, FE])
    wd_d = din("w_down", [NE, FE, D])
    ebase1_d = din("ebase1", [128, NE])
    eb16_d = din("ebase16", [128, 16 * NE])
    padbase_d = din("padbase", [128, NE])
    triu_d = din("triu", [128, 128])
    out_d = nc.dram_tensor("out", [NOWN, D], F32, kind="ExternalOutput").ap()
    xs_scr = nc.dram_tensor("xs_scr", [NE * CAP, D], BF16).ap()
    ys_scr = nc.dram_tensor("ys_scr", [NE * CAP, D], BF16).ap()
    kaug_d = nc.dram_tensor("kaug_s", [NH, KA, NTOK], BF16).ap()
    qaug_d = nc.dram_tensor("qaug_s", [NH, KA, NOWN], BF16).ap()
    x1_d = nc.dram_tensor("x1_s", [NOWN, D], F32).ap()

    st = ExitStack()
    with st:
        B = Builder(nc)
        sb = lambda name, shape, dt: st.enter_context(nc.sbuf_tensor("s_" + name, shape, dt))
        identf = sb("identf", [128, 128], F32)
        identb = sb("identb", [128, 128], BF16)
        maskf = sb("maskf", [128, 128], F32)
        maskb = sb("maskb", [128, 128], BF16)
        onesf = sb("onesf", [128, 64], F32)
        flag = sb("flag", [128, 1], F32)
        gcol = sb("gcol", [128, 8], F32)
        bfcol = sb("bfcol", [8, 1], F32)
        nbfcol = sb("nbfcol", [8, 1], F32)
        convb = sb("convbc", [128, 4], F32)
        bacol = sb("bacol", [128, 4], F32)
        bxcol = sb("bxcol", [128, 4], F32)
        lamcol = sb("lamcol", [128, 4], F32)
        s1col = sb("s1col", [128, 4], F32)
        s2col = sb("s2col", [128, 4], F32)
        hbacol = sb("hbacol", [128, 4], F32)
        hbxcol = sb("hbxcol", [128, 4], F32)
        hs1col = sb("hs1col", [128, 4], F32)
        hs2col = sb("hs2col", [128, 4], F32)
        qcol = sb("qcol", [128, 1], F32)
        mhalf = sb("mhalf", [128, 1], F32)
        g2bc = sb("g2bc", [128, D], F32)
        g3bc = g2bc
        rbbc = sb("rbbc", [128, 36], F32)
        wr = sb("wr", [128, 8, 36], BF16)
        comb = sb("comb", [128, 16, NE], F32)
        hstate = sb("hstate", [128, 4], F32)
        cstate = sb("cstate", [8, 1], F32)
        ss4 = sb("ss4", [128, 4], F32)
        rstd4 = sb("rstd4", [128, 4], F32)
        rt = sb("rt", [128, 64], F32)

        ar = Arena(nc, st, 186 * 1024)
        OFF_V = 0
        OFF_LRU = OFF_V + 34 * 1024
        OFF_ATT = OFF_LRU + 16 * 1024
        OFF_P = OFF_ATT + 32 * 1024
        ar.seek(OFF_V)
        vaug = ar.alloc([128, 32, NH, 65], BF16)
        ar.seek(OFF_LRU)
        lruo = ar.alloc([128, 4, NOWN], BF16)
        ar.seek(OFF_ATT)
        attT = ar.alloc([128, NH, NOWN], BF16)

        pb = [st.enter_context(nc.psum_tensor("pb%d" % i, [128, 512], F32)) for i in range(6)]
        pt = [st.enter_context(nc.psum_tensor("pt%d" % i, [128, 1024], BF16)) for i in range(2)]
        rr = {"pb": 0, "pt": 0}

        def nextpb():
            i = rr["pb"]
            rr["pb"] = (i + 1) % 6
            return i

        def nextpt():
            i = rr["pt"]
            rr["pt"] = (i + 1) % 2
            return i

        B.dma("sp", identf[:], ident_d, writes=["identf"], semgroup="csp")
        B.dma("sp", maskf[:], maskT_d, writes=["maskf"], semgroup="csp")
        B.dma("sp", flag[:], flag_d, writes=["flag"], semgroup="csp")
        B.dma("sp", gcol[:], gcol_d, writes=["gcol"], semgroup="csp")
        B.dma("sp", bfcol[:], bf_d, writes=["bfcol"], semgroup="csp")
        B.dma("sp", convb[:], convb_d, writes=["convb"], semgroup="csp")
        B.dma("sp", bacol[:], ba_d, writes=["bacol"], semgroup="csp")
        B.dma("sp", bxcol[:], bx_d, writes=["bxcol"], semgroup="csp")
        B.dma("sp", lamcol[:], lam_d, writes=["lamcol"], semgroup="csp")
        B.dma("sp", g2bc[:], g2bc_d, writes=["g2bc"], semgroup="csp")
        B.dma("sp", rbbc[:], rb_d, writes=["rbbc"], semgroup="csp")
        B.dma("pool", wr[:], wr_d.rearrange("(c p) f -> p c f", p=128), writes=["wr"], semgroup="cpl")
        B.op("dve", lambda e: e.tensor_copy(identb[:], identf[:]), reads=["identf"], writes=["identb"])
        B.op("dve", lambda e: e.tensor_copy(maskb[:], maskf[:]), reads=["maskf"], writes=["maskb"])
        B.op("dve", lambda e: e.memset(onesf[:], 1.0), writes=["onesf"])
        B.op("dve", lambda e: e.memset(hstate[:], 0.0), writes=["hstate"])
        B.op("dve", lambda e: e.memset(cstate[:], 0.0), writes=["cstate"])
        B.op("dve", lambda e: e.tensor_scalar(out=nbfcol[:], in0=bfcol[:], scalar1=-1.0, scalar2=None, op0=ALU.mult),
             reads=["bfcol"], writes=["nbfcol"])
        B.op("act", lambda e: e.activation(out=s1col[:], in_=lamcol[:], func=AF.Exp, scale=-1.0), reads=["lamcol"], writes=["s1col"])
        B.op("act", lambda e: e.activation(out=s1col[:], in_=s1col[:], func=AF.Ln, bias=1.0), reads=["s1col"], writes=["s1col"])
        B.op("dve", lambda e: e.tensor_scalar(out=s2col[:], in0=s1col[:], scalar1=-16.0, scalar2=None, op0=ALU.mult),
             reads=["s1col"], writes=["s2col"])
        B.op("dve", lambda e: e.tensor_scalar(out=s1col[:], in0=s1col[:], scalar1=-8.0, scalar2=None, op0=ALU.mult),
             reads=["s1col", "s2col"], writes=["s1col"])
        for dst, src, nm in ((hbacol, bacol, "hbacol"), (hbxcol, bxcol, "hbxcol"), (hs1col, s1col, "hs1col"), (hs2col, s2col, "hs2col")):
            B.op("dve", lambda e, dst=dst, src=src: e.tensor_scalar(out=dst[:], in0=src[:], scalar1=0.5, scalar2=None, op0=ALU.mult),
                 reads=["bacol", "bxcol", "s1col", "s2col"], writes=[nm])
        B.op("dve", lambda e: e.memset(qcol[:], 0.25), writes=["qcol"])
        B.op("dve", lambda e: e.memset(mhalf[:], -0.5), writes=["mhalf"])
        for h in range(NH):
            B.dma("sp", kaug_d[h, 70:71, :], kmrow_d, writes=[("kaugc", h)], semgroup="kscr")
        for t in range(8):
            B.dma("sp", kaug_d[:, 64:67, t * 512:(t + 1) * 512], ones_d, writes=[("kaug1", t)], semgroup="kscr")
        for t in range(4):
            B.dma("sp", qaug_d[:, 67:70, t * 512:(t + 1) * 512], ones_d, writes=[("qaug1", t)], semgroup="qscr")
            B.dma("sp", qaug_d[:, 70:71, t * 512:(t + 1) * 512], ones_d[:, 0:1, :], writes=[("qaug2", t)], semgroup="qscr")

        ar.seek(OFF_ATT)
        win = ar.alloc([128, 8, 2568], BF16)
        stage = [ar.alloc([128, 642], F32) for _ in range(2)]
        hT = [ar.alloc([128, 8, 512], BF16) for _ in range(2)]
        xt = [ar.alloc([128, D], F32) for _ in range(2)]
        xn = [ar.alloc([128, D], BF16) for _ in range(2)]
        rin = [ar.alloc([128, 4, 516], BF16) for _ in range(2)]
        cvb = ar.alloc([128, 4, 512], BF16)
        kst = ar.alloc([128, NH, 512], BF16)
        qst = ar.alloc([128, NH, 512], BF16)
        tr = ar.alloc([128, 512], F32)
        ti = ar.alloc([128, 512], F32)
        ta = ar.alloc([128, 512], F32)
        tm = ar.alloc([128, 512], F32)
        tu = ar.alloc([128, 512], F32)
        th = ar.alloc([128, 512], F32)
        gel = ar.alloc([128, 512], F32)
        fl = ar.alloc([128, 512], F32)
        fc = ar.alloc([128, 512], F32)
        fr = ar.alloc([128, 512], F32)
        fp = ar.alloc([128, 6, 512], BF16)
        cdiag = ar.alloc([128, 16, 128], BF16)
        bda = ar.alloc([128, 4, 128], BF16)
        bdx = ar.alloc([128, 4, 128], BF16)
        P1_END = ar.off
        B.dma("pool", cdiag[:], cdiag_d, writes=["cdiag"])
        B.dma("pool", bda[:], bda_d, writes=["bda"])
        B.dma("pool", bdx[:], bdx_d, writes=["bdx"])

        for dc in range(8):
            for q in range(4):
                s = (dc * 4 + q) % 2
                B.dma("sp", stage[s][:], w_in_d[dc * 128:(dc + 1) * 128, q * 642:(q + 1) * 642], writes=[("stage", s)])
                B.op("dve", lambda e, dc=dc, q=q, s=s: e.tensor_scalar(out=win[:, dc, q * 642:(q + 1) * 642], in0=stage[s][:],
                                                                     scalar1=gcol[:, dc:dc + 1], scalar2=None, op0=ALU.mult),
                     reads=[("stage", s), "gcol"], writes=[("win", dc)])
        WK, WV, WF, WG, WR = 512, 1024, 1536, 1544, 2056
        winr = [("win", dc) for dc in range(8)]

        B.op("dve", lambda e: e.memset(rin[1][:], 0.0), writes=[("rin", 1)])
        B.op("dve", lambda e: e.memset(rin[0][:, :, 0:4], 0.0), writes=[("rinh", 0)])
        B.op("dve", lambda e: e.memset(vaug[:], 1.0), writes=["vaug_init"])

        def tile_ctx(T):
            return T >= 4, hT[T % 2], ("hT", T % 2)

        def piece_N(T):
            own, hb, hkey = tile_ctx(T)
            for pr in range(2):
                blks = (2 * pr, 2 * pr + 1)
                for blk in blks:
                    g = T * 4 + blk
                    xb = xt[g % 2]
                    B.dma("sp", xb[:], xs[g * 128:(g + 1) * 128, :], writes=[("xt", g % 2)])
                    B.op("dve", lambda e, xb=xb, blk=blk: e.scalar_tensor_tensor(out=xn[0][:], in0=xb[:], scalar=1.0, in1=xb[:],
                                                                                 op0=ALU.mult, op1=ALU.mult, accum_out=ss4[:, blk:blk + 1]),
                         reads=[("xt", g % 2)], writes=[("xnjunk",), ("ss4", blk)])
                rk = [("rstd4", k) for k in blks]
                c0 = 2 * pr
                B.op("dve", lambda e, c0=c0: e.tensor_scalar(out=rstd4[:, c0:c0 + 2], in0=ss4[:, c0:c0 + 2], scalar1=1.0 / D, scalar2=1e-6,
                                                             op0=ALU.mult, op1=ALU.add),
                     reads=[("ss4", k) for k in blks], writes=rk)
                B.op("act", lambda e, c0=c0: e.activation(out=rstd4[:, c0:c0 + 2], in_=rstd4[:, c0:c0 + 2], func=AF.Sqrt), reads=rk, writes=rk)
                B.op("dve", lambda e, c0=c0: e.reciprocal(rstd4[:, c0:c0 + 2], rstd4[:, c0:c0 + 2]), reads=rk, writes=rk)
                for blk in blks:
                    g = T * 4 + blk
                    xb = xt[g % 2]
                    B.op("dve", lambda e, xb=xb, blk=blk: e.tensor_scalar(out=xn[1][:], in0=xb[:], scalar1=rstd4[:, blk:blk + 1], scalar2=None, op0=ALU.mult),
                         reads=[("xt", g % 2), ("rstd4", blk)], writes=[("xn", 1)])
                    ip = nextpt()
                    ptv = pt[ip][:].rearrange("p (a b) -> p a b", a=8)
                    for c in range(8):
                        B.op("pe", lambda e, c=c, ptv=ptv: e.transpose(ptv[:, c, :], xn[1][:, c * 128:(c + 1) * 128], identb[:]),
                             reads=[("xn", 1), "identb"], writes=[("pt", ip)])
                    B.op("dve", lambda e, ptv=ptv, hb=hb, blk=blk: e.tensor_copy(hb[:, :, blk * 128:(blk + 1) * 128], ptv),
                         reads=[("pt", ip)], writes=[hkey])

        def piece_K(T, pairs, last):
            own, hb, hkey = tile_ctx(T)
            for pr in pairs:
                ib = nextpb()
                for dc in range(8):
                    B.op("pe", lambda e, pr=pr, dc=dc, ib=ib: e.matmul(pb[ib][:, :], lhsT=win[:, dc, WK + pr * 128:WK + (pr + 1) * 128],
                                                                       rhs=hb[:, dc, :], start=(dc == 0), stop=(dc == 7)),
                         reads=[hkey] + winr, writes=[("pb", ib)])
                B.op("dve", lambda e, pr=pr, ib=ib: e.tensor_copy(kst[:, pr, :], pb[ib][:, :]), reads=[("pb", ib)], writes=[("kst", pr)])
            if last:
                kd = kaug_d[:, 0:64, T * 512:(T + 1) * 512].rearrange("(pr two) p t -> two p pr t", two=2)
                for two in range(2):
                    B.dma("sp", kd[two], kst[two * 64:(two + 1) * 64, 0:4, :],
                          reads=[("kst", pr) for pr in range(4)], writes=[("kaugd", T, two)], semkey="kst_out%d" % two)

        def piece_Q(T):
            own, hb, hkey = tile_ctx(T)
            if not own:
                return
            for pr in range(4):
                ib = nextpb()
                for dc in range(8):
                    B.op("pe", lambda e, pr=pr, dc=dc, ib=ib: e.matmul(pb[ib][:, :], lhsT=win[:, dc, pr * 128:(pr + 1) * 128],
                                                                       rhs=hb[:, dc, :], start=(dc == 0), stop=(dc == 7)),
                         reads=[hkey] + winr, writes=[("pb", ib)])
                B.op("act", lambda e, pr=pr, ib=ib: e.activation(out=qst[:, pr, :], in_=pb[ib][:, :], func=AF.Copy, scale=0.125),
                     reads=[("pb", ib)], writes=[("qst", pr)])
            qd = qaug_d[:, 0:64, (T - 4) * 512:(T - 3) * 512].rearrange("(pr two) p t -> two p pr t", two=2)
            for two in range(2):
                B.dma("sp", qd[two], qst[two * 64:(two + 1) * 64, 0:4, :],
                      reads=[("qst", pr) for pr in range(4)], writes=[("qaugd", T, two)], semkey="qst_out%d" % two)

        def piece_V(T):
            own, hb, hkey = tile_ctx(T)
            for blk in range(4):
                g = T * 4 + blk
                ib = nextpb()
                for dc in range(8):
                    B.op("pe", lambda e, blk=blk, dc=dc, ib=ib: e.matmul(pb[ib][:, :], lhsT=hb[:, dc, blk * 128:(blk + 1) * 128],
                                                                         rhs=win[:, dc, WV:WV + 512], start=(dc == 0), stop=(dc == 7)),
                         reads=[hkey] + winr, writes=[("pb", ib)])
                B.op("dve", lambda e, g=g, ib=ib: e.tensor_copy(vaug[:, g, :, 0:64], pb[ib][:, :].rearrange("p (h d) -> p h d", h=NH)),
                     reads=[("pb", ib), "vaug_init"], writes=[("vaug", g)])

        def piece_F(T):
            own, hb, hkey = tile_ctx(T)
            ib = nextpb()
            for dc in range(8):
                B.op("pe", lambda e, dc=dc, ib=ib: e.matmul(pb[ib][0:8, :], lhsT=win[:, dc, WF:WF + 8], rhs=hb[:, dc, :],
                                                            start=(dc == 0), stop=(dc == 7)),
                     reads=[hkey] + winr, writes=[("pb", ib)])
            B.op("act", lambda e, ib=ib: e.activation(out=fl[0:8, :], in_=pb[ib][0:8, :], func=AF.Exp, scale=-1.0, bias=nbfcol[:]),
                 reads=[("pb", ib), "nbfcol"], writes=["fl"])
            B.op("act", lambda e: e.activation(out=fl[0:8, :], in_=fl[0:8, :], func=AF.Ln, bias=1.0), reads=["fl"], writes=["fl"])
            B.op("dve", lambda e: e.memset(fr[0:8, :], 1.0), writes=["fr"])
            B.op("dve", lambda e: e.tensor_tensor_scan(out=fc[0:8, :], data0=fr[0:8, :], data1=fl[0:8, :], initial=cstate[:],
                                                       op0=ALU.mult, op1=ALU.add), reads=["fr", "fl", "cstate"], writes=["fc"])
            B.op("dve", lambda e: e.tensor_copy(cstate[:], fc[0:8, 511:512]), reads=["fc"], writes=["cstate"])
            B.op("dve", lambda e: e.tensor_copy(fp[0:8, 0, :], fc[0:8, :]), reads=["fc"], writes=[("fp", 0)])
            B.op("dve", lambda e: e.tensor_tensor(out=fr[0:8, :], in0=fc[0:8, :], in1=fp[0:8, 0, :], op=ALU.subtract),
                 reads=["fc", ("fp", 0)], writes=["fr"])
            B.op("dve", lambda e: e.tensor_copy(fp[0:8, 1, :], fr[0:8, :]), reads=["fr"], writes=[("fp", 1)])
            B.op("dve", lambda e: e.tensor_tensor(out=fl[0:8, :], in0=fr[0:8, :], in1=fp[0:8, 1, :], op=ALU.subtract),
                 reads=["fr", ("fp", 1)], writes=["fl"])
            B.op("dve", lambda e: e.tensor_copy(fp[0:8, 2, :], fl[0:8, :]), reads=["fl"], writes=[("fp", 2)])
            B.dma("sp", kaug_d[:, 67:70, T * 512:(T + 1) * 512], fp[0:8, 0:3, :], reads=[("fp", 0), ("fp", 1), ("fp", 2)],
                  writes=[("kaugp", T)], semkey="fpk_out")
            if own:
                B.op("dve", lambda e: e.tensor_scalar(out=fp[0:8, 3:6, :], in0=fp[0:8, 0:3, :], scalar1=-1.0, scalar2=None, op0=ALU.mult),
                     reads=[("fp", 0), ("fp", 1), ("fp", 2)], writes=[("fp", 3)])
                B.dma("sp", qaug_d[:, 64:67, (T - 4) * 512:(T - 3) * 512], fp[0:8, 3:6, :], reads=[("fp", 3)], writes=[("qaugp", T)], semkey="fpq_out")

        def piece_A(T):
            own, hb, hkey = tile_ctx(T)
            rb_ = rin[T % 2]
            rprev = rin[(T + 1) % 2]
            B.op("dve", lambda e: e.tensor_copy(rb_[:, :, 1:4], rprev[:, :, 513:516]),
                 reads=[("rin", (T + 1) % 2)], writes=[("rinh", T % 2)])
            for cc in range(4):
                ib = nextpb()
                for dc in range(8):
                    B.op("pe", lambda e, cc=cc, dc=dc, ib=ib: e.matmul(pb[ib][:, :], lhsT=win[:, dc, WR + cc * 128:WR + (cc + 1) * 128],
                                                                       rhs=hb[:, dc, :], start=(dc == 0), stop=(dc == 7)),
                         reads=[hkey] + winr, writes=[("pb", ib)])
                B.op("act", lambda e, cc=cc, ib=ib: e.activation(out=rb_[:, cc, 4:516], in_=pb[ib][:, :], func=AF.Copy),
                     reads=[("pb", ib)], writes=[("rin", T % 2)])
            for cc in range(4):
                ib = nextpb()
                for k in range(4):
                    B.op("pe", lambda e, cc=cc, k=k, ib=ib: e.matmul(pb[ib][:, :], lhsT=cdiag[:, k * 4 + cc, :],
                                                                     rhs=rb_[:, cc, 1 + k:1 + k + 512], start=(k == 0), stop=(k == 3)),
                         reads=[("rin", T % 2), ("rinh", T % 2), "cdiag"], writes=[("pb", ib)])
                B.op("act", lambda e, cc=cc, ib=ib: e.activation(out=cvb[:, cc, :], in_=pb[ib][:, :], func=AF.Identity, bias=convb[:, cc:cc + 1]),
                     reads=[("pb", ib), "convb"], writes=[("cvb", cc)])

        def piece_C(T, cc):
            own, hb, hkey = tile_ctx(T)
            ck = ("cvb", cc)
            iba = nextpb()
            B.op("pe", lambda e: e.matmul(pb[iba][:, :], lhsT=bda[:, cc, :], rhs=cvb[:, cc, :], start=True, stop=True),
                 reads=[ck, "bda"], writes=[("pb", iba)])
            ibx = nextpb()
            B.op("pe", lambda e: e.matmul(pb[ibx][:, :], lhsT=bdx[:, cc, :], rhs=cvb[:, cc, :], start=True, stop=True),
                 reads=[ck, "bdx"], writes=[("pb", ibx)])
            B.op("act", lambda e: e.activation(out=tr[:], in_=pb[iba][:, :], func=AF.Tanh, bias=hbacol[:, cc:cc + 1], scale=0.5),
                 reads=[("pb", iba), "hbacol"], writes=["tr"])
            B.op("act", lambda e: e.activation(out=ti[:], in_=pb[ibx][:, :], func=AF.Tanh, bias=hbxcol[:, cc:cc + 1], scale=0.5),
                 reads=[("pb", ibx), "hbxcol"], writes=["ti"])
            B.op("act", lambda e: e.activation(out=ta[:], in_=tr[:], func=AF.Exp, scale=hs1col[:, cc:cc + 1], bias=hs1col[:, cc:cc + 1]),
                 reads=["tr", "hs1col"], writes=["ta"])
            B.op("act", lambda e: e.activation(out=tm[:], in_=tr[:], func=AF.Exp, scale=hs2col[:, cc:cc + 1], bias=hs2col[:, cc:cc + 1]),
                 reads=["tr", "hs2col"], writes=["tm"])
            B.op("act", lambda e: e.activation(out=tm[:], in_=tm[:], func=AF.Sqrt, scale=-0.25, bias=qcol[:, 0:1]), reads=["tm", "qcol"], writes=["tm"])
            B.op("dve", lambda e: e.scalar_tensor_tensor(out=tu[:], in0=ti[:], scalar=1.0, in1=cvb[:, cc, :], op0=ALU.add, op1=ALU.mult),
                 reads=["ti", ck], writes=["tu"])
            B.op("dve", lambda e: e.tensor_tensor(out=tu[:], in0=tu[:], in1=tm[:], op=ALU.mult), reads=["tu", "tm"], writes=["tu"])
            if T == 4:
                B.op("dve", lambda e: e.tensor_scalar(out=hstate[:, cc:cc + 1], in0=hstate[:, cc:cc + 1], scalar1=flag[:, 0:1],
                                                      scalar2=None, op0=ALU.mult), reads=[("hstate", cc), "flag"], writes=[("hstate", cc)])
            B.op("dve", lambda e: e.tensor_tensor_scan(out=th[:], data0=ta[:], data1=tu[:], initial=hstate[:, cc:cc + 1],
                                                       op0=ALU.mult, op1=ALU.add),
                 reads=["ta", "tu", ("hstate", cc), "hstate"], writes=["th"])
            B.op("dve", lambda e: e.tensor_copy(hstate[:, cc:cc + 1], th[:, 511:512]), reads=["th"], writes=[("hstate", cc)])
            if own:
                ibg = nextpb()
                for dc in range(8):
                    B.op("pe", lambda e, dc=dc: e.matmul(pb[ibg][:, :], lhsT=win[:, dc, WG + cc * 128:WG + (cc + 1) * 128],
                                                         rhs=hb[:, dc, :], start=(dc == 0), stop=(dc == 7)),
                         reads=[hkey] + winr, writes=[("pb", ibg)])
                B.op("act", lambda e: e.activation(out=gel[:], in_=pb[ibg][:, :], func=AF.Gelu_apprx_tanh),
                     reads=[("pb", ibg)], writes=["gel"])
                B.op("dve", lambda e: e.tensor_tensor(out=lruo[:, cc, (T - 4) * 512:(T - 3) * 512], in0=th[:], in1=gel[:], op=ALU.mult),
                     reads=["th", "gel"], writes=[("lruo", T - 4)])

        for t in range(-1, 8):
            if t + 1 < 8:
                piece_N(t + 1)
            if t >= 0:
                piece_A(t)
                piece_F(t)
                piece_K(t, range(0, 2), False)
                piece_C(t, 0)
                piece_K(t, range(2, 4), True)
                piece_C(t, 1)
                piece_V(t)
                piece_C(t, 2)
                piece_Q(t)
                piece_C(t, 3)

        B.barrier()
        ar.seek(OFF_P)
        kaug = [ar.alloc([128, NTOK], BF16) for _ in range(2)]
        qaug = [ar.alloc([128, NOWN], BF16) for _ in range(2)]
        PT = [ar.alloc([128, 512], BF16) for _ in range(4)]
        osb = [ar.alloc([128, 512], F32) for _ in range(2)]
        kd_reads = [("kaugc", h) for h in range(NH)] + [("kaug1", t) for t in range(8)] + [("kaugd", t, two) for t in range(8) for two in range(2)] + [("kaugp", t) for t in range(8)]
        qd_reads = [("qaug1", t) for t in range(4)] + [("qaug2", t) for t in range(4)] + [("qaugd", t, two) for t in range(4, 8) for two in range(2)] + [("qaugp", t) for t in range(4, 8)]
        sidx = [0]
        oidx = 0
        dq = Deferred()
        step = 0
        S_LA = 2
        for h in range(NH):
            kb_ = kaug[h % 2]
            qb_ = qaug[h % 2]
            B.dma("sp", kb_[0:KA, :], kaug_d[h], reads=kd_reads, writes=[("kaug", h % 2)])
            B.dma("sp", qb_[0:KA, :], qaug_d[h], reads=qd_reads, writes=[("qaug", h % 2)])
            for G in range(4):
                nkb = 16 + 4 * G + 4
                io = 4 + (oidx % 2)
                oidx += 1
                okey = ("pb", io)
                for kb in range(nkb):
                    m = kb - (16 + 4 * G)
                    q0 = 0 if m < 0 else m * 128
                    isb = sidx[0] % 4
                    sidx[0] += 1
                    skey = ("pb", isb)
                    B.op("pe", lambda e, kb=kb, G=G, q0=q0, isb=isb, kb_=kb_, qb_=qb_, m=m: e.matmul(
                        pb[isb][:, q0:512], lhsT=kb_[0:KA, kb * 128:(kb + 1) * 128], rhs=qb_[0:KA, G * 512 + q0:(G + 1) * 512],
                        start=True, stop=(m < 0)), reads=[("kaug", h % 2), ("qaug", h % 2)], writes=[skey])
                    if m >= 0:
                        B.op("pe", lambda e, q0=q0, isb=isb: e.matmul(pb[isb][:, q0:q0 + 128], lhsT=identb[:], rhs=maskb[:], start=False, stop=True),
                             reads=["identb", "maskb"], writes=[skey])
                    pkey = ("PT", isb)
                    B.op("act", lambda e, q0=q0, isb=isb: e.activation(out=PT[isb][:, q0:512], in_=pb[isb][:, q0:512], func=AF.Exp),
                         reads=[skey], writes=[pkey])

                    def pv(kb=kb, q0=q0, isb=isb, io=io, h=h, nkb=nkb, okey=okey, pkey=pkey):
                        B.op("pe", lambda e: e.matmul(pb[io][0:65, q0:512], lhsT=vaug[:, kb, h, :], rhs=PT[isb][:, q0:512],
                                                      start=(kb == 0), stop=(kb == nkb - 1)),
                             reads=[pkey, ("vaug", kb)], writes=[okey])
                    dq.at(step + S_LA, pv)
                    dq.run(step)
                    step += 1
                ob = osb[oidx % 2]
                obk = ("osb", oidx % 2)

                def norm1(ob=ob, obk=obk, io=io, okey=okey):
                    B.op("dve", lambda e: e.tensor_copy(ob[0:65, :], pb[io][0:65, :]), reads=[okey], writes=[obk])
                    B.op("dve", lambda e: e.reciprocal(ob[64:65, :], ob[64:65, :]), reads=[obk], writes=[obk])

                def norm2(ob=ob, obk=obk, h=h, G=G):
                    ibc = sidx[0] % 4
                    sidx[0] += 1
                    B.op("pe", lambda e: e.matmul(pb[ibc][0:64, :], lhsT=onesf[64:65, 0:64], rhs=ob[64:65, :], start=True, stop=True),
                         reads=[obk, "onesf"], writes=[("pb", ibc)])
                    B.op("dve", lambda e: e.tensor_tensor(out=attT[0:64, h, G * 512:(G + 1) * 512], in0=ob[0:64, :],
                                                          in1=pb[ibc][0:64, :], op=ALU.mult),
                         reads=[obk, ("pb", ibc)], writes=[("attT", h, G)])
                dq.at(step + S_LA - 1, norm1)
                dq.at(step + S_LA + 9, norm2)
        dq.flush()

        B.barrier()
        ar.seek(OFF_P)
        h2tok = ar.alloc([128, 16, D], BF16)
        wgu = [ar.alloc([128, 8, 512], BF16) for _ in range(2)]
        wdn = [ar.alloc([128, 2, D], BF16) for _ in range(2)]
        OFF_P3 = ar.off
        woa = ar.alloc([128, NH, D], BF16)
        wol = ar.alloc([128, 4, D], BF16)
        x3 = [ar.alloc([128, D], F32) for _ in range(2)]
        x1t = [ar.alloc([128, D], F32) for _ in range(2)]
        xq = ar.alloc([128, D], F32)
        h2Tb = [ar.alloc([128, 8, 128], BF16) for _ in range(2)]
        ohb = sb("ohb", [128, 16, NE], BF16)
        cw = sb("cw", [128, 16, 2], F32)
        sidx = sb("sidx", [128, 16, 2], I32)
        ebase1 = sb("ebase1", [128, NE], F32)
        padbase = sb("padbase", [128, NE], F32)
        triuf = sb("triuf", [128, 128], F32)
        triub = sb("triub", [128, 128], BF16)
        onesb = sb("onesb", [128, 128], BF16)
        cntsb = sb("cntsb", [128, NE], F32)
        padi = sb("padi", [128, NE], I32)
        flf = sb("flf", [1, NE, 16], F32)
        flags = sb("flags", [1, NE * 18 + 1], I32)
        flf4 = sb("flf4", [1, NE], F32)
        flf3 = sb("flf3", [1, 1], F32)
        flf2 = sb("flf2", [1, NE], F32)
        svt = sb("svt", [128, 2 * NE + 16], F32)

        B.dma("sp", ebase1[:], ebase1_d, writes=["ebase1"])

        B.dma("sp", padbase[:], padbase_d, writes=["padbase"])
        B.dma("sp", triuf[:], triu_d, writes=["triuf"])
        B.op("dve", lambda e: e.tensor_copy(triub[:], triuf[:]), reads=["triuf"], writes=["triub"])
        B.op("dve", lambda e: e.memset(onesb[:], 1.0), writes=["onesb"])
        B.dma("pool", woa[0:64, :, :], w_out_d[0:512, :].rearrange("(h p) n -> p h n", p=64), writes=["woa"])
        B.dma("pool", wol[:], w_out_d[512:1024, :].rearrange("(c p) n -> p c n", p=128), writes=["wol"])

        def load_expert(e_):
            s = e_ % 2
            B.dma("pool", wgu[s][:, :, 0:256], wg_d[e_].rearrange("(c p) f -> p c f", p=128), writes=[("wg", s)], nobarrier=True)
            B.dma("pool", wgu[s][:, :, 256:512], wu_d[e_].rearrange("(c p) f -> p c f", p=128), writes=[("wu", s)], nobarrier=True)
            B.dma("pool", wdn[s][:], wd_d[e_].rearrange("(c p) n -> p c n", p=128), writes=[("wd", s)], nobarrier=True)

        def p3_block(j, part):
            if part == 1:
                return p3_back(j)
            xb = x3[j % 2]
            B.dma("sp", xb[:], xs[NOWN + j * 128:NOWN + (j + 1) * 128, :], writes=[("x3", j % 2)])
            x1b = x1t[j % 2]
            for half in range(2):
                ib = nextpb()
                for h in range(NH):
                    B.op("pe", lambda e, h=h, half=half, ib=ib: e.matmul(pb[ib][:, :], lhsT=attT[0:64, h, j * 128:(j + 1) * 128],
                                                                      rhs=woa[0:64, h, half * 512:(half + 1) * 512], start=(h == 0), stop=False),
                         reads=[("attT", h, j // 4), "woa"], writes=[("pb", ib)])
                for cc in range(4):
                    B.op("pe", lambda e, cc=cc, half=half, ib=ib: e.matmul(pb[ib][:, :], lhsT=lruo[:, cc, j * 128:(j + 1) * 128],
                                                                        rhs=wol[:, cc, half * 512:(half + 1) * 512], start=False, stop=(cc == 3)),
                         reads=[("lruo", j // 4), "wol"], writes=[("pb", ib)])
                B.op("dve", lambda e, half=half, ib=ib: e.tensor_tensor(out=x1b[:, half * 512:(half + 1) * 512], in0=pb[ib][:, :],
                                                                     in1=xb[:, half * 512:(half + 1) * 512], op=ALU.add),
                     reads=[("pb", ib), ("x3", j % 2)], writes=[("x1t", j % 2, half)])
            x1r = [("x1t", j % 2, 0), ("x1t", j % 2, 1)]
            B.dma("sp", x1_d[j * 128:(j + 1) * 128, :], x1b[:], reads=x1r, writes=[("x1d", j)], semkey=("x1t_out", j % 2))
            B.op("act", lambda e: e.activation(out=xq[:], in_=x1b[:], func=AF.Square, accum_out=ss4[:, 0:1]),
                 reads=x1r, writes=["xq", ("ss4", 0)])
            B.op("dve", lambda e: e.tensor_scalar(out=rstd4[:, 0:1], in0=ss4[:, 0:1], scalar1=1.0 / D, scalar2=1e-6, op0=ALU.mult, op1=ALU.add),
                 reads=[("ss4", 0)], writes=[("rstd4", 0)])
            B.op("pool", lambda e: e.tensor_tensor(out=rstd4[:, 0:1], in0=rstd4[:, 0:1], in1=mhalf[:], op=ALU.pow),
                 reads=[("rstd4", 0), "mhalf"], writes=[("rstd4", 0)])
            B.op("act", lambda e: e.activation(out=xq[:], in_=x1b[:], func=AF.Copy, scale=rstd4[:, 0:1]),
                 reads=x1r + [("rstd4", 0)], writes=["xq"])
            B.op("dve", lambda e: e.tensor_tensor(out=h2tok[:, j, :], in0=xq[:], in1=g2bc[:], op=ALU.mult), reads=["xq", "g2bc"], writes=[("h2tok", j)])
        def p3_back(j):
            ip = nextpt()
            ptv = pt[ip][:].rearrange("p (a b) -> p a b", a=8)
            for c in range(8):
                B.op("pe", lambda e, c=c: e.transpose(ptv[:, c, :], h2tok[:, j, c * 128:(c + 1) * 128], identb[:]),
                     reads=[("h2tok", j), "identb"], writes=[("pt", ip)])
            hT_ = h2Tb[j % 2]
            B.op("act", lambda e: e.activation(out=hT_[:], in_=ptv, func=AF.Copy), reads=[("pt", ip)], writes=[("h2Tb", j % 2)])
            ib = nextpb()
            for dc in range(8):
                B.op("pe", lambda e, dc=dc, ib=ib: e.matmul(pb[ib][:, 0:36], lhsT=hT_[:, dc, :], rhs=wr[:, dc, :],
                                                            start=(dc == 0), stop=(dc == 7)), reads=[("h2Tb", j % 2), "wr"], writes=[("pb", ib)])
            B.op("dve", lambda e, ib=ib: e.tensor_tensor(out=rt[:, 0:36], in0=pb[ib][:, 0:36], in1=rbbc[:], op=ALU.add),
                 reads=[("pb", ib), "rbbc"], writes=["rt"])
            R = dict(reads=["rt"], writes=["rt"])
            B.op("dve", lambda e: e.tensor_reduce(out=rt[:, 36:37], in_=rt[:, 0:4], axis=AX.X, op=ALU.max), **R)
            B.op("dve", lambda e: e.tensor_scalar(out=rt[:, 37:38], in0=rt[:, 36:37], scalar1=-1.0, scalar2=None, op0=ALU.mult), **R)
            B.op("act", lambda e: e.activation(out=rt[:, 38:42], in_=rt[:, 0:4], func=AF.Exp, bias=rt[:, 37:38], accum_out=rt[:, 42:43]), **R)
            B.op("dve", lambda e: e.reciprocal(rt[:, 42:43], rt[:, 42:43]), **R)
            B.op("dve", lambda e: e.tensor_scalar(out=rt[:, 38:42], in0=rt[:, 0:4], scalar1=rt[:, 36:37], scalar2=None, op0=ALU.is_equal), **R)
            B.op("dve", lambda e: e.tensor_scalar(out=rt[:, 44:52], in0=rt[:, 4:12], scalar1=rt[:, 38:39], scalar2=None, op0=ALU.mult), **R)
            for g in range(1, 4):
                B.op("dve", lambda e, g=g: e.scalar_tensor_tensor(out=rt[:, 44:52], in0=rt[:, 4 + 8 * g:12 + 8 * g], scalar=rt[:, 38 + g:39 + g],
                                                                  in1=rt[:, 44:52], op0=ALU.mult, op1=ALU.add), **R)
            B.op("dve", lambda e: e.max(out=rt[:, 52:60], in_=rt[:, 44:52]), **R)
            B.op("dve", lambda e: e.tensor_scalar(out=rt[:, 60:61], in0=rt[:, 52:53], scalar1=-1.0, scalar2=None, op0=ALU.mult), **R)
            B.op("act", lambda e: e.activation(out=rt[:, 4:12], in_=rt[:, 44:52], func=AF.Exp, bias=rt[:, 60:61]), **R)
            B.op("dve", lambda e: e.tensor_scalar(out=rt[:, 12:20], in0=rt[:, 44:52], scalar1=rt[:, 53:54], scalar2=None, op0=ALU.is_ge), **R)
            B.op("dve", lambda e: e.tensor_tensor(out=rt[:, 4:12], in0=rt[:, 4:12], in1=rt[:, 12:20], op=ALU.mult), **R)
            B.op("dve", lambda e: e.tensor_reduce(out=rt[:, 61:62], in_=rt[:, 4:12], axis=AX.X, op=ALU.add), **R)
            B.op("dve", lambda e: e.reciprocal(rt[:, 61:62], rt[:, 61:62]), **R)
            B.op("dve", lambda e: e.tensor_tensor(out=rt[:, 61:62], in0=rt[:, 61:62], in1=rt[:, 42:43], op=ALU.mult), **R)
            for g in range(4):
                B.op("dve", lambda e, g=g: e.tensor_scalar(out=comb[:, j, 8 * g:8 * g + 8], in0=rt[:, 4:12], scalar1=rt[:, 38 + g:39 + g],
                                                           scalar2=rt[:, 61:62], op0=ALU.mult, op1=ALU.mult),
                     reads=["rt"], writes=[("comb", j)])
                B.op("dve", lambda e, g=g: e.tensor_scalar(out=ohb[:, j, 8 * g:8 * g + 8], in0=rt[:, 12:20], scalar1=rt[:, 38 + g:39 + g],
                                                           scalar2=None, op0=ALU.mult),
                     reads=["rt"], writes=[("ohb", j)])

        for j in range(17):
            if j < 16:
                p3_block(j, 0)
            if j >= 1:
                p3_block(j - 1, 1)
            if j == 0:
                load_expert(0)
                load_expert(1)

        ohr = [("ohb", j) for j in range(16)]
        combr = [("comb", j) for j in range(16)]
        ar.seek(OFF_P3)
        pre = ar.alloc([128, 16, NE], F32)
        sva = ar.alloc([128, 16, NE], F32)
        msk = ar.alloc([128, 16, NE], F32)
        ebase16 = ar.alloc([128, 16, NE], F32)
        top8a = ar.alloc([128, 16, 8], F32)
        B.dma("sp", ebase16[:], eb16_d.rearrange("p (a b) -> p a b", a=16), writes=["ebase16", "woa", "wol"])
        ibt = nextpb()
        ibk = nextpb()
        totv = pb[ibt][:, :].rearrange("p (a b) -> p a b", a=16)
        bkv = pb[ibk][:, :].rearrange("p (a b) -> p a b", a=16)
        for j in range(16):
            B.op("pe", lambda e, j=j: e.matmul(totv[:, j, :], lhsT=onesb[:], rhs=ohb[:, j, :], start=True, stop=True),
                 reads=[("ohb", j), "onesb"], writes=[("pb", ibt)])
        for j in range(16):
            B.op("pe", lambda e, j=j: e.matmul(bkv[:, j, :], lhsT=triub[:], rhs=ohb[:, j, :], start=True, stop=True),
                 reads=[("ohb", j), "triub"], writes=[("pb", ibk)])
        PRE = dict(reads=["pre"], writes=["pre"])
        B.op("dve", lambda e: e.memset(pre[:, 0, :], 0.0), reads=["pre"], writes=["pre", "woa", "wol"])
        for j in range(1, 16):
            B.op("dve", lambda e, j=j: e.tensor_tensor(out=pre[:, j, :], in0=totv[:, j - 1, :], in1=pre[:, j - 1, :], op=ALU.add),
                 reads=["pre", ("pb", ibt)], writes=["pre"])
        B.op("dve", lambda e: e.tensor_tensor(out=cntsb[:], in0=totv[:, 15, :], in1=pre[:, 15, :], op=ALU.add),
             reads=["pre", ("pb", ibt)], writes=["cntsb"])
        for k in range(16):
            B.op("dve", lambda e, k=k: e.tensor_scalar(out=flf[0:1, :, k], in0=cntsb[0:1, :], scalar1=128.0 * k, scalar2=None, op0=ALU.is_gt),
                 reads=["cntsb"], writes=["flf"])
        B.op("dve", lambda e: e.tensor_copy(flags[:, 0:NE * 16], flf[:].rearrange("p a b -> p (a b)")), reads=["flf"], writes=["flags0"])
        B.op("dve", lambda e: e.tensor_scalar(out=flf2[0:1, :], in0=cntsb[0:1, :], scalar1=256.0, scalar2=None, op0=ALU.is_gt),
             reads=["cntsb"], writes=["flf2"])
        B.op("dve", lambda e: e.tensor_copy(flags[:, NE * 16:NE * 17], flf2[:]), reads=["flf2", "flags0"], writes=["flags1"])
        B.op("dve", lambda e: e.tensor_reduce(out=flf3[:], in_=flf2[:], axis=AX.X, op=ALU.max), reads=["flf2"], writes=["flf3"])
        B.op("dve", lambda e: e.tensor_copy(flags[:, NE * 17:NE * 17 + 1], flf3[:]), reads=["flf3", "flags1"], writes=["flags2"])
        B.op("dve", lambda e: e.tensor_tensor(out=flf4[:], in0=flf[0:1, :, 0], in1=flf[0:1, :, 1], op=ALU.subtract), reads=["flf"], writes=["flf4"])
        B.op("dve", lambda e: e.tensor_copy(flags[:, NE * 17 + 1:NE * 18 + 1], flf4[:]), reads=["flf4", "flags2"], writes=["flags"])
        B.flags_ap = flags
        B.flags_key = "flags"
        B.op("dve", lambda e: e.tensor_tensor(out=svt[:, 0:NE], in0=cntsb[:], in1=padbase[:], op=ALU.add), reads=["cntsb", "padbase"], writes=["svt"])
        B.op("dve", lambda e: e.tensor_copy(padi[:], svt[:, 0:NE]), reads=["svt"], writes=["padi"])
        B.op("dve", lambda e: e.tensor_tensor(out=sva[:], in0=bkv, in1=pre[:], op=ALU.add), reads=["pre", ("pb", ibk)], writes=["sva"])
        B.op("dve", lambda e: e.tensor_tensor(out=sva[:], in0=sva[:], in1=ebase16[:], op=ALU.add), reads=["sva", "ebase16"], writes=["sva"])
        B.op("dve", lambda e: e.tensor_tensor(out=sva[:], in0=sva[:], in1=ohb[:], op=ALU.mult), reads=["sva"] + ohr, writes=["sva"])
        for j in range(16):
            B.op("dve", lambda e, j=j: e.max(out=top8a[:, j, :], in_=sva[:, j, :]), reads=["sva"], writes=["top8a"])
        for q in range(2):
            B.op("dve", lambda e, q=q: e.tensor_tensor(out=msk[:], in0=sva[:], in1=top8a[:, :, q:q + 1].to_broadcast([128, 16, NE]), op=ALU.is_equal),
                 reads=["sva", "top8a"], writes=["msk"])
            B.op("dve", lambda e: e.tensor_tensor(out=msk[:], in0=msk[:], in1=comb[:], op=ALU.mult), reads=["msk"] + combr, writes=["msk"])
            B.op("dve", lambda e, q=q: e.tensor_reduce(out=cw[:, :, q], in_=msk[:], axis=AX.X, op=ALU.add), reads=["msk"], writes=[("cwq", q)])
        B.op("dve", lambda e: e.tensor_scalar(out=svt[:, 0:32].rearrange("p (a b) -> p a b", a=16), in0=top8a[:, :, 0:2], scalar1=-1.0, scalar2=None, op0=ALU.add),
             reads=["top8a", "svt"], writes=["svt"])
        B.op("dve", lambda e: e.tensor_copy(sidx[:], svt[:, 0:32].rearrange("p (a b) -> p a b", a=16)), reads=["svt"], writes=[("sidx", j) for j in range(16)])

        B.barrier()
        ar.seek(OFF_P3)
        xin = [ar.alloc([128, D], BF16) for _ in range(4)]
        xT = [ar.alloc([128, 8, 128], BF16) for _ in range(2)]
        sg = [ar.alloc([128, 256], F32) for _ in range(2)]
        hid = [ar.alloc([128, 256], BF16) for _ in range(2)]
        hidT = [ar.alloc([128, 2, 128], BF16) for _ in range(2)]
        ysb = [ar.alloc([128, D], BF16) for _ in range(2)]
        zt = ar.alloc([128, D], BF16)
        assert ar.off <= OFF_P3 + 24 * 1024, ar.off - OFF_P3
        B.op("dve", lambda e: e.memset(zt[:], 0.0), writes=["zt"])

        regcache = {}

        def bcreg(e):
            if "bc" not in regcache:
                regcache["bc"] = e.to_reg(NE * CAP - 1)
            return regcache["bc"]

        def ind_scatter(src_ap, idx_ap, reads, wkey, grp):
            def fn(e):
                return e.indirect_dma_start(out=xs_scr[:, :], out_offset=bass.IndirectOffsetOnAxis(ap=idx_ap, axis=0),
                                            in_=src_ap, in_offset=None, bounds_check=bcreg(e), oob_is_err=False)
            B.op("pool", fn, reads=reads, writes=[wkey], dma=True, semgroup=grp)

        tok_keys = []
        for j in range(16):
            for q in range(2):
                ind_scatter(h2tok[:, j, :], sidx[:, j, q:q + 1], [("h2tok", j), ("sidx", j)], ("sct", j, q), "scat")
                tok_keys.append(("sct", j, q))
        for e_ in range(NE):
            ind_scatter(zt[:, :], padi[:, e_:e_ + 1], ["zt", "padi"], ("scz", e_), "scz%d" % (e_ // 4))
        scat_keys = tok_keys + [("scz", e_) for e_ in range(NE)]

        ys_keys = []
        tcount = [0]

        def moe_tiles(e_, ks, flag):
            s = e_ % 2
            wkeys = [("wg", s), ("wu", s)]
            B.begin_cond(flag)
            T_ = []
            for k in ks:
                tno = tcount[0]
                tcount[0] += 1
                T_.append(dict(k=k, b2=tno % 2, igu=tno % 2, iy=2 + 2 * (tno % 2), row0=e_ * CAP + k * 128,
                               bx=(e_ % 2) * 2 + (k % 2)))
            for t in T_:
                B.dma("sp", xin[t["bx"]][:], xs_scr[t["row0"]:t["row0"] + 128, :],
                      reads=tok_keys + [("scz", e2) for e2 in range((e_ // 4) * 4, (e_ // 4) * 4 + 4)], writes=[("xin", t["bx"])])
            for t in T_:
                ip = nextpt()
                t["ip"] = ip
                t["ptv"] = pt[ip][:].rearrange("p (a b) -> p a b", a=8)
                for c in range(8):
                    B.op("pe", lambda e, c=c, t=t: e.transpose(t["ptv"][:, c, :], xin[t["bx"]][:, c * 128:(c + 1) * 128], identb[:]),
                         reads=[("xin", t["bx"]), "identb"], writes=[("pt", ip)])
                B.op("dve", lambda e, t=t: e.tensor_copy(xT[t["b2"]][:], t["ptv"]), reads=[("pt", ip)], writes=[("xT", t["b2"])])
            for t in T_:
                for dc in range(8):
                    B.op("pe", lambda e, dc=dc, t=t: e.matmul(pb[t["igu"]][:, :], lhsT=xT[t["b2"]][:, dc, :], rhs=wgu[s][:, dc, :],
                                                              start=(dc == 0), stop=(dc == 7)),
                         reads=[("xT", t["b2"])] + wkeys, writes=[("pb", t["igu"])])
                B.op("act", lambda e, t=t: e.activation(out=sg[t["b2"]][:], in_=pb[t["igu"]][:, 0:256], func=AF.Silu),
                     reads=[("pb", t["igu"])], writes=[("sg", t["b2"])])
                B.op("dve", lambda e, t=t: e.tensor_tensor(out=hid[t["b2"]][:], in0=pb[t["igu"]][:, 256:512], in1=sg[t["b2"]][:], op=ALU.mult),
                     reads=[("pb", t["igu"]), ("sg", t["b2"])], writes=[("hid", t["b2"])])
            for t in T_:
                ip2 = nextpt()
                ptv2 = pt[ip2][:, 0:256].rearrange("p (a b) -> p a b", a=2)
                for fc_ in range(2):
                    B.op("pe", lambda e, fc_=fc_, t=t, ptv2=ptv2: e.transpose(ptv2[:, fc_, :], hid[t["b2"]][:, fc_ * 128:(fc_ + 1) * 128], identb[:]),
                         reads=[("hid", t["b2"]), "identb"], writes=[("pt", ip2)])
                B.op("dve", lambda e, t=t, ptv2=ptv2: e.tensor_copy(hidT[t["b2"]][:], ptv2), reads=[("pt", ip2)], writes=[("hidT", t["b2"])])
            for t in T_:
                iy, b2 = t["iy"], t["b2"]
                for half in range(2):
                    for fc_ in range(2):
                        B.op("pe", lambda e, fc_=fc_, half=half, iy=iy, b2=b2: e.matmul(pb[iy + half][:, :], lhsT=hidT[b2][:, fc_, :],
                                                                                      rhs=wdn[s][:, fc_, half * 512:(half + 1) * 512],
                                                                                      start=(fc_ == 0), stop=(fc_ == 1)),
                             reads=[("hidT", b2), ("wd", s)], writes=[("pb", iy + half)])
                B.op("act", lambda e, iy=iy, b2=b2: e.activation(out=ysb[b2][:, 0:512], in_=pb[iy][:, :], func=AF.Copy),
                     reads=[("pb", iy)], writes=[("ysb", b2, 0)])
                B.op("dve", lambda e, iy=iy, b2=b2: e.tensor_copy(ysb[b2][:, 512:1024], pb[iy + 1][:, :]),
                     reads=[("pb", iy + 1)], writes=[("ysb", b2, 1)])
                ykey = ("ysd", e_, t["k"], flag)
                B.dma("act", ys_scr[t["row0"]:t["row0"] + 128, :], ysb[b2][:], reads=[("ysb", b2, 0), ("ysb", b2, 1)], writes=[ykey],
                      semkey=("ys_out", b2))
                ys_keys.append(ykey)
            B.end_cond()

        def moe_tile(e_, k):
            moe_tiles(e_, [k], e_ * 16 + k)

        for e_ in range(NE):
            moe_tiles(e_, [0, 1], e_ * 16 + 1)
            moe_tiles(e_, [0], NE * 17 + 1 + e_)
            if e_ + 2 < NE:
                load_expert(e_ + 2)

        wst = zt.bitcast(F32)
        B.begin_cond(NE * 17)
        for e_ in range(NE):
            s = e_ % 2
            B.begin_cond(NE * 16 + e_)
            for dc in range(8):
                B.dma("sp", wst[:, 0:256], wg_d[e_, dc * 128:(dc + 1) * 128, :], reads=scat_keys, writes=["zt"], semkey="wst")
                B.op("dve", lambda e, dc=dc, s=s: e.tensor_copy(wgu[s][:, dc, 0:256], wst[:, 0:256]), reads=["zt"], writes=[("wg", s)])
                B.dma("sp", wst[:, 256:512], wu_d[e_, dc * 128:(dc + 1) * 128, :], reads=scat_keys, writes=["zt2"], semkey="wst2")
                B.op("dve", lambda e, dc=dc, s=s: e.tensor_copy(wgu[s][:, dc, 256:512], wst[:, 256:512]), reads=["zt2"], writes=[("wu", s)])
            for fc_ in range(2):
                for half in range(2):
                    B.dma("sp", wst[:, :], wd_d[e_, fc_ * 128:(fc_ + 1) * 128, half * 512:(half + 1) * 512], reads=scat_keys,
                          writes=["zt", "zt2"], semkey="wst")
                    B.op("dve", lambda e, fc_=fc_, half=half, s=s: e.tensor_copy(wdn[s][:, fc_, half * 512:(half + 1) * 512], wst[:, :]),
                         reads=["zt", "zt2"], writes=[("wd", s)])
            for k in range(2, 16):
                moe_tile(e_, k)
            B.end_cond()
        B.end_cond()

        B.barrier()
        ar.seek(OFF_P + 32 * 1024)
        rg = [ar.alloc([128, D], BF16) for _ in range(8)]
        B.dma("sp", g2bc[:], g3bc_d, reads=[], writes=["g2bc"], semkey="g3load")
        def ind_gather(dst_ap, idx_ap, reads, wkey):
            def fn(e):
                return e.indirect_dma_start(out=dst_ap, out_offset=None, in_=ys_scr[:, :],
                                            in_offset=bass.IndirectOffsetOnAxis(ap=idx_ap, axis=0),
                                            bounds_check=bcreg(e), oob_is_err=False)
            B.op("pool", fn, reads=reads, writes=[wkey], dma=True)

        for j in range(16):
            xb = x3[j % 2]
            ob = x1t[j % 2]
            r1 = rg[(j % 4) * 2]
            r2 = rg[(j % 4) * 2 + 1]
            ind_gather(r1[:, :], sidx[:, j, 0:1], ys_keys + [("sidx", j)], ("rg", (j % 4) * 2))
            ind_gather(r2[:, :], sidx[:, j, 1:2], ys_keys + [("sidx", j)], ("rg", (j % 4) * 2 + 1))
            B.dma("sp", xb[:], x1_d[j * 128:(j + 1) * 128, :], reads=[("x1d", j)], writes=[("x3", j % 2)])
            B.op("dve", lambda e, xb=xb, r1=r1, j=j: e.scalar_tensor_tensor(out=xb[:], in0=r1[:], scalar=cw[:, j, 0:1], in1=xb[:],
                                                                        op0=ALU.mult, op1=ALU.add),
                 reads=[("x3", j % 2), ("rg", (j % 4) * 2), ("cwq", 0)], writes=[("x3", j % 2)])
            B.op("dve", lambda e, xb=xb, r2=r2, j=j: e.scalar_tensor_tensor(out=xb[:], in0=r2[:], scalar=cw[:, j, 1:2], in1=xb[:],
                                                                        op0=ALU.mult, op1=ALU.add),
                 reads=[("x3", j % 2), ("rg", (j % 4) * 2 + 1), ("cwq", 1)], writes=[("x3", j % 2)])
            pj = j % 2
            obk = [("x1t", pj, 0), ("x1t", pj, 1)]
            B.op("act", lambda e, xb=xb, ob=ob, pj=pj: e.activation(out=ob[:], in_=xb[:], func=AF.Square, accum_out=ss4[:, pj:pj + 1]),
                 reads=[("x3", pj)], writes=obk + [("ss4", pj)])
            B.op("dve", lambda e, pj=pj: e.tensor_scalar(out=rstd4[:, pj:pj + 1], in0=ss4[:, pj:pj + 1], scalar1=1.0 / D, scalar2=1e-6, op0=ALU.mult, op1=ALU.add),
                 reads=[("ss4", pj)], writes=[("rstd4", pj)])
            B.op("pool", lambda e, pj=pj: e.tensor_tensor(out=rstd4[:, pj:pj + 1], in0=rstd4[:, pj:pj + 1], in1=mhalf[:], op=ALU.pow),
                 reads=[("rstd4", pj), "mhalf"], writes=[("rstd4", pj)])
            B.op("act", lambda e, xb=xb, ob=ob, pj=pj: e.activation(out=ob[:], in_=xb[:], func=AF.Copy, scale=rstd4[:, pj:pj + 1]),
                 reads=[("x3", pj), ("rstd4", pj)], writes=obk)
            B.op("dve", lambda e, ob=ob: e.tensor_tensor(out=ob[:], in0=ob[:], in1=g3bc[:], op=ALU.mult), reads=obk + ["g2bc"], writes=obk)
            B.dma("sp", out_d[j * 128:(j + 1) * 128, :], ob[:], reads=[("x1t", j % 2, 0), ("x1t", j % 2, 1)], writes=[("outd", j)],
                  semkey=("ob_out", j % 2))

        if os.environ.get("MK_NOPS"):
            B.ops = B.ops[:int(os.environ["MK_NOPS"])]
        B.emit(st)
        _NC_CACHE["B"] = B
    return nc


_NC_CACHE = {}


def _layout_inputs(inp):
    f = lambda a: np.ascontiguousarray(np.asarray(a, dtype=np.float32))
    x = f(inp["x"])
    common = {}
    common["ident"] = np.eye(128, dtype=np.float32)
    common["onesd"] = np.ones((NH, 3, 512), ml_dtypes.bfloat16)
    ee = np.arange(NE, dtype=np.float32)[None, :] * CAP
    common["ebase1"] = f(np.broadcast_to(ee + 1.0, (128, NE)))
    common["ebase16"] = f(np.broadcast_to((ee + 1.0)[:, None, :], (128, 16, NE)).reshape(128, 16 * NE))
    common["padbase"] = f(ee + np.arange(128, dtype=np.float32)[:, None])
    common["triu"] = (np.arange(128)[:, None] < np.arange(128)[None, :]).astype(np.float32)
    k = np.arange(128)
    common["maskT"] = np.where(k[:, None] <= k[None, :], 0.0, NEG).astype(np.float32)
    common["gcol"] = f(inp["mix_norm"][0].reshape(8, 128).T)
    common["w_in"] = f(inp["w_in"][0])
    common["bfcol"] = f(inp["b_forget"][0].reshape(8, 1))
    cw = f(inp["conv_w"][0])
    cd = np.zeros((128, 16, 128), np.float32)
    for tap in range(4):
        for cc in range(4):
            cd[k, tap * 4 + cc, k] = cw[tap, cc * 128:(cc + 1) * 128]
    common["cdiag"] = cd
    common["convb"] = f(inp["conv_b"][0].reshape(4, 128).T)
    for nm, key in (("bda", "w_a"), ("bdx", "w_x")):
        w = f(inp[key][0])
        bd = np.zeros((128, 4, 128), np.float32)
        for cc in range(4):
            bd[0:64, cc, 0:64] = w[2 * cc]
            bd[64:128, cc, 64:128] = w[2 * cc + 1]
        common[nm] = bd
    common["bacol"] = f(inp["b_a"][0].reshape(4, 128).T)
    common["bxcol"] = f(inp["b_x"][0].reshape(4, 128).T)
    common["lamcol"] = f(inp["lru_lambda"][0].reshape(4, 128).T)
    common["w_out"] = f(inp["w_out"][0])
    common["g2bc"] = f(np.broadcast_to(inp["ffn_norm"][0][None, :], (128, D)))
    common["g3bc"] = f(np.broadcast_to(np.asarray(inp["final_norm"])[None, :], (128, D)))
    wi = np.asarray(inp["w_inner"][0], dtype=np.float32)
    common["wr"] = f(np.concatenate([np.asarray(inp["w_group"][0], dtype=np.float32), wi.transpose(1, 0, 2).reshape(D, 32)], axis=1))
    rb = np.concatenate([np.asarray(inp["b_group"][0], dtype=np.float32), np.asarray(inp["b_inner"][0], dtype=np.float32).reshape(32)])
    common["rbbc"] = f(np.broadcast_to(rb[None, :], (128, 36)))
    common["w_gate"] = f(np.asarray(inp["w_gate"][0]).reshape(NE, D, FE))
    common["w_up"] = f(np.asarray(inp["w_up"][0]).reshape(NE, D, FE))
    common["w_down"] = f(np.asarray(inp["w_down"][0]).reshape(NE, FE, D))
    maps = []
    for c in range(8):
        b, half = c // 2, c % 2
        m = dict(common)
        if half == 1:
            m["xs"] = f(x[b])
            m["kmrow"] = np.zeros((1, NTOK), ml_dtypes.bfloat16)
        else:
            m["xs"] = f(np.concatenate([np.zeros((NOWN, D), np.float32), x[b, :NOWN]], axis=0))
            km = np.zeros((1, NTOK), np.float32)
            km[0, :NOWN] = NEG
            m["kmrow"] = km.astype(ml_dtypes.bfloat16)
        m["flag"] = np.full((128, 1), float(half), np.float32)
        maps.append(m)
    return maps


def kernel(**inputs):
    if "nc" not in _NC_CACHE:
        _NC_CACHE["nc"] = build_program()
    nc = _NC_CACHE["nc"]
    maps = _layout_inputs(inputs)
    res = run_bass_kernel_spmd(nc, maps, core_ids=list(range(8)))
    out = np.zeros((4, SEQ, D), np.float32)
    for c in range(8):
        b, half = c // 2, c % 2
        out[b, half * NOWN:(half + 1) * NOWN] = res.results[c]["out"]
    return out
```

```python
import os
import numpy as np
import ml_dtypes
from contextlib import ExitStack
import concourse.bass as bass
import concourse.mybir as mybir
from concourse.bass_utils import run_bass_kernel_spmd

F32 = mybir.dt.float32
BF16 = mybir.dt.bfloat16
AF = mybir.ActivationFunctionType
ALU = mybir.AluOpType
AX = mybir.AxisListType

ENGS = ("sp", "act", "dve", "pool", "pe")

D = 1024
SEQ = 4096
NTOK = 4096
NOWN = 2048
NH = 8
HD = 64
KA = 71
NEG = -30000.0
NE = 32
FE = 256
CAP = 2048 + 128
I32 = mybir.dt.int32
STAGE = int(os.environ.get("MK_STAGE", "9"))


class Builder:
    def __init__(self, nc):
        self.nc = nc
        self.ops = []
        self.last_write = {}
        self.reads_since = {}
        self.same_engine_sync = {"act": True, "dve": True, "pool": True, "pe": False, "sp": False}
        self.barrier_deps = set()
        self.names = {}
        self.cur_cond = None
        self.flags_ap = None
        self.flags_key = None

    def op(self, eng, fn, reads=(), writes=(), dma=False, nobarrier=False, semgroup=None, semkey=None):
        deps = set()
        for b in reads:
            w = self.last_write.get(b)
            if w is not None:
                deps.add(w)
        for b in writes:
            w = self.last_write.get(b)
            if w is not None:
                deps.add(w)
            deps.update(self.reads_since.get(b, ()))
        if not nobarrier:
            deps.update(self.barrier_deps)
        idx = len(self.ops)
        self.ops.append(dict(eng=eng, fn=fn, deps=deps, dma=dma, wkey=(("grp", semgroup) if semgroup else (("sk", semkey) if semkey else (writes[0] if writes else None))),
                             grp=semgroup is not None, cond=self.cur_cond))
        for b in reads:
            self.reads_since.setdefault(b, []).append(idx)
        for b in writes:
            self.last_write[b] = idx
            self.reads_since[b] = []
        return idx

    def dma(self, eng, out, in_, reads=(), writes=(), nobarrier=False, semgroup=None, semkey=None, **kw):
        def fn(e):
            return e.dma_start(out=out, in_=in_, **kw)
        return self.op(eng, fn, reads, writes, dma=True, nobarrier=nobarrier, semgroup=semgroup, semkey=semkey)

    def begin_cond(self, c):
        self.cur_cond = (self.cur_cond or ()) + (c,)

    def end_cond(self):
        self.cur_cond = self.cur_cond[:-1] or None

    def barrier(self):
        last = {}
        deps = set()
        for i, o in enumerate(self.ops):
            if o["dma"]:
                deps.add(i)
            else:
                last[o["eng"]] = i
        latest = {}
        for i in deps:
            latest[self.ops[i]["wkey"]] = max(latest.get(self.ops[i]["wkey"], -1), i)
        self.barrier_deps = set(latest.values()) | set(last.values())

    def emit(self, stack):
        nc = self.nc
        ops = self.ops
        n = len(ops)
        needed = [False] * n
        for i, o in enumerate(ops):
            pruned = set()
            for d in o["deps"]:
                od = ops[d]
                if (not od["dma"]) and od["eng"] == o["eng"] and not self.same_engine_sync[o["eng"]]:
                    continue
                pruned.add(d)
            o["deps"] = pruned
            for d in pruned:
                needed[d] = True
        if self.flags_key is not None:
            needed[self.last_write[self.flags_key]] = True
        eng_sem = {e: stack.enter_context(nc.semaphore("S_" + e)) for e in ENGS}
        dma_sems = {}
        dma_count = {}
        eng_count = {e: 0 for e in ENGS}
        for i, o in enumerate(ops):
            if o["dma"]:
                k = o["wkey"]
                if k not in dma_sems:
                    dma_sems[k] = stack.enter_context(nc.semaphore("D%d" % len(dma_sems)))
                    dma_count[k] = 0
                dma_count[k] += 16
                o["sig"] = (dma_sems[k], dma_count[k], ("d", k))
            elif needed[i]:
                eng_count[o["eng"]] += 1
                o["sig"] = (eng_sem[o["eng"]], eng_count[o["eng"]], ("e", o["eng"]))
            else:
                o["sig"] = None
        for o in ops:
            if o["dma"] and o["grp"]:
                sem, val, key = o["sig"]
                o["sig"] = (sem, dma_count[o["wkey"]], key)
        self.n_sems = len(dma_sems) + len(ENGS)
        print("ops", n, "sems", self.n_sems)
        per_eng = {e: [] for e in ENGS}
        for i, o in enumerate(ops):
            per_eng[o["eng"]].append(i)
        final_waits = [(dma_sems[k], dma_count[k], ("d", k)) for k in dma_sems]

        flag_sig = None
        if self.flags_key is not None:
            fo = ops[self.last_write[self.flags_key]]
            flag_sig = fo["sig"]
            assert flag_sig is not None

        def run_engine(ename, e):
            known = {}
            reg = None

            def emit_op(i):
                o = ops[i]
                need = {}
                for d in o["deps"]:
                    sem, val, key = ops[d]["sig"]
                    if known.get(key, 0) >= val:
                        continue
                    if key not in need or need[key][1] < val:
                        need[key] = (sem, val)
                for key, (sem, val) in need.items():
                    e.wait_ge(sem, val)
                    known[key] = val
                ins = o["fn"](e)
                try:
                    self.names[ins.ins.name] = i
                except Exception:
                    pass
                if o["sig"] is not None:
                    ins.then_inc(o["sig"][0], 16 if o["dma"] else 1)

            def emit_list(idxs, depth):
                nonlocal reg
                p = 0
                while p < len(idxs):
                    cpath = ops[idxs[p]]["cond"] or ()
                    if len(cpath) <= depth:
                        emit_op(idxs[p])
                        p += 1
                        continue
                    c = cpath[depth]
                    blk = []
                    while p < len(idxs):
                        cp2 = ops[idxs[p]]["cond"] or ()
                        if len(cp2) > depth and cp2[depth] == c:
                            blk.append(idxs[p])
                            p += 1
                        else:
                            break
                    if reg is None:
                        reg = e.alloc_register("cr_" + ename)
                    sem, val, key = flag_sig
                    if known.get(key, 0) < val:
                        e.wait_ge(sem, val)
                        known[key] = val
                    e.reg_load(reg, self.flags_ap[0:1, c:c + 1])
                    snap = dict(known)
                    with e.If(reg):
                        emit_list(blk, depth + 1)
                    known.clear()
                    known.update(snap)
                    comp = {}
                    for i in blk:
                        sg_ = ops[i]["sig"]
                        if sg_ is None:
                            continue
                        sem, val, key = sg_
                        inc = 16 if ops[i]["dma"] else 1
                        if key not in comp:
                            comp[key] = [sem, val - inc, 0]
                        comp[key][2] += inc
                    if comp:
                        with e.Else():
                            for key, (sem, prev, tot) in comp.items():
                                if prev > 0:
                                    e.wait_ge(sem, prev)
                                e.sem_inc(sem, tot)

            emit_list(per_eng[ename], 0)
            if ename == "sp":
                for sem, val, key in final_waits:
                    if known.get(key, 0) < val:
                        e.wait_ge(sem, val)

        block = stack.enter_context(nc.Block())

        @block.sync
        def _(e):
            run_engine("sp", e)

        @block.scalar
        def _(e):
            run_engine("act", e)

        @block.vector
        def _(e):
            run_engine("dve", e)

        @block.gpsimd
        def _(e):
            run_engine("pool", e)

        @block.tensor
        def _(e):
            run_engine("pe", e)


class Deferred:
    def __init__(self):
        self.q = []
        self.n = 0

    def at(self, target, fn):
        self.q.append((target, self.n, fn))
        self.n += 1

    def run(self, now):
        ready = sorted([x for x in self.q if x[0] <= now])
        self.q = [x for x in self.q if x[0] > now]
        for _, _, fn in ready:
            fn()

    def flush(self):
        self.run(1 << 60)


class Arena:
    def __init__(self, nc, stack, nbytes):
        self.t = stack.enter_context(nc.sbuf_tensor("arena", [128, nbytes // 4], F32))
        self.nbytes = nbytes
        self.off = 0

    def seek(self, off):
        self.off = off

    def alloc(self, shape, dt):
        n = 1
        for s in shape[1:]:
            n *= s
        esz = 4 if dt == F32 else 2
        nb = (n * esz + 31) // 32 * 32
        assert self.off + nb <= self.nbytes, (self.off, nb, self.nbytes)
        v = self.t[:, self.off // 4:(self.off + nb) // 4]
        if dt != F32:
            v = v.bitcast(dt)
        v = v[:, 0:n]
        if len(shape) == 3:
            v = v.rearrange("p (a b) -> p a b", a=shape[1])
        elif len(shape) == 4:
            v = v.rearrange("p (a b c) -> p a b c", a=shape[1], b=shape[2])
        self.off += nb
        return v


def build_program():
    nc = bass.Bass("TRN2", target_bir_lowering=False)
    din = lambda name, shape, dt=F32: nc.dram_tensor(name, list(shape), dt, kind="ExternalInput").ap()
    xs = din("xs", [NTOK, D])
    ident_d = din("ident", [128, 128])
    maskT_d = din("maskT", [128, 128])
    flag_d = din("flag", [128, 1])
    kmrow_d = din("kmrow", [1, NTOK], BF16)
    ones_d = din("onesd", [NH, 3, 512], BF16)
    gcol_d = din("gcol", [128, 8])
    w_in_d = din("w_in", [D, 2568])
    bf_d = din("bfcol", [8, 1])
    cdiag_d = din("cdiag", [128, 16, 128])
    convb_d = din("convb", [128, 4])
    bda_d = din("bda", [128, 4, 128])
    bdx_d = din("bdx", [128, 4, 128])
    ba_d = din("bacol", [128, 4])
    bx_d = din("bxcol", [128, 4])
    lam_d = din("lamcol", [128, 4])
    w_out_d = din("w_out", [D, D])
    g2bc_d = din("g2bc", [128, D])
    g3bc_d = din("g3bc", [128, D])
    wr_d = din("wr", [D, 36])
    rb_d = din("rbbc", [128, 36])
    wg_d = din("w_gate", [NE, D, FE])
    wu_d = din("w_up", [NE, D, FE])
    wd_d = din("w_down", [NE, FE, D])
    ebase1_d = din("ebase1", [128, NE])
    eb16_d = din("ebase16", [128, 16 * NE])
    padbase_d = din("padbase", [128, NE])
    triu_d = din("triu", [128, 128])
    out_d = nc.dram_tensor("out", [NOWN, D], F32, kind="ExternalOutput").ap()
    xs_scr = nc.dram_tensor("xs_scr", [NE * CAP, D], BF16).ap()
    ys_scr = nc.dram_tensor("ys_scr", [NE * CAP, D], BF16).ap()
    kaug_d = nc.dram_tensor("kaug_s", [NH, KA, NTOK], BF16).ap()
    qaug_d = nc.dram_tensor("qaug_s", [NH, KA, NOWN], BF16).ap()
    x1_d = nc.dram_tensor("x1_s", [NOWN, D], F32).ap()

    st = ExitStack()
    with st:
        B = Builder(nc)
        sb = lambda name, shape, dt: st.enter_context(nc.sbuf_tensor("s_" + name, shape, dt))
        identf = sb("identf", [128, 128], F32)
        identb = sb("identb", [128, 128], BF16)
        maskf = sb("maskf", [128, 128], F32)
        maskb = sb("maskb", [128, 128], BF16)
        onesf = sb("onesf", [128, 64], F32)
        flag = sb("flag", [128, 1], F32)
        gcol = sb("gcol", [128, 8], F32)
        bfcol = sb("bfcol", [8, 1], F32)
        nbfcol = sb("nbfcol", [8, 1], F32)
        convb = sb("convbc", [128, 4], F32)
        bacol = sb("bacol", [128, 4], F32)
        bxcol = sb("bxcol", [128, 4], F32)
        lamcol = sb("lamcol", [128, 4], F32)
        s1col = sb("s1col", [128, 4], F32)
        s2col = sb("s2col", [128, 4], F32)
        hbacol = sb("hbacol", [128, 4], F32)
        hbxcol = sb("hbxcol", [128, 4], F32)
        hs1col = sb("hs1col", [128, 4], F32)
        hs2col = sb("hs2col", [128, 4], F32)
        qcol = sb("qcol", [128, 1], F32)
        mhalf = sb("mhalf", [128, 1], F32)
        mhalf2 = sb("mhalf2", [128, 2], F32)
        g2bc = sb("g2bc", [128, D], F32)
        g3bc = g2bc
        rbbc = sb("rbbc", [128, 36], F32)
        wr = sb("wr", [128, 8, 36], BF16)
        comb = sb("comb", [128, 16, NE], F32)
        hstate = sb("hstate", [128, 4], F32)
        cstate = sb("cstate", [8, 1], F32)
        ss4 = sb("ss4", [128, 4], F32)
        rstd4 = sb("rstd4", [128, 4], F32)
        rt = sb("rt", [128, 64], F32)

        ar = Arena(nc, st, 186 * 1024)
        OFF_V = 0
        OFF_LRU = OFF_V + 34 * 1024
        OFF_ATT = OFF_LRU + 16 * 1024
        OFF_P = OFF_ATT + 32 * 1024
        ar.seek(OFF_V)
        vaug = ar.alloc([128, 32, NH, 65], BF16)
        ar.seek(OFF_LRU)
        lruo = ar.alloc([128, 4, NOWN], BF16)
        ar.seek(OFF_ATT)
        attT = ar.alloc([128, NH, NOWN], BF16)

        pb = [st.enter_context(nc.psum_tensor("pb%d" % i, [128, 512], F32)) for i in range(6)]
        pt = [st.enter_context(nc.psum_tensor("pt%d" % i, [128, 1024], BF16)) for i in range(2)]
        rr = {"pb": 0, "pt": 0}

        def nextpb():
            i = rr["pb"]
            rr["pb"] = (i + 1) % 6
            return i

        def nextpt():
            i = rr["pt"]
            rr["pt"] = (i + 1) % 2
            return i

        B.dma("sp", identf[:], ident_d, writes=["identf"], semgroup="csp")
        B.dma("sp", maskf[:], maskT_d, writes=["maskf"], semgroup="csp")
        B.dma("sp", flag[:], flag_d, writes=["flag"], semgroup="csp")
        B.dma("sp", gcol[:], gcol_d, writes=["gcol"], semgroup="csp")
        B.dma("sp", bfcol[:], bf_d, writes=["bfcol"], semgroup="csp")
        B.dma("sp", convb[:], convb_d, writes=["convb"], semgroup="csp")
        B.dma("sp", bacol[:], ba_d, writes=["bacol"], semgroup="csp")
        B.dma("sp", bxcol[:], bx_d, writes=["bxcol"], semgroup="csp")
        B.dma("sp", lamcol[:], lam_d, writes=["lamcol"], semgroup="csp")
        B.dma("sp", g2bc[:], g2bc_d, writes=["g2bc"], semgroup="csp")
        B.dma("sp", rbbc[:], rb_d, writes=["rbbc"], semgroup="csp")
        B.dma("pool", wr[:], wr_d.rearrange("(c p) f -> p c f", p=128), writes=["wr"], semgroup="cpl")
        B.op("dve", lambda e: e.tensor_copy(identb[:], identf[:]), reads=["identf"], writes=["identb"])
        B.op("dve", lambda e: e.tensor_copy(maskb[:], maskf[:]), reads=["maskf"], writes=["maskb"])
        B.op("dve", lambda e: e.memset(onesf[:], 1.0), writes=["onesf"])
        B.op("dve", lambda e: e.memset(hstate[:], 0.0), writes=["hstate"])
        B.op("dve", lambda e: e.memset(cstate[:], 0.0), writes=["cstate"])
        B.op("dve", lambda e: e.tensor_scalar(out=nbfcol[:], in0=bfcol[:], scalar1=-1.0, scalar2=None, op0=ALU.mult),
             reads=["bfcol"], writes=["nbfcol"])
        B.op("act", lambda e: e.activation(out=s1col[:], in_=lamcol[:], func=AF.Exp, scale=-1.0), reads=["lamcol"], writes=["s1col"])
        B.op("act", lambda e: e.activation(out=s1col[:], in_=s1col[:], func=AF.Ln, bias=1.0), reads=["s1col"], writes=["s1col"])
        B.op("dve", lambda e: e.tensor_scalar(out=s2col[:], in0=s1col[:], scalar1=-16.0, scalar2=None, op0=ALU.mult),
             reads=["s1col"], writes=["s2col"])
        B.op("dve", lambda e: e.tensor_scalar(out=s1col[:], in0=s1col[:], scalar1=-8.0, scalar2=None, op0=ALU.mult),
             reads=["s1col", "s2col"], writes=["s1col"])
        for dst, src, nm in ((hbacol, bacol, "hbacol"), (hbxcol, bxcol, "hbxcol"), (hs1col, s1col, "hs1col"), (hs2col, s2col, "hs2col")):
            B.op("dve", lambda e, dst=dst, src=src: e.tensor_scalar(out=dst[:], in0=src[:], scalar1=0.5, scalar2=None, op0=ALU.mult),
                 reads=["bacol", "bxcol", "s1col", "s2col"], writes=[nm])
        B.op("dve", lambda e: e.memset(qcol[:], 0.25), writes=["qcol"])
        B.op("dve", lambda e: e.memset(mhalf[:], -0.5), writes=["mhalf"])
        B.op("dve", lambda e: e.memset(mhalf2[:], -0.5), writes=["mhalf2"])
        for h in range(NH):
            B.dma("sp", kaug_d[h, 70:71, :], kmrow_d, writes=[("kaugc", h)], semgroup="kscr")
        for t in range(8):
            B.dma("sp", kaug_d[:, 64:67, t * 512:(t + 1) * 512], ones_d, writes=[("kaug1", t)], semgroup="kscr")
        for t in range(4):
            B.dma("sp", qaug_d[:, 67:70, t * 512:(t + 1) * 512], ones_d, writes=[("qaug1", t)], semgroup="qscr")
            B.dma("sp", qaug_d[:, 70:71, t * 512:(t + 1) * 512], ones_d[:, 0:1, :], writes=[("qaug2", t)], semgroup="qscr")

        ar.seek(OFF_ATT)
        win = ar.alloc([128, 8, 2568], BF16)
        stage = [ar.alloc([128, 642], F32) for _ in range(2)]
        hT = [ar.alloc([128, 8, 512], BF16) for _ in range(2)]
        xt = [ar.alloc([128, D], F32) for _ in range(2)]
        xn = [ar.alloc([128, D], BF16) for _ in range(2)]
        rin = [ar.alloc([128, 4, 516], BF16) for _ in range(2)]
        cvb = ar.alloc([128, 4, 512], BF16)
        kst = ar.alloc([128, NH, 512], BF16)
        qst = ar.alloc([128, NH, 512], BF16)
        tr = ar.alloc([128, 512], F32)
        ti = ar.alloc([128, 512], F32)
        ta = ar.alloc([128, 512], F32)
        tm = ar.alloc([128, 512], F32)
        tu = ar.alloc([128, 512], F32)
        th = ar.alloc([128, 512], F32)
        gel = ar.alloc([128, 512], F32)
        fl = ar.alloc([128, 512], F32)
        fc = ar.alloc([128, 512], F32)
        fr = ar.alloc([128, 512], F32)
        fp = ar.alloc([128, 6, 512], BF16)
        cdiag = ar.alloc([128, 16, 128], BF16)
        bda = ar.alloc([128, 4, 128], BF16)
        bdx = ar.alloc([128, 4, 128], BF16)
        P1_END = ar.off
        B.dma("pool", cdiag[:], cdiag_d, writes=["cdiag"])
        B.dma("pool", bda[:], bda_d, writes=["bda"])
        B.dma("pool", bdx[:], bdx_d, writes=["bdx"])

        for dc in range(8):
            for q in range(4):
                s = (dc * 4 + q) % 2
                B.dma("sp", stage[s][:], w_in_d[dc * 128:(dc + 1) * 128, q * 642:(q + 1) * 642], writes=[("stage", s)])
                B.op("dve", lambda e, dc=dc, q=q, s=s: e.tensor_scalar(out=win[:, dc, q * 642:(q + 1) * 642], in0=stage[s][:],
                                                                     scalar1=gcol[:, dc:dc + 1], scalar2=None, op0=ALU.mult),
                     reads=[("stage", s), "gcol"], writes=[("win", dc)])
        WK, WV, WF, WG, WR = 512, 1024, 1536, 1544, 2056
        winr = [("win", dc) for dc in range(8)]

        B.op("dve", lambda e: e.memset(rin[1][:], 0.0), writes=[("rin", 1)])
        B.op("dve", lambda e: e.memset(rin[0][:, :, 0:4], 0.0), writes=[("rinh", 0)])
        B.op("dve", lambda e: e.memset(vaug[:], 1.0), writes=["vaug_init"])

        def tile_ctx(T):
            return T >= 4, hT[T % 2], ("hT", T % 2)

        def piece_N(T):
            own, hb, hkey = tile_ctx(T)
            for pr in range(2):
                blks = (2 * pr, 2 * pr + 1)
                for blk in blks:
                    g = T * 4 + blk
                    xb = xt[g % 2]
                    B.dma("sp", xb[:], xs[g * 128:(g + 1) * 128, :], writes=[("xt", g % 2)])
                    B.op("dve", lambda e, xb=xb, blk=blk: e.scalar_tensor_tensor(out=xn[0][:], in0=xb[:], scalar=1.0, in1=xb[:],
                                                                                 op0=ALU.mult, op1=ALU.mult, accum_out=ss4[:, blk:blk + 1]),
                         reads=[("xt", g % 2)], writes=[("xnjunk",), ("ss4", blk)])
                rk = [("rstd4", k) for k in blks]
                c0 = 2 * pr
                B.op("dve", lambda e, c0=c0: e.tensor_scalar(out=rstd4[:, c0:c0 + 2], in0=ss4[:, c0:c0 + 2], scalar1=1.0 / D, scalar2=1e-6,
                                                             op0=ALU.mult, op1=ALU.add),
                     reads=[("ss4", k) for k in blks], writes=rk)
                B.op("pool", lambda e, c0=c0: e.tensor_tensor(out=rstd4[:, c0:c0 + 2], in0=rstd4[:, c0:c0 + 2], in1=mhalf2[:], op=ALU.pow),
                     reads=rk + ["mhalf2"], writes=rk)
                for blk in blks:
                    g = T * 4 + blk
                    xb = xt[g % 2]
                    B.op("dve", lambda e, xb=xb, blk=blk: e.tensor_scalar(out=xn[1][:], in0=xb[:], scalar1=rstd4[:, blk:blk + 1], scalar2=None, op0=ALU.mult),
                         reads=[("xt", g % 2), ("rstd4", blk)], writes=[("xn", 1)])
                    ip = nextpt()
                    ptv = pt[ip][:].rearrange("p (a b) -> p a b", a=8)
                    for c in range(8):
                        B.op("pe", lambda e, c=c, ptv=ptv: e.transpose(ptv[:, c, :], xn[1][:, c * 128:(c + 1) * 128], identb[:]),
                             reads=[("xn", 1), "identb"], writes=[("pt", ip)])
                    B.op("dve", lambda e, ptv=ptv, hb=hb, blk=blk: e.tensor_copy(hb[:, :, blk * 128:(blk + 1) * 128], ptv),
                         reads=[("pt", ip)], writes=[hkey])

        def piece_K(T, pairs, last):
            own, hb, hkey = tile_ctx(T)
            for pr in pairs:
                ib = nextpb()
                for dc in range(8):
                    B.op("pe", lambda e, pr=pr, dc=dc, ib=ib: e.matmul(pb[ib][:, :], lhsT=win[:, dc, WK + pr * 128:WK + (pr + 1) * 128],
                                                                       rhs=hb[:, dc, :], start=(dc == 0), stop=(dc == 7)),
                         reads=[hkey] + winr, writes=[("pb", ib)])
                B.op("dve", lambda e, pr=pr, ib=ib: e.tensor_copy(kst[:, pr, :], pb[ib][:, :]), reads=[("pb", ib)], writes=[("kst", pr)])
            if last:
                kd = kaug_d[:, 0:64, T * 512:(T + 1) * 512].rearrange("(pr two) p t -> two p pr t", two=2)
                for two in range(2):
                    B.dma("sp", kd[two], kst[two * 64:(two + 1) * 64, 0:4, :],
                          reads=[("kst", pr) for pr in range(4)], writes=[("kaugd", T, two)], semkey="kst_out%d" % two)

        def piece_Q(T):
            own, hb, hkey = tile_ctx(T)
            if not own:
                return
            for pr in range(4):
                ib = nextpb()
                for dc in range(8):
                    B.op("pe", lambda e, pr=pr, dc=dc, ib=ib: e.matmul(pb[ib][:, :], lhsT=win[:, dc, pr * 128:(pr + 1) * 128],
                                                                       rhs=hb[:, dc, :], start=(dc == 0), stop=(dc == 7)),
                         reads=[hkey] + winr, writes=[("pb", ib)])
                B.op("act", lambda e, pr=pr, ib=ib: e.activation(out=qst[:, pr, :], in_=pb[ib][:, :], func=AF.Copy, scale=0.125),
                     reads=[("pb", ib)], writes=[("qst", pr)])
            qd = qaug_d[:, 0:64, (T - 4) * 512:(T - 3) * 512].rearrange("(pr two) p t -> two p pr t", two=2)
            for two in range(2):
                B.dma("sp", qd[two], qst[two * 64:(two + 1) * 64, 0:4, :],
                      reads=[("qst", pr) for pr in range(4)], writes=[("qaugd", T, two)], semkey="qst_out%d" % two)

        def piece_V(T):
            own, hb, hkey = tile_ctx(T)
            for blk in range(4):
                g = T * 4 + blk
                ib = nextpb()
                for dc in range(8):
                    B.op("pe", lambda e, blk=blk, dc=dc, ib=ib: e.matmul(pb[ib][:, :], lhsT=hb[:, dc, blk * 128:(blk + 1) * 128],
                                                                         rhs=win[:, dc, WV:WV + 512], start=(dc == 0), stop=(dc == 7)),
                         reads=[hkey] + winr, writes=[("pb", ib)])
                B.op("dve", lambda e, g=g, ib=ib: e.tensor_copy(vaug[:, g, :, 0:64], pb[ib][:, :].rearrange("p (h d) -> p h d", h=NH)),
                     reads=[("pb", ib), "vaug_init"], writes=[("vaug", g)])

        def piece_F(T):
            own, hb, hkey = tile_ctx(T)
            ib = nextpb()
            for dc in range(8):
                B.op("pe", lambda e, dc=dc, ib=ib: e.matmul(pb[ib][0:8, :], lhsT=win[:, dc, WF:WF + 8], rhs=hb[:, dc, :],
                                                            start=(dc == 0), stop=(dc == 7)),
                     reads=[hkey] + winr, writes=[("pb", ib)])
            B.op("act", lambda e, ib=ib: e.activation(out=fl[0:8, :], in_=pb[ib][0:8, :], func=AF.Exp, scale=-1.0, bias=nbfcol[:]),
                 reads=[("pb", ib), "nbfcol"], writes=["fl"])
            B.op("act", lambda e: e.activation(out=fl[0:8, :], in_=fl[0:8, :], func=AF.Ln, bias=1.0), reads=["fl"], writes=["fl"])
            B.op("dve", lambda e: e.memset(fr[0:8, :], 1.0), writes=["fr"])
            B.op("dve", lambda e: e.tensor_tensor_scan(out=fc[0:8, :], data0=fr[0:8, :], data1=fl[0:8, :], initial=cstate[:],
                                                       op0=ALU.mult, op1=ALU.add), reads=["fr", "fl", "cstate"], writes=["fc"])
            B.op("dve", lambda e: e.tensor_copy(cstate[:], fc[0:8, 511:512]), reads=["fc"], writes=["cstate"])
            B.op("dve", lambda e: e.tensor_copy(fp[0:8, 0, :], fc[0:8, :]), reads=["fc"], writes=[("fp", 0)])
            B.op("dve", lambda e: e.tensor_tensor(out=fr[0:8, :], in0=fc[0:8, :], in1=fp[0:8, 0, :], op=ALU.subtract),
                 reads=["fc", ("fp", 0)], writes=["fr"])
            B.op("dve", lambda e: e.tensor_copy(fp[0:8, 1, :], fr[0:8, :]), reads=["fr"], writes=[("fp", 1)])
            B.op("dve", lambda e: e.tensor_tensor(out=fl[0:8, :], in0=fr[0:8, :], in1=fp[0:8, 1, :], op=ALU.subtract),
                 reads=["fr", ("fp", 1)], writes=["fl"])
            B.op("dve", lambda e: e.tensor_copy(fp[0:8, 2, :], fl[0:8, :]), reads=["fl"], writes=[("fp", 2)])
            B.dma("sp", kaug_d[:, 67:70, T * 512:(T + 1) * 512], fp[0:8, 0:3, :], reads=[("fp", 0), ("fp", 1), ("fp", 2)],
                  writes=[("kaugp", T)], semkey="fpk_out")
            if own:
                B.op("dve", lambda e: e.tensor_scalar(out=fp[0:8, 3:6, :], in0=fp[0:8, 0:3, :], scalar1=-1.0, scalar2=None, op0=ALU.mult),
                     reads=[("fp", 0), ("fp", 1), ("fp", 2)], writes=[("fp", 3)])
                B.dma("sp", qaug_d[:, 64:67, (T - 4) * 512:(T - 3) * 512], fp[0:8, 3:6, :], reads=[("fp", 3)], writes=[("qaugp", T)], semkey="fpq_out")

        def piece_A(T):
            own, hb, hkey = tile_ctx(T)
            rb_ = rin[T % 2]
            rprev = rin[(T + 1) % 2]
            B.op("dve", lambda e: e.tensor_copy(rb_[:, :, 1:4], rprev[:, :, 513:516]),
                 reads=[("rin", (T + 1) % 2)], writes=[("rinh", T % 2)])
            for cc in range(4):
                ib = nextpb()
                for dc in range(8):
                    B.op("pe", lambda e, cc=cc, dc=dc, ib=ib: e.matmul(pb[ib][:, :], lhsT=win[:, dc, WR + cc * 128:WR + (cc + 1) * 128],
                                                                       rhs=hb[:, dc, :], start=(dc == 0), stop=(dc == 7)),
                         reads=[hkey] + winr, writes=[("pb", ib)])
                B.op("act", lambda e, cc=cc, ib=ib: e.activation(out=rb_[:, cc, 4:516], in_=pb[ib][:, :], func=AF.Copy),
                     reads=[("pb", ib)], writes=[("rin", T % 2)])
            for cc in range(4):
                ib = nextpb()
                for k in range(4):
                    B.op("pe", lambda e, cc=cc, k=k, ib=ib: e.matmul(pb[ib][:, :], lhsT=cdiag[:, k * 4 + cc, :],
                                                                     rhs=rb_[:, cc, 1 + k:1 + k + 512], start=(k == 0), stop=(k == 3)),
                         reads=[("rin", T % 2), ("rinh", T % 2), "cdiag"], writes=[("pb", ib)])
                B.op("act", lambda e, cc=cc, ib=ib: e.activation(out=cvb[:, cc, :], in_=pb[ib][:, :], func=AF.Identity, bias=convb[:, cc:cc + 1]),
                     reads=[("pb", ib), "convb"], writes=[("cvb", cc)])

        def piece_C(T, cc):
            own, hb, hkey = tile_ctx(T)
            ck = ("cvb", cc)
            iba = nextpb()
            B.op("pe", lambda e: e.matmul(pb[iba][:, :], lhsT=bda[:, cc, :], rhs=cvb[:, cc, :], start=True, stop=True),
                 reads=[ck, "bda"], writes=[("pb", iba)])
            ibx = nextpb()
            B.op("pe", lambda e: e.matmul(pb[ibx][:, :], lhsT=bdx[:, cc, :], rhs=cvb[:, cc, :], start=True, stop=True),
                 reads=[ck, "bdx"], writes=[("pb", ibx)])
            B.op("act", lambda e: e.activation(out=tr[:], in_=pb[iba][:, :], func=AF.Tanh, bias=hbacol[:, cc:cc + 1], scale=0.5),
                 reads=[("pb", iba), "hbacol"], writes=["tr"])
            B.op("act", lambda e: e.activation(out=ti[:], in_=pb[ibx][:, :], func=AF.Tanh, bias=hbxcol[:, cc:cc + 1], scale=0.5),
                 reads=[("pb", ibx), "hbxcol"], writes=["ti"])
            B.op("act", lambda e: e.activation(out=ta[:], in_=tr[:], func=AF.Exp, scale=hs1col[:, cc:cc + 1], bias=hs1col[:, cc:cc + 1]),
                 reads=["tr", "hs1col"], writes=["ta"])
            B.op("act", lambda e: e.activation(out=tm[:], in_=tr[:], func=AF.Exp, scale=hs2col[:, cc:cc + 1], bias=hs2col[:, cc:cc + 1]),
                 reads=["tr", "hs2col"], writes=["tm"])
            B.op("act", lambda e: e.activation(out=tm[:], in_=tm[:], func=AF.Sqrt, scale=-0.25, bias=qcol[:, 0:1]), reads=["tm", "qcol"], writes=["tm"])
            B.op("dve", lambda e: e.scalar_tensor_tensor(out=tu[:], in0=ti[:], scalar=1.0, in1=cvb[:, cc, :], op0=ALU.add, op1=ALU.mult),
                 reads=["ti", ck], writes=["tu"])
            B.op("dve", lambda e: e.tensor_tensor(out=tu[:], in0=tu[:], in1=tm[:], op=ALU.mult), reads=["tu", "tm"], writes=["tu"])
            if T == 4:
                B.op("dve", lambda e: e.tensor_scalar(out=hstate[:, cc:cc + 1], in0=hstate[:, cc:cc + 1], scalar1=flag[:, 0:1],
                                                      scalar2=None, op0=ALU.mult), reads=[("hstate", cc), "flag"], writes=[("hstate", cc)])
            B.op("dve", lambda e: e.tensor_tensor_scan(out=th[:], data0=ta[:], data1=tu[:], initial=hstate[:, cc:cc + 1],
                                                       op0=ALU.mult, op1=ALU.add),
                 reads=["ta", "tu", ("hstate", cc), "hstate"], writes=["th"])
            B.op("dve", lambda e: e.tensor_copy(hstate[:, cc:cc + 1], th[:, 511:512]), reads=["th"], writes=[("hstate", cc)])
            if own:
                ibg = nextpb()
                for dc in range(8):
                    B.op("pe", lambda e, dc=dc: e.matmul(pb[ibg][:, :], lhsT=win[:, dc, WG + cc * 128:WG + (cc + 1) * 128],
                                                         rhs=hb[:, dc, :], start=(dc == 0), stop=(dc == 7)),
                         reads=[hkey] + winr, writes=[("pb", ibg)])
                B.op("act", lambda e: e.activation(out=gel[:], in_=pb[ibg][:, :], func=AF.Gelu_apprx_tanh),
                     reads=[("pb", ibg)], writes=["gel"])
                B.op("dve", lambda e: e.tensor_tensor(out=lruo[:, cc, (T - 4) * 512:(T - 3) * 512], in0=th[:], in1=gel[:], op=ALU.mult),
                     reads=["th", "gel"], writes=[("lruo", T - 4)])

        for t in range(-1, 8):
            if t + 1 < 8:
                piece_N(t + 1)
            if t >= 0:
                piece_A(t)
                piece_F(t)
                piece_K(t, range(0, 2), False)
                piece_C(t, 0)
                piece_K(t, range(2, 4), True)
                piece_C(t, 1)
                piece_V(t)
                piece_C(t, 2)
                piece_Q(t)
                piece_C(t, 3)

        B.barrier()
        ar.seek(OFF_P)
        kaug = [ar.alloc([128, NTOK], BF16) for _ in range(2)]
        qaug = [ar.alloc([128, NOWN], BF16) for _ in range(2)]
        PT = [ar.alloc([128, 512], BF16) for _ in range(4)]
        osb = [ar.alloc([128, 512], F32) for _ in range(2)]
        kd_reads = [("kaugc", h) for h in range(NH)] + [("kaug1", t) for t in range(8)] + [("kaugd", t, two) for t in range(8) for two in range(2)] + [("kaugp", t) for t in range(8)]
        qd_reads = [("qaug1", t) for t in range(4)] + [("qaug2", t) for t in range(4)] + [("qaugd", t, two) for t in range(4, 8) for two in range(2)] + [("qaugp", t) for t in range(4, 8)]
        sidx = [0]
        oidx = 0
        dq = Deferred()
        step = 0
        S_LA = 2
        for h in range(NH):
            kb_ = kaug[h % 2]
            qb_ = qaug[h % 2]
            B.dma("sp", kb_[0:KA, :], kaug_d[h], reads=kd_reads, writes=[("kaug", h % 2)])
            B.dma("sp", qb_[0:KA, :], qaug_d[h], reads=qd_reads, writes=[("qaug", h % 2)])
            for G in range(4):
                nkb = 16 + 4 * G + 4
                io = 4 + (oidx % 2)
                oidx += 1
                okey = ("pb", io)
                for kb in range(nkb):
                    m = kb - (16 + 4 * G)
                    q0 = 0 if m < 0 else m * 128
                    isb = sidx[0] % 4
                    sidx[0] += 1
                    skey = ("pb", isb)
                    B.op("pe", lambda e, kb=kb, G=G, q0=q0, isb=isb, kb_=kb_, qb_=qb_, m=m: e.matmul(
                        pb[isb][:, q0:512], lhsT=kb_[0:KA, kb * 128:(kb + 1) * 128], rhs=qb_[0:KA, G * 512 + q0:(G + 1) * 512],
                        start=True, stop=(m < 0)), reads=[("kaug", h % 2), ("qaug", h % 2)], writes=[skey])
                    if m >= 0:
                        B.op("pe", lambda e, q0=q0, isb=isb: e.matmul(pb[isb][:, q0:q0 + 128], lhsT=identb[:], rhs=maskb[:], start=False, stop=True),
                             reads=["identb", "maskb"], writes=[skey])
                    pkey = ("PT", isb)
                    B.op("act", lambda e, q0=q0, isb=isb: e.activation(out=PT[isb][:, q0:512], in_=pb[isb][:, q0:512], func=AF.Exp),
                         reads=[skey], writes=[pkey])

                    def pv(kb=kb, q0=q0, isb=isb, io=io, h=h, nkb=nkb, okey=okey, pkey=pkey):
                        B.op("pe", lambda e: e.matmul(pb[io][0:65, q0:512], lhsT=vaug[:, kb, h, :], rhs=PT[isb][:, q0:512],
                                                      start=(kb == 0), stop=(kb == nkb - 1)),
                             reads=[pkey, ("vaug", kb)], writes=[okey])
                    dq.at(step + S_LA, pv)
                    dq.run(step)
                    step += 1
                ob = osb[oidx % 2]
                obk = ("osb", oidx % 2)

                def norm1(ob=ob, obk=obk, io=io, okey=okey):
                    B.op("dve", lambda e: e.tensor_copy(ob[0:65, :], pb[io][0:65, :]), reads=[okey], writes=[obk])
                    B.op("dve", lambda e: e.reciprocal(ob[64:65, :], ob[64:65, :]), reads=[obk], writes=[obk])

                def norm2(ob=ob, obk=obk, h=h, G=G):
                    ibc = sidx[0] % 4
                    sidx[0] += 1
                    B.op("pe", lambda e: e.matmul(pb[ibc][0:64, :], lhsT=onesf[64:65, 0:64], rhs=ob[64:65, :], start=True, stop=True),
                         reads=[obk, "onesf"], writes=[("pb", ibc)])
                    B.op("dve", lambda e: e.tensor_tensor(out=attT[0:64, h, G * 512:(G + 1) * 512], in0=ob[0:64, :],
                                                          in1=pb[ibc][0:64, :], op=ALU.mult),
                         reads=[obk, ("pb", ibc)], writes=[("attT", h, G)])
                dq.at(step + S_LA - 1, norm1)
                dq.at(step + S_LA + 9, norm2)
        dq.flush()

        B.barrier()
        ar.seek(OFF_P)
        h2tok = ar.alloc([128, 16, D], BF16)
        wgu = [ar.alloc([128, 8, 512], BF16) for _ in range(2)]
        wdn = [ar.alloc([128, 2, D], BF16) for _ in range(2)]
        OFF_P3 = ar.off
        woa = ar.alloc([128, NH, D], BF16)
        wol = ar.alloc([128, 4, D], BF16)
        x3 = [ar.alloc([128, D], F32) for _ in range(2)]
        x1t = [ar.alloc([128, D], F32) for _ in range(2)]
        xq = ar.alloc([128, D], F32)
        h2Tb = [ar.alloc([128, 8, 128], BF16) for _ in range(2)]
        ohb = sb("ohb", [128, 16, NE], BF16)
        cw = sb("cw", [128, 16, 2], F32)
        sidx = sb("sidx", [128, 16, 2], I32)
        ebase1 = sb("ebase1", [128, NE], F32)
        padbase = sb("padbase", [128, NE], F32)
        triuf = sb("triuf", [128, 128], F32)
        triub = sb("triub", [128, 128], BF16)
        onesb = sb("onesb", [128, 128], BF16)
        cntsb = sb("cntsb", [128, NE], F32)
        padi = sb("padi", [128, NE], I32)
        flf = sb("flf", [1, NE, 16], F32)
        flags = sb("flags", [1, NE * 18 + 1], I32)
        flf4 = sb("flf4", [1, NE], F32)
        flf3 = sb("flf3", [1, 1], F32)
        flf2 = sb("flf2", [1, NE], F32)
        svt = sb("svt", [128, 2 * NE + 16], F32)

        B.dma("sp", ebase1[:], ebase1_d, writes=["ebase1"])

        B.dma("sp", padbase[:], padbase_d, writes=["padbase"])
        B.dma("sp", triuf[:], triu_d, writes=["triuf"])
        B.op("dve", lambda e: e.tensor_copy(triub[:], triuf[:]), reads=["triuf"], writes=["triub"])
        B.op("dve", lambda e: e.memset(onesb[:], 1.0), writes=["onesb"])
        B.dma("pool", woa[0:64, :, :], w_out_d[0:512, :].rearrange("(h p) n -> p h n", p=64), writes=["woa"])
        B.dma("pool", wol[:], w_out_d[512:1024, :].rearrange("(c p) n -> p c n", p=128), writes=["wol"])

        def load_expert(e_):
            s = e_ % 2
            B.dma("pool", wgu[s][:, :, 0:256], wg_d[e_].rearrange("(c p) f -> p c f", p=128), writes=[("wg", s)], nobarrier=True)
            B.dma("pool", wgu[s][:, :, 256:512], wu_d[e_].rearrange("(c p) f -> p c f", p=128), writes=[("wu", s)], nobarrier=True)
            B.dma("pool", wdn[s][:], wd_d[e_].rearrange("(c p) n -> p c n", p=128), writes=[("wd", s)], nobarrier=True)

        def p3_block(j, part):
            if part == 1:
                return p3_back(j)
            xb = x3[j % 2]
            B.dma("sp", xb[:], xs[NOWN + j * 128:NOWN + (j + 1) * 128, :], writes=[("x3", j % 2)])
            x1b = x1t[j % 2]
            for half in range(2):
                ib = nextpb()
                for h in range(NH):
                    B.op("pe", lambda e, h=h, half=half, ib=ib: e.matmul(pb[ib][:, :], lhsT=attT[0:64, h, j * 128:(j + 1) * 128],
                                                                      rhs=woa[0:64, h, half * 512:(half + 1) * 512], start=(h == 0), stop=False),
                         reads=[("attT", h, j // 4), "woa"], writes=[("pb", ib)])
                for cc in range(4):
                    B.op("pe", lambda e, cc=cc, half=half, ib=ib: e.matmul(pb[ib][:, :], lhsT=lruo[:, cc, j * 128:(j + 1) * 128],
                                                                        rhs=wol[:, cc, half * 512:(half + 1) * 512], start=False, stop=(cc == 3)),
                         reads=[("lruo", j // 4), "wol"], writes=[("pb", ib)])
                B.op("dve", lambda e, half=half, ib=ib: e.tensor_tensor(out=x1b[:, half * 512:(half + 1) * 512], in0=pb[ib][:, :],
                                                                     in1=xb[:, half * 512:(half + 1) * 512], op=ALU.add),
                     reads=[("pb", ib), ("x3", j % 2)], writes=[("x1t", j % 2, half)])
            x1r = [("x1t", j % 2, 0), ("x1t", j % 2, 1)]
            B.dma("sp", x1_d[j * 128:(j + 1) * 128, :], x1b[:], reads=x1r, writes=[("x1d", j)], semkey=("x1t_out", j % 2))
            B.op("act", lambda e: e.activation(out=xq[:], in_=x1b[:], func=AF.Square, accum_out=ss4[:, 0:1]),
                 reads=x1r, writes=["xq", ("ss4", 0)])
            B.op("dve", lambda e: e.tensor_scalar(out=rstd4[:, 0:1], in0=ss4[:, 0:1], scalar1=1.0 / D, scalar2=1e-6, op0=ALU.mult, op1=ALU.add),
                 reads=[("ss4", 0)], writes=[("rstd4", 0)])
            B.op("pool", lambda e: e.tensor_tensor(out=rstd4[:, 0:1], in0=rstd4[:, 0:1], in1=mhalf[:], op=ALU.pow),
                 reads=[("rstd4", 0), "mhalf"], writes=[("rstd4", 0)])
            B.op("act", lambda e: e.activation(out=xq[:], in_=x1b[:], func=AF.Copy, scale=rstd4[:, 0:1]),
                 reads=x1r + [("rstd4", 0)], writes=["xq"])
            B.op("dve", lambda e: e.tensor_tensor(out=h2tok[:, j, :], in0=xq[:], in1=g2bc[:], op=ALU.mult), reads=["xq", "g2bc"], writes=[("h2tok", j)])
        def p3_back(j):
            ip = nextpt()
            ptv = pt[ip][:].rearrange("p (a b) -> p a b", a=8)
            for c in range(8):
                B.op("pe", lambda e, c=c: e.transpose(ptv[:, c, :], h2tok[:, j, c * 128:(c + 1) * 128], identb[:]),
                     reads=[("h2tok", j), "identb"], writes=[("pt", ip)])
            hT_ = h2Tb[j % 2]
            B.op("act", lambda e: e.activation(out=hT_[:], in_=ptv, func=AF.Copy), reads=[("pt", ip)], writes=[("h2Tb", j % 2)])
            ib = nextpb()
            for dc in range(8):
                B.op("pe", lambda e, dc=dc, ib=ib: e.matmul(pb[ib][:, 0:36], lhsT=hT_[:, dc, :], rhs=wr[:, dc, :],
                                                            start=(dc == 0), stop=(dc == 7)), reads=[("h2Tb", j % 2), "wr"], writes=[("pb", ib)])
            B.op("dve", lambda e, ib=ib: e.tensor_tensor(out=rt[:, 0:36], in0=pb[ib][:, 0:36], in1=rbbc[:], op=ALU.add),
                 reads=[("pb", ib), "rbbc"], writes=["rt"])
            R = dict(reads=["rt"], writes=["rt"])
            B.op("dve", lambda e: e.tensor_reduce(out=rt[:, 36:37], in_=rt[:, 0:4], axis=AX.X, op=ALU.max), **R)
            B.op("dve", lambda e: e.tensor_scalar(out=rt[:, 37:38], in0=rt[:, 36:37], scalar1=-1.0, scalar2=None, op0=ALU.mult), **R)
            B.op("act", lambda e: e.activation(out=rt[:, 38:42], in_=rt[:, 0:4], func=AF.Exp, bias=rt[:, 37:38], accum_out=rt[:, 42:43]), **R)
            B.op("dve", lambda e: e.reciprocal(rt[:, 42:43], rt[:, 42:43]), **R)
            B.op("dve", lambda e: e.tensor_scalar(out=rt[:, 38:42], in0=rt[:, 0:4], scalar1=rt[:, 36:37], scalar2=None, op0=ALU.is_equal), **R)
            B.op("dve", lambda e: e.tensor_scalar(out=rt[:, 44:52], in0=rt[:, 4:12], scalar1=rt[:, 38:39], scalar2=None, op0=ALU.mult), **R)
            for g in range(1, 4):
                B.op("dve", lambda e, g=g: e.scalar_tensor_tensor(out=rt[:, 44:52], in0=rt[:, 4 + 8 * g:12 + 8 * g], scalar=rt[:, 38 + g:39 + g],
                                                                  in1=rt[:, 44:52], op0=ALU.mult, op1=ALU.add), **R)
            B.op("dve", lambda e: e.max(out=rt[:, 52:60], in_=rt[:, 44:52]), **R)
            B.op("dve", lambda e: e.tensor_scalar(out=rt[:, 60:61], in0=rt[:, 52:53], scalar1=-1.0, scalar2=None, op0=ALU.mult), **R)
            B.op("act", lambda e: e.activation(out=rt[:, 4:12], in_=rt[:, 44:52], func=AF.Exp, bias=rt[:, 60:61]), **R)
            B.op("dve", lambda e: e.tensor_scalar(out=rt[:, 12:20], in0=rt[:, 44:52], scalar1=rt[:, 53:54], scalar2=None, op0=ALU.is_ge), **R)
            B.op("dve", lambda e: e.tensor_tensor(out=rt[:, 4:12], in0=rt[:, 4:12], in1=rt[:, 12:20], op=ALU.mult), **R)
            B.op("dve", lambda e: e.tensor_reduce(out=rt[:, 61:62], in_=rt[:, 4:12], axis=AX.X, op=ALU.add), **R)
            B.op("dve", lambda e: e.reciprocal(rt[:, 61:62], rt[:, 61:62]), **R)
            B.op("dve", lambda e: e.tensor_tensor(out=rt[:, 61:62], in0=rt[:, 61:62], in1=rt[:, 42:43], op=ALU.mult), **R)
            for g in range(4):
                B.op("dve", lambda e, g=g: e.tensor_scalar(out=comb[:, j, 8 * g:8 * g + 8], in0=rt[:, 4:12], scalar1=rt[:, 38 + g:39 + g],
                                                           scalar2=rt[:, 61:62], op0=ALU.mult, op1=ALU.mult),
                     reads=["rt"], writes=[("comb", j)])
                B.op("dve", lambda e, g=g: e.tensor_scalar(out=ohb[:, j, 8 * g:8 * g + 8], in0=rt[:, 12:20], scalar1=rt[:, 38 + g:39 + g],
                                                           scalar2=None, op0=ALU.mult),
                     reads=["rt"], writes=[("ohb", j)])

        for j in range(17):
            if j < 16:
                p3_block(j, 0)
            if j >= 1:
                p3_block(j - 1, 1)
            if j == 0:
                load_expert(0)
                load_expert(1)

        ohr = [("ohb", j) for j in range(16)]
        combr = [("comb", j) for j in range(16)]
        ar.seek(OFF_P3)
        pre = ar.alloc([128, 16, NE], F32)
        sva = ar.alloc([128, 16, NE], F32)
        msk = ar.alloc([128, 16, NE], F32)
        ebase16 = ar.alloc([128, 16, NE], F32)
        top8a = ar.alloc([128, 16, 8], F32)
        B.dma("sp", ebase16[:], eb16_d.rearrange("p (a b) -> p a b", a=16), writes=["ebase16", "woa", "wol"])
        ibt = nextpb()
        ibk = nextpb()
        totv = pb[ibt][:, :].rearrange("p (a b) -> p a b", a=16)
        bkv = pb[ibk][:, :].rearrange("p (a b) -> p a b", a=16)
        for j in range(16):
            B.op("pe", lambda e, j=j: e.matmul(totv[:, j, :], lhsT=onesb[:], rhs=ohb[:, j, :], start=True, stop=True),
                 reads=[("ohb", j), "onesb"], writes=[("pb", ibt)])
        for j in range(16):
            B.op("pe", lambda e, j=j: e.matmul(bkv[:, j, :], lhsT=triub[:], rhs=ohb[:, j, :], start=True, stop=True),
                 reads=[("ohb", j), "triub"], writes=[("pb", ibk)])
        PRE = dict(reads=["pre"], writes=["pre"])
        B.op("dve", lambda e: e.memset(pre[:, 0, :], 0.0), reads=["pre"], writes=["pre", "woa", "wol"])
        for j in range(1, 16):
            B.op("dve", lambda e, j=j: e.tensor_tensor(out=pre[:, j, :], in0=totv[:, j - 1, :], in1=pre[:, j - 1, :], op=ALU.add),
                 reads=["pre", ("pb", ibt)], writes=["pre"])
        B.op("dve", lambda e: e.tensor_tensor(out=cntsb[:], in0=totv[:, 15, :], in1=pre[:, 15, :], op=ALU.add),
             reads=["pre", ("pb", ibt)], writes=["cntsb"])
        for k in range(16):
            B.op("dve", lambda e, k=k: e.tensor_scalar(out=flf[0:1, :, k], in0=cntsb[0:1, :], scalar1=128.0 * k, scalar2=None, op0=ALU.is_gt),
                 reads=["cntsb"], writes=["flf"])
        B.op("dve", lambda e: e.tensor_copy(flags[:, 0:NE * 16], flf[:].rearrange("p a b -> p (a b)")), reads=["flf"], writes=["flags0"])
        B.op("dve", lambda e: e.tensor_scalar(out=flf2[0:1, :], in0=cntsb[0:1, :], scalar1=256.0, scalar2=None, op0=ALU.is_gt),
             reads=["cntsb"], writes=["flf2"])
        B.op("dve", lambda e: e.tensor_copy(flags[:, NE * 16:NE * 17], flf2[:]), reads=["flf2", "flags0"], writes=["flags1"])
        B.op("dve", lambda e: e.tensor_reduce(out=flf3[:], in_=flf2[:], axis=AX.X, op=ALU.max), reads=["flf2"], writes=["flf3"])
        B.op("dve", lambda e: e.tensor_copy(flags[:, NE * 17:NE * 17 + 1], flf3[:]), reads=["flf3", "flags1"], writes=["flags2"])
        B.op("dve", lambda e: e.tensor_tensor(out=flf4[:], in0=flf[0:1, :, 0], in1=flf[0:1, :, 1], op=ALU.subtract), reads=["flf"], writes=["flf4"])
        B.op("dve", lambda e: e.tensor_copy(flags[:, NE * 17 + 1:NE * 18 + 1], flf4[:]), reads=["flf4", "flags2"], writes=["flags"])
        B.flags_ap = flags
        B.flags_key = "flags"
        B.op("dve", lambda e: e.tensor_tensor(out=svt[:, 0:NE], in0=cntsb[:], in1=padbase[:], op=ALU.add), reads=["cntsb", "padbase"], writes=["svt"])
        B.op("dve", lambda e: e.tensor_copy(padi[:], svt[:, 0:NE]), reads=["svt"], writes=["padi"])
        B.op("dve", lambda e: e.tensor_tensor(out=sva[:], in0=bkv, in1=pre[:], op=ALU.add), reads=["pre", ("pb", ibk)], writes=["sva"])
        B.op("dve", lambda e: e.tensor_tensor(out=sva[:], in0=sva[:], in1=ebase16[:], op=ALU.add), reads=["sva", "ebase16"], writes=["sva"])
        B.op("dve", lambda e: e.tensor_tensor(out=sva[:], in0=sva[:], in1=ohb[:], op=ALU.mult), reads=["sva"] + ohr, writes=["sva"])
        for j in range(16):
            B.op("dve", lambda e, j=j: e.max(out=top8a[:, j, :], in_=sva[:, j, :]), reads=["sva"], writes=["top8a"])
        for q in range(2):
            B.op("dve", lambda e, q=q: e.tensor_tensor(out=msk[:], in0=sva[:], in1=top8a[:, :, q:q + 1].to_broadcast([128, 16, NE]), op=ALU.is_equal),
                 reads=["sva", "top8a"], writes=["msk"])
            B.op("dve", lambda e: e.tensor_tensor(out=msk[:], in0=msk[:], in1=comb[:], op=ALU.mult), reads=["msk"] + combr, writes=["msk"])
            B.op("dve", lambda e, q=q: e.tensor_reduce(out=cw[:, :, q], in_=msk[:], axis=AX.X, op=ALU.add), reads=["msk"], writes=[("cwq", q)])
        B.op("dve", lambda e: e.tensor_scalar(out=svt[:, 0:32].rearrange("p (a b) -> p a b", a=16), in0=top8a[:, :, 0:2], scalar1=-1.0, scalar2=None, op0=ALU.add),
             reads=["top8a", "svt"], writes=["svt"])
        B.op("dve", lambda e: e.tensor_copy(sidx[:], svt[:, 0:32].rearrange("p (a b) -> p a b", a=16)), reads=["svt"], writes=[("sidx", j) for j in range(16)])

        B.barrier()
        ar.seek(OFF_P3)
        xin = [ar.alloc([128, D], BF16) for _ in range(4)]
        xT = [ar.alloc([128, 8, 128], BF16) for _ in range(2)]
        sg = [ar.alloc([128, 256], F32) for _ in range(2)]
        hid = [ar.alloc([128, 256], BF16) for _ in range(2)]
        hidT = [ar.alloc([128, 2, 128], BF16) for _ in range(2)]
        ysb = [ar.alloc([128, D], BF16) for _ in range(2)]
        zt = ar.alloc([128, D], BF16)
        assert ar.off <= OFF_P3 + 24 * 1024, ar.off - OFF_P3
        B.op("dve", lambda e: e.memset(zt[:], 0.0), writes=["zt"])

        regcache = {}

        def bcreg(e):
            if "bc" not in regcache:
                regcache["bc"] = e.to_reg(NE * CAP - 1)
            return regcache["bc"]

        def ind_scatter(src_ap, idx_ap, reads, wkey, grp):
            def fn(e):
                return e.indirect_dma_start(out=xs_scr[:, :], out_offset=bass.IndirectOffsetOnAxis(ap=idx_ap, axis=0),
                                            in_=src_ap, in_offset=None, bounds_check=bcreg(e), oob_is_err=False)
            B.op("pool", fn, reads=reads, writes=[wkey], dma=True, semgroup=grp)

        tok_keys = []
        for j in range(16):
            for q in range(2):
                ind_scatter(h2tok[:, j, :], sidx[:, j, q:q + 1], [("h2tok", j), ("sidx", j)], ("sct", j, q), "scat")
                tok_keys.append(("sct", j, q))
        for e_ in range(NE):
            ind_scatter(zt[:, :], padi[:, e_:e_ + 1], ["zt", "padi"], ("scz", e_), "scz%d" % (e_ // 4))
        scat_keys = tok_keys + [("scz", e_) for e_ in range(NE)]

        ys_keys = []
        tcount = [0]

        def moe_tiles(e_, ks, flag):
            s = e_ % 2
            wkeys = [("wg", s), ("wu", s)]
            B.begin_cond(flag)
            T_ = []
            for k in ks:
                tno = tcount[0]
                tcount[0] += 1
                T_.append(dict(k=k, b2=tno % 2, igu=tno % 2, iy=2 + 2 * (tno % 2), row0=e_ * CAP + k * 128,
                               bx=(e_ % 2) * 2 + (k % 2)))
            for t in T_:
                B.dma("sp", xin[t["bx"]][:], xs_scr[t["row0"]:t["row0"] + 128, :],
                      reads=tok_keys + [("scz", e2) for e2 in range((e_ // 4) * 4, (e_ // 4) * 4 + 4)], writes=[("xin", t["bx"])])
            for t in T_:
                ip = nextpt()
                t["ip"] = ip
                t["ptv"] = pt[ip][:].rearrange("p (a b) -> p a b", a=8)
                for c in range(8):
                    B.op("pe", lambda e, c=c, t=t: e.transpose(t["ptv"][:, c, :], xin[t["bx"]][:, c * 128:(c + 1) * 128], identb[:]),
                         reads=[("xin", t["bx"]), "identb"], writes=[("pt", ip)])
                B.op("dve", lambda e, t=t: e.tensor_copy(xT[t["b2"]][:], t["ptv"]), reads=[("pt", ip)], writes=[("xT", t["b2"])])
            for t in T_:
                for dc in range(8):
                    B.op("pe", lambda e, dc=dc, t=t: e.matmul(pb[t["igu"]][:, :], lhsT=xT[t["b2"]][:, dc, :], rhs=wgu[s][:, dc, :],
                                                              start=(dc == 0), stop=(dc == 7)),
                         reads=[("xT", t["b2"])] + wkeys, writes=[("pb", t["igu"])])
                B.op("act", lambda e, t=t: e.activation(out=sg[t["b2"]][:], in_=pb[t["igu"]][:, 0:256], func=AF.Silu),
                     reads=[("pb", t["igu"])], writes=[("sg", t["b2"])])
                B.op("dve", lambda e, t=t: e.tensor_tensor(out=hid[t["b2"]][:], in0=pb[t["igu"]][:, 256:512], in1=sg[t["b2"]][:], op=ALU.mult),
                     reads=[("pb", t["igu"]), ("sg", t["b2"])], writes=[("hid", t["b2"])])
            for t in T_:
                ip2 = nextpt()
                ptv2 = pt[ip2][:, 0:256].rearrange("p (a b) -> p a b", a=2)
                for fc_ in range(2):
                    B.op("pe", lambda e, fc_=fc_, t=t, ptv2=ptv2: e.transpose(ptv2[:, fc_, :], hid[t["b2"]][:, fc_ * 128:(fc_ + 1) * 128], identb[:]),
                         reads=[("hid", t["b2"]), "identb"], writes=[("pt", ip2)])
                B.op("dve", lambda e, t=t, ptv2=ptv2: e.tensor_copy(hidT[t["b2"]][:], ptv2), reads=[("pt", ip2)], writes=[("hidT", t["b2"])])
            for t in T_:
                iy, b2 = t["iy"], t["b2"]
                for half in range(2):
                    for fc_ in range(2):
                        B.op("pe", lambda e, fc_=fc_, half=half, iy=iy, b2=b2: e.matmul(pb[iy + half][:, :], lhsT=hidT[b2][:, fc_, :],
                                                                                      rhs=wdn[s][:, fc_, half * 512:(half + 1) * 512],
                                                                                      start=(fc_ == 0), stop=(fc_ == 1)),
                             reads=[("hidT", b2), ("wd", s)], writes=[("pb", iy + half)])
                B.op("act", lambda e, iy=iy, b2=b2: e.activation(out=ysb[b2][:, 0:512], in_=pb[iy][:, :], func=AF.Copy),
                     reads=[("pb", iy)], writes=[("ysb", b2, 0)])
                B.op("dve", lambda e, iy=iy, b2=b2: e.tensor_copy(ysb[b2][:, 512:1024], pb[iy + 1][:, :]),
                     reads=[("pb", iy + 1)], writes=[("ysb", b2, 1)])
                ykey = ("ysd", e_, t["k"], flag)
                B.dma("act", ys_scr[t["row0"]:t["row0"] + 128, :], ysb[b2][:], reads=[("ysb", b2, 0), ("ysb", b2, 1)], writes=[ykey],
                      semkey=("ys_out", b2))
                ys_keys.append(ykey)
            B.end_cond()

        def moe_tile(e_, k):
            moe_tiles(e_, [k], e_ * 16 + k)

        for e_ in range(NE):
            moe_tiles(e_, [0, 1], e_ * 16 + 1)
            moe_tiles(e_, [0], NE * 17 + 1 + e_)
            if e_ + 2 < NE:
                load_expert(e_ + 2)

        wst = zt.bitcast(F32)
        B.begin_cond(NE * 17)
        for e_ in range(NE):
            s = e_ % 2
            B.begin_cond(NE * 16 + e_)
            for dc in range(8):
                B.dma("sp", wst[:, 0:256], wg_d[e_, dc * 128:(dc + 1) * 128, :], reads=scat_keys, writes=["zt"], semkey="wst")
                B.op("dve", lambda e, dc=dc, s=s: e.tensor_copy(wgu[s][:, dc, 0:256], wst[:, 0:256]), reads=["zt"], writes=[("wg", s)])
                B.dma("sp", wst[:, 256:512], wu_d[e_, dc * 128:(dc + 1) * 128, :], reads=scat_keys, writes=["zt2"], semkey="wst2")
                B.op("dve", lambda e, dc=dc, s=s: e.tensor_copy(wgu[s][:, dc, 256:512], wst[:, 256:512]), reads=["zt2"], writes=[("wu", s)])
            for fc_ in range(2):
                for half in range(2):
                    B.dma("sp", wst[:, :], wd_d[e_, fc_ * 128:(fc_ + 1) * 128, half * 512:(half + 1) * 512], reads=scat_keys,
                          writes=["zt", "zt2"], semkey="wst")
                    B.op("dve", lambda e, fc_=fc_, half=half, s=s: e.tensor_copy(wdn[s][:, fc_, half * 512:(half + 1) * 512], wst[:, :]),
                         reads=["zt", "zt2"], writes=[("wd", s)])
            for k in range(2, 16):
                moe_tile(e_, k)
            B.end_cond()
        B.end_cond()

        B.barrier()
        ar.seek(OFF_P + 32 * 1024)
        rg = [ar.alloc([128, D], BF16) for _ in range(8)]
        B.dma("sp", g2bc[:], g3bc_d, reads=[], writes=["g2bc"], semkey="g3load")
        def ind_gather(dst_ap, idx_ap, reads, wkey):
            def fn(e):
                return e.indirect_dma_start(out=dst_ap, out_offset=None, in_=ys_scr[:, :],
                                            in_offset=bass.IndirectOffsetOnAxis(ap=idx_ap, axis=0),
                                            bounds_check=bcreg(e), oob_is_err=False)
            B.op("pool", fn, reads=reads, writes=[wkey], dma=True)

        for j in range(16):
            xb = x3[j % 2]
            ob = x1t[j % 2]
            r1 = rg[(j % 4) * 2]
            r2 = rg[(j % 4) * 2 + 1]
            ind_gather(r1[:, :], sidx[:, j, 0:1], ys_keys + [("sidx", j)], ("rg", (j % 4) * 2))
            ind_gather(r2[:, :], sidx[:, j, 1:2], ys_keys + [("sidx", j)], ("rg", (j % 4) * 2 + 1))
            B.dma("sp", xb[:], x1_d[j * 128:(j + 1) * 128, :], reads=[("x1d", j)], writes=[("x3", j % 2)])
            B.op("dve", lambda e, xb=xb, r1=r1, j=j: e.scalar_tensor_tensor(out=xb[:], in0=r1[:], scalar=cw[:, j, 0:1], in1=xb[:],
                                                                        op0=ALU.mult, op1=ALU.add),
                 reads=[("x3", j % 2), ("rg", (j % 4) * 2), ("cwq", 0)], writes=[("x3", j % 2)])
            B.op("dve", lambda e, xb=xb, r2=r2, j=j: e.scalar_tensor_tensor(out=xb[:], in0=r2[:], scalar=cw[:, j, 1:2], in1=xb[:],
                                                                        op0=ALU.mult, op1=ALU.add),
                 reads=[("x3", j % 2), ("rg", (j % 4) * 2 + 1), ("cwq", 1)], writes=[("x3", j % 2)])
            pj = j % 2
            obk = [("x1t", pj, 0), ("x1t", pj, 1)]
            B.op("act", lambda e, xb=xb, ob=ob, pj=pj: e.activation(out=ob[:], in_=xb[:], func=AF.Square, accum_out=ss4[:, pj:pj + 1]),
                 reads=[("x3", pj)], writes=obk + [("ss4", pj)])
            B.op("dve", lambda e, pj=pj: e.tensor_scalar(out=rstd4[:, pj:pj + 1], in0=ss4[:, pj:pj + 1], scalar1=1.0 / D, scalar2=1e-6, op0=ALU.mult, op1=ALU.add),
                 reads=[("ss4", pj)], writes=[("rstd4", pj)])
            B.op("pool", lambda e, pj=pj: e.tensor_tensor(out=rstd4[:, pj:pj + 1], in0=rstd4[:, pj:pj + 1], in1=mhalf[:], op=ALU.pow),
                 reads=[("rstd4", pj), "mhalf"], writes=[("rstd4", pj)])
            B.op("act", lambda e, xb=xb, ob=ob, pj=pj: e.activation(out=ob[:], in_=xb[:], func=AF.Copy, scale=rstd4[:, pj:pj + 1]),
                 reads=[("x3", pj), ("rstd4", pj)], writes=obk)
            B.op("dve", lambda e, ob=ob: e.tensor_tensor(out=ob[:], in0=ob[:], in1=g3bc[:], op=ALU.mult), reads=obk + ["g2bc"], writes=obk)
            B.dma("sp", out_d[j * 128:(j + 1) * 128, :], ob[:], reads=[("x1t", j % 2, 0), ("x1t", j % 2, 1)], writes=[("outd", j)],
                  semkey=("ob_out", j % 2))

        if os.environ.get("MK_NOPS"):
            B.ops = B.ops[:int(os.environ["MK_NOPS"])]
        B.emit(st)
        _NC_CACHE["B"] = B
    return nc


_NC_CACHE = {}


def _layout_inputs(inp):
    f = lambda a: np.ascontiguousarray(np.asarray(a, dtype=np.float32))
    x = f(inp["x"])
    common = {}
    common["ident"] = np.eye(128, dtype=np.float32)
    common["onesd"] = np.ones((NH, 3, 512), ml_dtypes.bfloat16)
    ee = np.arange(NE, dtype=np.float32)[None, :] * CAP
    common["ebase1"] = f(np.broadcast_to(ee + 1.0, (128, NE)))
    common["ebase16"] = f(np.broadcast_to((ee + 1.0)[:, None, :], (128, 16, NE)).reshape(128, 16 * NE))
    common["padbase"] = f(ee + np.arange(128, dtype=np.float32)[:, None])
    common["triu"] = (np.arange(128)[:, None] < np.arange(128)[None, :]).astype(np.float32)
    k = np.arange(128)
    common["maskT"] = np.where(k[:, None] <= k[None, :], 0.0, NEG).astype(np.float32)
    common["gcol"] = f(inp["mix_norm"][0].reshape(8, 128).T)
    common["w_in"] = f(inp["w_in"][0])
    common["bfcol"] = f(inp["b_forget"][0].reshape(8, 1))
    cw = f(inp["conv_w"][0])
    cd = np.zeros((128, 16, 128), np.float32)
    for tap in range(4):
        for cc in range(4):
            cd[k, tap * 4 + cc, k] = cw[tap, cc * 128:(cc + 1) * 128]
    common["cdiag"] = cd
    common["convb"] = f(inp["conv_b"][0].reshape(4, 128).T)
    for nm, key in (("bda", "w_a"), ("bdx", "w_x")):
        w = f(inp[key][0])
        bd = np.zeros((128, 4, 128), np.float32)
        for cc in range(4):
            bd[0:64, cc, 0:64] = w[2 * cc]
            bd[64:128, cc, 64:128] = w[2 * cc + 1]
        common[nm] = bd
    common["bacol"] = f(inp["b_a"][0].reshape(4, 128).T)
    common["bxcol"] = f(inp["b_x"][0].reshape(4, 128).T)
    common["lamcol"] = f(inp["lru_lambda"][0].reshape(4, 128).T)
    common["w_out"] = f(inp["w_out"][0])
    common["g2bc"] = f(np.broadcast_to(inp["ffn_norm"][0][None, :], (128, D)))
    common["g3bc"] = f(np.broadcast_to(np.asarray(inp["final_norm"])[None, :], (128, D)))
    wi = np.asarray(inp["w_inner"][0], dtype=np.float32)
    common["wr"] = f(np.concatenate([np.asarray(inp["w_group"][0], dtype=np.float32), wi.transpose(1, 0, 2).reshape(D, 32)], axis=1))
    rb = np.concatenate([np.asarray(inp["b_group"][0], dtype=np.float32), np.asarray(inp["b_inner"][0], dtype=np.float32).reshape(32)])
    common["rbbc"] = f(np.broadcast_to(rb[None, :], (128, 36)))
    common["w_gate"] = f(np.asarray(inp["w_gate"][0]).reshape(NE, D, FE))
    common["w_up"] = f(np.asarray(inp["w_up"][0]).reshape(NE, D, FE))
    common["w_down"] = f(np.asarray(inp["w_down"][0]).reshape(NE, FE, D))
    maps = []
    for c in range(8):
        b, half = c // 2, c % 2
        m = dict(common)
        if half == 1:
            m["xs"] = f(x[b])
            m["kmrow"] = np.zeros((1, NTOK), ml_dtypes.bfloat16)
        else:
            m["xs"] = f(np.concatenate([np.zeros((NOWN, D), np.float32), x[b, :NOWN]], axis=0))
            km = np.zeros((1, NTOK), np.float32)
            km[0, :NOWN] = NEG
            m["kmrow"] = km.astype(ml_dtypes.bfloat16)
        m["flag"] = np.full((128, 1), float(half), np.float32)
        maps.append(m)
    return maps


def kernel(**inputs):
    if "nc" not in _NC_CACHE:
        _NC_CACHE["nc"] = build_program()
    nc = _NC_CACHE["nc"]
    maps = _layout_inputs(inputs)
    res = run_bass_kernel_spmd(nc, maps, core_ids=list(range(8)))
    out = np.zeros((4, SEQ, D), np.float32)
    for c in range(8):
        b, half = c // 2, c % 2
        out[b, half * NOWN:(half + 1) * NOWN] = res.results[c]["out"]
    return out
```

```python
import os
import numpy as np
import ml_dtypes
from contextlib import ExitStack
import concourse.bass as bass
import concourse.mybir as mybir
from concourse.bass_utils import run_bass_kernel_spmd

F32 = mybir.dt.float32
BF16 = mybir.dt.bfloat16
AF = mybir.ActivationFunctionType
ALU = mybir.AluOpType
AX = mybir.AxisListType

ENGS = ("sp", "act", "dve", "pool", "pe")

D = 1024
SEQ = 4096
NTOK = 4096
NOWN = 2048
NH = 8
HD = 64
KA = 71
NEG = -30000.0
NE = 32
FE = 256
CAP = 2048 + 128
I32 = mybir.dt.int32
STAGE = int(os.environ.get("MK_STAGE", "9"))


class Builder:
    def __init__(self, nc):
        self.nc = nc
        self.ops = []
        self.last_write = {}
        self.reads_since = {}
        self.same_engine_sync = {"act": True, "dve": True, "pool": True, "pe": False, "sp": False}
        self.barrier_deps = set()
        self.names = {}
        self.cur_cond = None
        self.flags_ap = None
        self.flags_key = None

    def op(self, eng, fn, reads=(), writes=(), dma=False, nobarrier=False, semgroup=None, semkey=None):
        deps = set()
        for b in reads:
            w = self.last_write.get(b)
            if w is not None:
                deps.add(w)
        for b in writes:
            w = self.last_write.get(b)
            if w is not None:
                deps.add(w)
            deps.update(self.reads_since.get(b, ()))
        if not nobarrier:
            deps.update(self.barrier_deps)
        idx = len(self.ops)
        self.ops.append(dict(eng=eng, fn=fn, deps=deps, dma=dma, wkey=(("grp", semgroup) if semgroup else (("sk", semkey) if semkey else (writes[0] if writes else None))),
                             grp=semgroup is not None, cond=self.cur_cond))
        for b in reads:
            self.reads_since.setdefault(b, []).append(idx)
        for b in writes:
            self.last_write[b] = idx
            self.reads_since[b] = []
        return idx

    def dma(self, eng, out, in_, reads=(), writes=(), nobarrier=False, semgroup=None, semkey=None, **kw):
        def fn(e):
            return e.dma_start(out=out, in_=in_, **kw)
        return self.op(eng, fn, reads, writes, dma=True, nobarrier=nobarrier, semgroup=semgroup, semkey=semkey)

    def begin_cond(self, c):
        self.cur_cond = (self.cur_cond or ()) + (c,)

    def end_cond(self):
        self.cur_cond = self.cur_cond[:-1] or None

    def barrier(self):
        last = {}
        deps = set()
        for i, o in enumerate(self.ops):
            if o["dma"]:
                deps.add(i)
            else:
                last[o["eng"]] = i
        latest = {}
        for i in deps:
            latest[self.ops[i]["wkey"]] = max(latest.get(self.ops[i]["wkey"], -1), i)
        self.barrier_deps = set(latest.values()) | set(last.values())

    def emit(self, stack):
        nc = self.nc
        ops = self.ops
        n = len(ops)
        needed = [False] * n
        for i, o in enumerate(ops):
            pruned = set()
            for d in o["deps"]:
                od = ops[d]
                if (not od["dma"]) and od["eng"] == o["eng"] and not self.same_engine_sync[o["eng"]]:
                    continue
                pruned.add(d)
            o["deps"] = pruned
            for d in pruned:
                needed[d] = True
        if self.flags_key is not None:
            needed[self.last_write[self.flags_key]] = True
        eng_sem = {e: stack.enter_context(nc.semaphore("S_" + e)) for e in ENGS}
        dma_sems = {}
        dma_count = {}
        eng_count = {e: 0 for e in ENGS}
        for i, o in enumerate(ops):
            if o["dma"]:
                k = o["wkey"]
                if k not in dma_sems:
                    dma_sems[k] = stack.enter_context(nc.semaphore("D%d" % len(dma_sems)))
                    dma_count[k] = 0
                dma_count[k] += 16
                o["sig"] = (dma_sems[k], dma_count[k], ("d", k))
            elif needed[i]:
                eng_count[o["eng"]] += 1
                o["sig"] = (eng_sem[o["eng"]], eng_count[o["eng"]], ("e", o["eng"]))
            else:
                o["sig"] = None
        for o in ops:
            if o["dma"] and o["grp"]:
                sem, val, key = o["sig"]
                o["sig"] = (sem, dma_count[o["wkey"]], key)
        self.n_sems = len(dma_sems) + len(ENGS)
        print("ops", n, "sems", self.n_sems)
        per_eng = {e: [] for e in ENGS}
        for i, o in enumerate(ops):
            per_eng[o["eng"]].append(i)
        final_waits = [(dma_sems[k], dma_count[k], ("d", k)) for k in dma_sems]

        flag_sig = None
        if self.flags_key is not None:
            fo = ops[self.last_write[self.flags_key]]
            flag_sig = fo["sig"]
            assert flag_sig is not None

        def run_engine(ename, e):
            known = {}
            reg = None

            def emit_op(i):
                o = ops[i]
                need = {}
                for d in o["deps"]:
                    sem, val, key = ops[d]["sig"]
                    if known.get(key, 0) >= val:
                        continue
                    if key not in need or need[key][1] < val:
                        need[key] = (sem, val)
                for key, (sem, val) in need.items():
                    e.wait_ge(sem, val)
                    known[key] = val
                ins = o["fn"](e)
                try:
                    self.names[ins.ins.name] = i
                except Exception:
                    pass
                if o["sig"] is not None:
                    ins.then_inc(o["sig"][0], 16 if o["dma"] else 1)

            def emit_list(idxs, depth):
                nonlocal reg
                p = 0
                while p < len(idxs):
                    cpath = ops[idxs[p]]["cond"] or ()
                    if len(cpath) <= depth:
                        emit_op(idxs[p])
                        p += 1
                        continue
                    c = cpath[depth]
                    blk = []
                    while p < len(idxs):
                        cp2 = ops[idxs[p]]["cond"] or ()
                        if len(cp2) > depth and cp2[depth] == c:
                            blk.append(idxs[p])
                            p += 1
                        else:
                            break
                    if reg is None:
                        reg = e.alloc_register("cr_" + ename)
                    sem, val, key = flag_sig
                    if known.get(key, 0) < val:
                        e.wait_ge(sem, val)
                        known[key] = val
                    e.reg_load(reg, self.flags_ap[0:1, c:c + 1])
                    snap = dict(known)
                    with e.If(reg):
                        emit_list(blk, depth + 1)
                    known.clear()
                    known.update(snap)
                    comp = {}
                    for i in blk:
                        sg_ = ops[i]["sig"]
                        if sg_ is None:
                            continue
                        sem, val, key = sg_
                        inc = 16 if ops[i]["dma"] else 1
                        if key not in comp:
                            comp[key] = [sem, val - inc, 0]
                        comp[key][2] += inc
                    if comp:
                        with e.Else():
                            for key, (sem, prev, tot) in comp.items():
                                if prev > 0:
                                    e.wait_ge(sem, prev)
                                e.sem_inc(sem, tot)

            emit_list(per_eng[ename], 0)
            if ename == "sp":
                for sem, val, key in final_waits:
                    if known.get(key, 0) < val:
                        e.wait_ge(sem, val)

        block = stack.enter_context(nc.Block())

        @block.sync
        def _(e):
            run_engine("sp", e)

        @block.scalar
        def _(e):
            run_engine("act", e)

        @block.vector
        def _(e):
            run_engine("dve", e)

        @block.gpsimd
        def _(e):
            run_engine("pool", e)

        @block.tensor
        def _(e):
            run_engine("pe", e)


class Deferred:
    def __init__(self):
        self.q = []
        self.n = 0

    def at(self, target, fn):
        self.q.append((target, self.n, fn))
        self.n += 1

    def run(self, now):
        ready = sorted([x for x in self.q if x[0] <= now])
        self.q = [x for x in self.q if x[0] > now]
        for _, _, fn in ready:
            fn()

    def flush(self):
        self.run(1 << 60)


class Arena:
    def __init__(self, nc, stack, nbytes):
        self.t = stack.enter_context(nc.sbuf_tensor("arena", [128, nbytes // 4], F32))
        self.nbytes = nbytes
        self.off = 0

    def seek(self, off):
        self.off = off

    def alloc(self, shape, dt):
        n = 1
        for s in shape[1:]:
            n *= s
        esz = 4 if dt == F32 else 2
        nb = (n * esz + 31) // 32 * 32
        assert self.off + nb <= self.nbytes, (self.off, nb, self.nbytes)
        v = self.t[:, self.off // 4:(self.off + nb) // 4]
        if dt != F32:
            v = v.bitcast(dt)
        v = v[:, 0:n]
        if len(shape) == 3:
            v = v.rearrange("p (a b) -> p a b", a=shape[1])
        elif len(shape) == 4:
            v = v.rearrange("p (a b c) -> p a b c", a=shape[1], b=shape[2])
        self.off += nb
        return v


def build_program():
    nc = bass.Bass("TRN2", target_bir_lowering=False)
    din = lambda name, shape, dt=F32: nc.dram_tensor(name, list(shape), dt, kind="ExternalInput").ap()
    xs = din("xs", [NTOK, D])
    ident_d = din("ident", [128, 128])
    maskT_d = din("maskT", [128, 128])
    flag_d = din("flag", [128, 1])
    kmrow_d = din("kmrow", [1, NTOK], BF16)
    ones_d = din("onesd", [NH, 3, 512], BF16)
    gcol_d = din("gcol", [128, 8])
    w_in_d = din("w_in", [D, 2568])
    bf_d = din("bfcol", [8, 1])
    cdiag_d = din("cdiag", [128, 16, 128])
    convb_d = din("convb", [128, 4])
    bda_d = din("bda", [128, 4, 128])
    bdx_d = din("bdx", [128, 4, 128])
    ba_d = din("bacol", [128, 4])
    bx_d = din("bxcol", [128, 4])
    lam_d = din("lamcol", [128, 4])
    w_out_d = din("w_out", [D, D])
    g2bc_d = din("g2bc", [128, D])
    g3bc_d = din("g3bc", [128, D])
    wr_d = din("wr", [D, 36])
    rb_d = din("rbbc", [128, 36])
    wg_d = din("w_gate", [NE, D, FE])
    wu_d = din("w_up", [NE, D, FE])
    wd_d = din("w_down", [NE, FE, D])
    ebase1_d = din("ebase1", [128, NE])
    eb16_d = din("ebase16", [128, 16 * NE])
    padbase_d = din("padbase", [128, NE])
    triu_d = din("triu", [128, 128])
    out_d = nc.dram_tensor("out", [NOWN, D], F32, kind="ExternalOutput").ap()
    xs_scr = nc.dram_tensor("xs_scr", [NE * CAP, D], BF16).ap()
    ys_scr = nc.dram_tensor("ys_scr", [NE * CAP, D], BF16).ap()
    kaug_d = nc.dram_tensor("kaug_s", [NH, KA, NTOK], BF16).ap()
    qaug_d = nc.dram_tensor("qaug_s", [NH, KA, NOWN], BF16).ap()
    x1_d = nc.dram_tensor("x1_s", [NOWN, D], F32).ap()

    st = ExitStack()
    with st:
        B = Builder(nc)
        sb = lambda name, shape, dt: st.enter_context(nc.sbuf_tensor("s_" + name, shape, dt))
        identf = sb("identf", [128, 128], F32)
        identb = sb("identb", [128, 128], BF16)
        maskf = sb("maskf", [128, 128], F32)
        maskb = sb("maskb", [128, 128], BF16)
        onesf = sb("onesf", [128, 64], F32)
        flag = sb("flag", [128, 1], F32)
        gcol = sb("gcol", [128, 8], F32)
        bfcol = sb("bfcol", [8, 1], F32)
        nbfcol = sb("nbfcol", [8, 1], F32)
        convb = sb("convbc", [128, 4], F32)
        bacol = sb("bacol", [128, 4], F32)
        bxcol = sb("bxcol", [128, 4], F32)
        lamcol = sb("lamcol", [128, 4], F32)
        s1col = sb("s1col", [128, 4], F32)
        s2col = sb("s2col", [128, 4], F32)
        hbacol = sb("hbacol", [128, 4], F32)
        hbxcol = sb("hbxcol", [128, 4], F32)
        hs1col = sb("hs1col", [128, 4], F32)
        hs2col = sb("hs2col", [128, 4], F32)
        qcol = sb("qcol", [128, 1], F32)
        mhalf = sb("mhalf", [128, 1], F32)
        g2bc = sb("g2bc", [128, D], F32)
        g3bc = g2bc
        rbbc = sb("rbbc", [128, 36], F32)
        wr = sb("wr", [128, 8, 36], BF16)
        comb = sb("comb", [128, 16, NE], F32)
        hstate = sb("hstate", [128, 4], F32)
        cstate = sb("cstate", [8, 1], F32)
        ss4 = sb("ss4", [128, 4], F32)
        rstd4 = sb("rstd4", [128, 4], F32)
        rt = sb("rt", [128, 64], F32)

        ar = Arena(nc, st, 186 * 1024)
        OFF_V = 0
        OFF_LRU = OFF_V + 34 * 1024
        OFF_ATT = OFF_LRU + 16 * 1024
        OFF_P = OFF_ATT + 32 * 1024
        ar.seek(OFF_V)
        vaug = ar.alloc([128, 32, NH, 65], BF16)
        ar.seek(OFF_LRU)
        lruo = ar.alloc([128, 4, NOWN], BF16)
        ar.seek(OFF_ATT)
        attT = ar.alloc([128, NH, NOWN], BF16)

        pb = [st.enter_context(nc.psum_tensor("pb%d" % i, [128, 512], F32)) for i in range(6)]
        pt = [st.enter_context(nc.psum_tensor("pt%d" % i, [128, 1024], BF16)) for i in range(2)]
        rr = {"pb": 0, "pt": 0}

        def nextpb():
            i = rr["pb"]
            rr["pb"] = (i + 1) % 6
            return i

        def nextpt():
            i = rr["pt"]
            rr["pt"] = (i + 1) % 2
            return i

        B.dma("sp", identf[:], ident_d, writes=["identf"], semgroup="csp")
        B.dma("sp", maskf[:], maskT_d, writes=["maskf"], semgroup="csp")
        B.dma("sp", flag[:], flag_d, writes=["flag"], semgroup="csp")
        B.dma("sp", gcol[:], gcol_d, writes=["gcol"], semgroup="csp")
        B.dma("sp", bfcol[:], bf_d, writes=["bfcol"], semgroup="csp")
        B.dma("sp", convb[:], convb_d, writes=["convb"], semgroup="csp")
        B.dma("sp", bacol[:], ba_d, writes=["bacol"], semgroup="csp")
        B.dma("sp", bxcol[:], bx_d, writes=["bxcol"], semgroup="csp")
        B.dma("sp", lamcol[:], lam_d, writes=["lamcol"], semgroup="csp")
        B.dma("sp", g2bc[:], g2bc_d, writes=["g2bc"], semgroup="csp")
        B.dma("sp", rbbc[:], rb_d, writes=["rbbc"], semgroup="csp")
        B.dma("pool", wr[:], wr_d.rearrange("(c p) f -> p c f", p=128), writes=["wr"], semgroup="cpl")
        B.op("dve", lambda e: e.tensor_copy(identb[:], identf[:]), reads=["identf"], writes=["identb"])
        B.op("dve", lambda e: e.tensor_copy(maskb[:], maskf[:]), reads=["maskf"], writes=["maskb"])
        B.op("dve", lambda e: e.memset(onesf[:], 1.0), writes=["onesf"])
        B.op("dve", lambda e: e.memset(hstate[:], 0.0), writes=["hstate"])
        B.op("dve", lambda e: e.memset(cstate[:], 0.0), writes=["cstate"])
        B.op("dve", lambda e: e.tensor_scalar(out=nbfcol[:], in0=bfcol[:], scalar1=-1.0, scalar2=None, op0=ALU.mult),
             reads=["bfcol"], writes=["nbfcol"])
        B.op("act", lambda e: e.activation(out=s1col[:], in_=lamcol[:], func=AF.Exp, scale=-1.0), reads=["lamcol"], writes=["s1col"])
        B.op("act", lambda e: e.activation(out=s1col[:], in_=s1col[:], func=AF.Ln, bias=1.0), reads=["s1col"], writes=["s1col"])
        B.op("dve", lambda e: e.tensor_scalar(out=s2col[:], in0=s1col[:], scalar1=-16.0, scalar2=None, op0=ALU.mult),
             reads=["s1col"], writes=["s2col"])
        B.op("dve", lambda e: e.tensor_scalar(out=s1col[:], in0=s1col[:], scalar1=-8.0, scalar2=None, op0=ALU.mult),
             reads=["s1col", "s2col"], writes=["s1col"])
        for dst, src, nm in ((hbacol, bacol, "hbacol"), (hbxcol, bxcol, "hbxcol"), (hs1col, s1col, "hs1col"), (hs2col, s2col, "hs2col")):
            B.op("dve", lambda e, dst=dst, src=src: e.tensor_scalar(out=dst[:], in0=src[:], scalar1=0.5, scalar2=None, op0=ALU.mult),
                 reads=["bacol", "bxcol", "s1col", "s2col"], writes=[nm])
        B.op("dve", lambda e: e.memset(qcol[:], 0.25), writes=["qcol"])
        B.op("dve", lambda e: e.memset(mhalf[:], -0.5), writes=["mhalf"])
        for h in range(NH):
            B.dma("sp", kaug_d[h, 70:71, :], kmrow_d, writes=[("kaugc", h)], semgroup="kscr")
        for t in range(8):
            B.dma("sp", kaug_d[:, 64:67, t * 512:(t + 1) * 512], ones_d, writes=[("kaug1", t)], semgroup="kscr")
        for t in range(4):
            B.dma("sp", qaug_d[:, 67:70, t * 512:(t + 1) * 512], ones_d, writes=[("qaug1", t)], semgroup="qscr")
            B.dma("sp", qaug_d[:, 70:71, t * 512:(t + 1) * 512], ones_d[:, 0:1, :], writes=[("qaug2", t)], semgroup="qscr")

        ar.seek(OFF_ATT)
        win = ar.alloc([128, 8, 2568], BF16)
        stage = [ar.alloc([128, 642], F32) for _ in range(2)]
        hT = [ar.alloc([128, 8, 512], BF16) for _ in range(2)]
        xt = [ar.alloc([128, D], F32) for _ in range(2)]
        xn = [ar.alloc([128, D], BF16) for _ in range(2)]
        rin = [ar.alloc([128, 4, 516], BF16) for _ in range(2)]
        cvb = ar.alloc([128, 4, 512], BF16)
        kst = ar.alloc([128, NH, 512], BF16)
        qst = ar.alloc([128, NH, 512], BF16)
        tr = ar.alloc([128, 512], F32)
        ti = ar.alloc([128, 512], F32)
        ta = ar.alloc([128, 512], F32)
        tm = ar.alloc([128, 512], F32)
        tu = ar.alloc([128, 512], F32)
        th = ar.alloc([128, 512], F32)
        gel = ar.alloc([128, 512], F32)
        fl = ar.alloc([128, 512], F32)
        fc = ar.alloc([128, 512], F32)
        fr = ar.alloc([128, 512], F32)
        fp = ar.alloc([128, 6, 512], BF16)
        cdiag = ar.alloc([128, 16, 128], BF16)
        bda = ar.alloc([128, 4, 128], BF16)
        bdx = ar.alloc([128, 4, 128], BF16)
        P1_END = ar.off
        B.dma("pool", cdiag[:], cdiag_d, writes=["cdiag"])
        B.dma("pool", bda[:], bda_d, writes=["bda"])
        B.dma("pool", bdx[:], bdx_d, writes=["bdx"])

        for dc in range(8):
            for q in range(4):
                s = (dc * 4 + q) % 2
                B.dma("sp", stage[s][:], w_in_d[dc * 128:(dc + 1) * 128, q * 642:(q + 1) * 642], writes=[("stage", s)])
                B.op("dve", lambda e, dc=dc, q=q, s=s: e.tensor_scalar(out=win[:, dc, q * 642:(q + 1) * 642], in0=stage[s][:],
                                                                     scalar1=gcol[:, dc:dc + 1], scalar2=None, op0=ALU.mult),
                     reads=[("stage", s), "gcol"], writes=[("win", dc)])
        WK, WV, WF, WG, WR = 512, 1024, 1536, 1544, 2056
        winr = [("win", dc) for dc in range(8)]

        B.op("dve", lambda e: e.memset(rin[1][:], 0.0), writes=[("rin", 1)])
        B.op("dve", lambda e: e.memset(rin[0][:, :, 0:4], 0.0), writes=[("rinh", 0)])
        B.op("dve", lambda e: e.memset(vaug[:], 1.0), writes=["vaug_init"])

        def tile_ctx(T):
            return T >= 4, hT[T % 2], ("hT", T % 2)

        def piece_N(T):
            own, hb, hkey = tile_ctx(T)
            for pr in range(2):
                blks = (2 * pr, 2 * pr + 1)
                for blk in blks:
                    g = T * 4 + blk
                    xb = xt[g % 2]
                    B.dma("sp", xb[:], xs[g * 128:(g + 1) * 128, :], writes=[("xt", g % 2)])
                    B.op("dve", lambda e, xb=xb, blk=blk: e.scalar_tensor_tensor(out=xn[0][:], in0=xb[:], scalar=1.0, in1=xb[:],
                                                                                 op0=ALU.mult, op1=ALU.mult, accum_out=ss4[:, blk:blk + 1]),
                         reads=[("xt", g % 2)], writes=[("xnjunk",), ("ss4", blk)])
                rk = [("rstd4", k) for k in blks]
                c0 = 2 * pr
                B.op("dve", lambda e, c0=c0: e.tensor_scalar(out=rstd4[:, c0:c0 + 2], in0=ss4[:, c0:c0 + 2], scalar1=1.0 / D, scalar2=1e-6,
                                                             op0=ALU.mult, op1=ALU.add),
                     reads=[("ss4", k) for k in blks], writes=rk)
                B.op("act", lambda e, c0=c0: e.activation(out=rstd4[:, c0:c0 + 2], in_=rstd4[:, c0:c0 + 2], func=AF.Sqrt), reads=rk, writes=rk)
                B.op("dve", lambda e, c0=c0: e.reciprocal(rstd4[:, c0:c0 + 2], rstd4[:, c0:c0 + 2]), reads=rk, writes=rk)
                for blk in blks:
                    g = T * 4 + blk
                    xb = xt[g % 2]
                    B.op("dve", lambda e, xb=xb, blk=blk: e.tensor_scalar(out=xn[1][:], in0=xb[:], scalar1=rstd4[:, blk:blk + 1], scalar2=None, op0=ALU.mult),
                         reads=[("xt", g % 2), ("rstd4", blk)], writes=[("xn", 1)])
                    ip = nextpt()
                    ptv = pt[ip][:].rearrange("p (a b) -> p a b", a=8)
                    for c in range(8):
                        B.op("pe", lambda e, c=c, ptv=ptv: e.transpose(ptv[:, c, :], xn[1][:, c * 128:(c + 1) * 128], identb[:]),
                             reads=[("xn", 1), "identb"], writes=[("pt", ip)])
                    B.op("dve", lambda e, ptv=ptv, hb=hb, blk=blk: e.tensor_copy(hb[:, :, blk * 128:(blk + 1) * 128], ptv),
                         reads=[("pt", ip)], writes=[hkey])

        def piece_K(T, pairs, last):
            own, hb, hkey = tile_ctx(T)
            for pr in pairs:
                ib = nextpb()
                for dc in range(8):
                    B.op("pe", lambda e, pr=pr, dc=dc, ib=ib: e.matmul(pb[ib][:, :], lhsT=win[:, dc, WK + pr * 128:WK + (pr + 1) * 128],
                                                                       rhs=hb[:, dc, :], start=(dc == 0), stop=(dc == 7)),
                         reads=[hkey] + winr, writes=[("pb", ib)])
                B.op("dve", lambda e, pr=pr, ib=ib: e.tensor_copy(kst[:, pr, :], pb[ib][:, :]), reads=[("pb", ib)], writes=[("kst", pr)])
            if last:
                kd = kaug_d[:, 0:64, T * 512:(T + 1) * 512].rearrange("(pr two) p t -> two p pr t", two=2)
                for two in range(2):
                    B.dma("sp", kd[two], kst[two * 64:(two + 1) * 64, 0:4, :],
                          reads=[("kst", pr) for pr in range(4)], writes=[("kaugd", T, two)], semkey="kst_out%d" % two)

        def piece_Q(T):
            own, hb, hkey = tile_ctx(T)
            if not own:
                return
            for pr in range(4):
                ib = nextpb()
                for dc in range(8):
                    B.op("pe", lambda e, pr=pr, dc=dc, ib=ib: e.matmul(pb[ib][:, :], lhsT=win[:, dc, pr * 128:(pr + 1) * 128],
                                                                       rhs=hb[:, dc, :], start=(dc == 0), stop=(dc == 7)),
                         reads=[hkey] + winr, writes=[("pb", ib)])
                B.op("act", lambda e, pr=pr, ib=ib: e.activation(out=qst[:, pr, :], in_=pb[ib][:, :], func=AF.Copy, scale=0.125),
                     reads=[("pb", ib)], writes=[("qst", pr)])
            qd = qaug_d[:, 0:64, (T - 4) * 512:(T - 3) * 512].rearrange("(pr two) p t -> two p pr t", two=2)
            for two in range(2):
                B.dma("sp", qd[two], qst[two * 64:(two + 1) * 64, 0:4, :],
                      reads=[("qst", pr) for pr in range(4)], writes=[("qaugd", T, two)], semkey="qst_out%d" % two)

        def piece_V(T):
            own, hb, hkey = tile_ctx(T)
            for blk in range(4):
                g = T * 4 + blk
                ib = nextpb()
                for dc in range(8):
                    B.op("pe", lambda e, blk=blk, dc=dc, ib=ib: e.matmul(pb[ib][:, :], lhsT=hb[:, dc, blk * 128:(blk + 1) * 128],
                                                                         rhs=win[:, dc, WV:WV + 512], start=(dc == 0), stop=(dc == 7)),
                         reads=[hkey] + winr, writes=[("pb", ib)])
                B.op("dve", lambda e, g=g, ib=ib: e.tensor_copy(vaug[:, g, :, 0:64], pb[ib][:, :].rearrange("p (h d) -> p h d", h=NH)),
                     reads=[("pb", ib), "vaug_init"], writes=[("vaug", g)])

        def piece_F(T):
            own, hb, hkey = tile_ctx(T)
            ib = nextpb()
            for dc in range(8):
                B.op("pe", lambda e, dc=dc, ib=ib: e.matmul(pb[ib][0:8, :], lhsT=win[:, dc, WF:WF + 8], rhs=hb[:, dc, :],
                                                            start=(dc == 0), stop=(dc == 7)),
                     reads=[hkey] + winr, writes=[("pb", ib)])
            B.op("act", lambda e, ib=ib: e.activation(out=fl[0:8, :], in_=pb[ib][0:8, :], func=AF.Exp, scale=-1.0, bias=nbfcol[:]),
                 reads=[("pb", ib), "nbfcol"], writes=["fl"])
            B.op("act", lambda e: e.activation(out=fl[0:8, :], in_=fl[0:8, :], func=AF.Ln, bias=1.0), reads=["fl"], writes=["fl"])
            B.op("dve", lambda e: e.memset(fr[0:8, :], 1.0), writes=["fr"])
            B.op("dve", lambda e: e.tensor_tensor_scan(out=fc[0:8, :], data0=fr[0:8, :], data1=fl[0:8, :], initial=cstate[:],
                                                       op0=ALU.mult, op1=ALU.add), reads=["fr", "fl", "cstate"], writes=["fc"])
            B.op("dve", lambda e: e.tensor_copy(cstate[:], fc[0:8, 511:512]), reads=["fc"], writes=["cstate"])
            B.op("dve", lambda e: e.tensor_copy(fp[0:8, 0, :], fc[0:8, :]), reads=["fc"], writes=[("fp", 0)])
            B.op("dve", lambda e: e.tensor_tensor(out=fr[0:8, :], in0=fc[0:8, :], in1=fp[0:8, 0, :], op=ALU.subtract),
                 reads=["fc", ("fp", 0)], writes=["fr"])
            B.op("dve", lambda e: e.tensor_copy(fp[0:8, 1, :], fr[0:8, :]), reads=["fr"], writes=[("fp", 1)])
            B.op("dve", lambda e: e.tensor_tensor(out=fl[0:8, :], in0=fr[0:8, :], in1=fp[0:8, 1, :], op=ALU.subtract),
                 reads=["fr", ("fp", 1)], writes=["fl"])
            B.op("dve", lambda e: e.tensor_copy(fp[0:8, 2, :], fl[0:8, :]), reads=["fl"], writes=[("fp", 2)])
            B.dma("sp", kaug_d[:, 67:70, T * 512:(T + 1) * 512], fp[0:8, 0:3, :], reads=[("fp", 0), ("fp", 1), ("fp", 2)],
                  writes=[("kaugp", T)], semkey="fpk_out")
            if own:
                B.op("dve", lambda e: e.tensor_scalar(out=fp[0:8, 3:6, :], in0=fp[0:8, 0:3, :], scalar1=-1.0, scalar2=None, op0=ALU.mult),
                     reads=[("fp", 0), ("fp", 1), ("fp", 2)], writes=[("fp", 3)])
                B.dma("sp", qaug_d[:, 64:67, (T - 4) * 512:(T - 3) * 512], fp[0:8, 3:6, :], reads=[("fp", 3)], writes=[("qaugp", T)], semkey="fpq_out")

        def piece_A(T):
            own, hb, hkey = tile_ctx(T)
            rb_ = rin[T % 2]
            rprev = rin[(T + 1) % 2]
            B.op("dve", lambda e: e.tensor_copy(rb_[:, :, 1:4], rprev[:, :, 513:516]),
                 reads=[("rin", (T + 1) % 2)], writes=[("rinh", T % 2)])
            for cc in range(4):
                ib = nextpb()
                for dc in range(8):
                    B.op("pe", lambda e, cc=cc, dc=dc, ib=ib: e.matmul(pb[ib][:, :], lhsT=win[:, dc, WR + cc * 128:WR + (cc + 1) * 128],
                                                                       rhs=hb[:, dc, :], start=(dc == 0), stop=(dc == 7)),
                         reads=[hkey] + winr, writes=[("pb", ib)])
                B.op("act", lambda e, cc=cc, ib=ib: e.activation(out=rb_[:, cc, 4:516], in_=pb[ib][:, :], func=AF.Copy),
                     reads=[("pb", ib)], writes=[("rin", T % 2)])
            for cc in range(4):
                ib = nextpb()
                for k in range(4):
                    B.op("pe", lambda e, cc=cc, k=k, ib=ib: e.matmul(pb[ib][:, :], lhsT=cdiag[:, k * 4 + cc, :],
                                                                     rhs=rb_[:, cc, 1 + k:1 + k + 512], start=(k == 0), stop=(k == 3)),
                         reads=[("rin", T % 2), ("rinh", T % 2), "cdiag"], writes=[("pb", ib)])
                B.op("act", lambda e, cc=cc, ib=ib: e.activation(out=cvb[:, cc, :], in_=pb[ib][:, :], func=AF.Identity, bias=convb[:, cc:cc + 1]),
                     reads=[("pb", ib), "convb"], writes=[("cvb", cc)])

        def piece_C(T, cc):
            own, hb, hkey = tile_ctx(T)
            ck = ("cvb", cc)
            iba = nextpb()
            B.op("pe", lambda e: e.matmul(pb[iba][:, :], lhsT=bda[:, cc, :], rhs=cvb[:, cc, :], start=True, stop=True),
                 reads=[ck, "bda"], writes=[("pb", iba)])
            ibx = nextpb()
            B.op("pe", lambda e: e.matmul(pb[ibx][:, :], lhsT=bdx[:, cc, :], rhs=cvb[:, cc, :], start=True, stop=True),
                 reads=[ck, "bdx"], writes=[("pb", ibx)])
            B.op("act", lambda e: e.activation(out=tr[:], in_=pb[iba][:, :], func=AF.Tanh, bias=hbacol[:, cc:cc + 1], scale=0.5),
                 reads=[("pb", iba), "hbacol"], writes=["tr"])
            B.op("act", lambda e: e.activation(out=ti[:], in_=pb[ibx][:, :], func=AF.Tanh, bias=hbxcol[:, cc:cc + 1], scale=0.5),
                 reads=[("pb", ibx), "hbxcol"], writes=["ti"])
            B.op("act", lambda e: e.activation(out=ta[:], in_=tr[:], func=AF.Exp, scale=hs1col[:, cc:cc + 1], bias=hs1col[:, cc:cc + 1]),
                 reads=["tr", "hs1col"], writes=["ta"])
            B.op("act", lambda e: e.activation(out=tm[:], in_=tr[:], func=AF.Exp, scale=hs2col[:, cc:cc + 1], bias=hs2col[:, cc:cc + 1]),
                 reads=["tr", "hs2col"], writes=["tm"])
            B.op("act", lambda e: e.activation(out=tm[:], in_=tm[:], func=AF.Sqrt, scale=-0.25, bias=qcol[:, 0:1]), reads=["tm", "qcol"], writes=["tm"])
            B.op("dve", lambda e: e.scalar_tensor_tensor(out=tu[:], in0=ti[:], scalar=1.0, in1=cvb[:, cc, :], op0=ALU.add, op1=ALU.mult),
                 reads=["ti", ck], writes=["tu"])
            B.op("dve", lambda e: e.tensor_tensor(out=tu[:], in0=tu[:], in1=tm[:], op=ALU.mult), reads=["tu", "tm"], writes=["tu"])
            if T == 4:
                B.op("dve", lambda e: e.tensor_scalar(out=hstate[:, cc:cc + 1], in0=hstate[:, cc:cc + 1], scalar1=flag[:, 0:1],
                                                      scalar2=None, op0=ALU.mult), reads=[("hstate", cc), "flag"], writes=[("hstate", cc)])
            B.op("dve", lambda e: e.tensor_tensor_scan(out=th[:], data0=ta[:], data1=tu[:], initial=hstate[:, cc:cc + 1],
                                                       op0=ALU.mult, op1=ALU.add),
                 reads=["ta", "tu", ("hstate", cc), "hstate"], writes=["th"])
            B.op("dve", lambda e: e.tensor_copy(hstate[:, cc:cc + 1], th[:, 511:512]), reads=["th"], writes=[("hstate", cc)])
            if own:
                ibg = nextpb()
                for dc in range(8):
                    B.op("pe", lambda e, dc=dc: e.matmul(pb[ibg][:, :], lhsT=win[:, dc, WG + cc * 128:WG + (cc + 1) * 128],
                                                         rhs=hb[:, dc, :], start=(dc == 0), stop=(dc == 7)),
                         reads=[hkey] + winr, writes=[("pb", ibg)])
                B.op("act", lambda e: e.activation(out=gel[:], in_=pb[ibg][:, :], func=AF.Gelu_apprx_tanh),
                     reads=[("pb", ibg)], writes=["gel"])
                B.op("dve", lambda e: e.tensor_tensor(out=lruo[:, cc, (T - 4) * 512:(T - 3) * 512], in0=th[:], in1=gel[:], op=ALU.mult),
                     reads=["th", "gel"], writes=[("lruo", T - 4)])

        for t in range(-1, 8):
            if t + 1 < 8:
                piece_N(t + 1)
            if t >= 0:
                piece_A(t)
                piece_F(t)
                piece_K(t, range(0, 2), False)
                piece_C(t, 0)
                piece_K(t, range(2, 4), True)
                piece_C(t, 1)
                piece_V(t)
                piece_C(t, 2)
                piece_Q(t)
                piece_C(t, 3)

        B.barrier()
        ar.seek(OFF_P)
        kaug = [ar.alloc([128, NTOK], BF16) for _ in range(2)]
        qaug = [ar.alloc([128, NOWN], BF16) for _ in range(2)]
        PT = [ar.alloc([128, 512], BF16) for _ in range(4)]
        osb = [ar.alloc([128, 512], F32) for _ in range(2)]
        kd_reads = [("kaugc", h) for h in range(NH)] + [("kaug1", t) for t in range(8)] + [("kaugd", t, two) for t in range(8) for two in range(2)] + [("kaugp", t) for t in range(8)]
        qd_reads = [("qaug1", t) for t in range(4)] + [("qaug2", t) for t in range(4)] + [("qaugd", t, two) for t in range(4, 8) for two in range(2)] + [("qaugp", t) for t in range(4, 8)]
        sidx = [0]
        oidx = 0
        dq = Deferred()
        step = 0
        S_LA = 2
        for h in range(NH):
            kb_ = kaug[h % 2]
            qb_ = qaug[h % 2]
            B.dma("sp", kb_[0:KA, :], kaug_d[h], reads=kd_reads, writes=[("kaug", h % 2)])
            B.dma("sp", qb_[0:KA, :], qaug_d[h], reads=qd_reads, writes=[("qaug", h % 2)])
            for G in range(4):
                nkb = 16 + 4 * G + 4
                io = 4 + (oidx % 2)
                oidx += 1
                okey = ("pb", io)
                for kb in range(nkb):
                    m = kb - (16 + 4 * G)
                    q0 = 0 if m < 0 else m * 128
                    isb = sidx[0] % 4
                    sidx[0] += 1
                    skey = ("pb", isb)
                    B.op("pe", lambda e, kb=kb, G=G, q0=q0, isb=isb, kb_=kb_, qb_=qb_, m=m: e.matmul(
                        pb[isb][:, q0:512], lhsT=kb_[0:KA, kb * 128:(kb + 1) * 128], rhs=qb_[0:KA, G * 512 + q0:(G + 1) * 512],
                        start=True, stop=(m < 0)), reads=[("kaug", h % 2), ("qaug", h % 2)], writes=[skey])
                    if m >= 0:
                        B.op("pe", lambda e, q0=q0, isb=isb: e.matmul(pb[isb][:, q0:q0 + 128], lhsT=identb[:], rhs=maskb[:], start=False, stop=True),
                             reads=["identb", "maskb"], writes=[skey])
                    pkey = ("PT", isb)
                    B.op("act", lambda e, q0=q0, isb=isb: e.activation(out=PT[isb][:, q0:512], in_=pb[isb][:, q0:512], func=AF.Exp),
                         reads=[skey], writes=[pkey])

                    def pv(kb=kb, q0=q0, isb=isb, io=io, h=h, nkb=nkb, okey=okey, pkey=pkey):
                        B.op("pe", lambda e: e.matmul(pb[io][0:65, q0:512], lhsT=vaug[:, kb, h, :], rhs=PT[isb][:, q0:512],
                                                      start=(kb == 0), stop=(kb == nkb - 1)),
                             reads=[pkey, ("vaug", kb)], writes=[okey])
                    dq.at(step + S_LA, pv)
                    dq.run(step)
                    step += 1
                ob = osb[oidx % 2]
                obk = ("osb", oidx % 2)

                def norm1(ob=ob, obk=obk, io=io, okey=okey):
                    B.op("dve", lambda e: e.tensor_copy(ob[0:65, :], pb[io][0:65, :]), reads=[okey], writes=[obk])
                    B.op("dve", lambda e: e.reciprocal(ob[64:65, :], ob[64:65, :]), reads=[obk], writes=[obk])

                def norm2(ob=ob, obk=obk, h=h, G=G):
                    ibc = sidx[0] % 4
                    sidx[0] += 1
                    B.op("pe", lambda e: e.matmul(pb[ibc][0:64, :], lhsT=onesf[64:65, 0:64], rhs=ob[64:65, :], start=True, stop=True),
                         reads=[obk, "onesf"], writes=[("pb", ibc)])
                    B.op("dve", lambda e: e.tensor_tensor(out=attT[0:64, h, G * 512:(G + 1) * 512], in0=ob[0:64, :],
                                                          in1=pb[ibc][0:64, :], op=ALU.mult),
                         reads=[obk, ("pb", ibc)], writes=[("attT", h, G)])
                dq.at(step + S_LA - 1, norm1)
                dq.at(step + S_LA + 9, norm2)
        dq.flush()

        B.barrier()
        ar.seek(OFF_P)
        h2tok = ar.alloc([128, 16, D], BF16)
        wgu = [ar.alloc([128, 8, 512], BF16) for _ in range(2)]
        wdn = [ar.alloc([128, 2, D], BF16) for _ in range(2)]
        OFF_P3 = ar.off
        woa = ar.alloc([128, NH, D], BF16)
        wol = ar.alloc([128, 4, D], BF16)
        x3 = [ar.alloc([128, D], F32) for _ in range(2)]
        x1t = [ar.alloc([128, D], F32) for _ in range(2)]
        xq = ar.alloc([128, D], F32)
        h2Tb = [ar.alloc([128, 8, 128], BF16) for _ in range(2)]
        ohb = sb("ohb", [128, 16, NE], BF16)
        cw = sb("cw", [128, 16, 2], F32)
        sidx = sb("sidx", [128, 16, 2], I32)
        ebase1 = sb("ebase1", [128, NE], F32)
        padbase = sb("padbase", [128, NE], F32)
        triuf = sb("triuf", [128, 128], F32)
        triub = sb("triub", [128, 128], BF16)
        onesb = sb("onesb", [128, 128], BF16)
        cntsb = sb("cntsb", [128, NE], F32)
        padi = sb("padi", [128, NE], I32)
        flf = sb("flf", [1, NE, 16], F32)
        flags = sb("flags", [1, NE * 18 + 1], I32)
        flf4 = sb("flf4", [1, NE], F32)
        flf3 = sb("flf3", [1, 1], F32)
        flf2 = sb("flf2", [1, NE], F32)
        svt = sb("svt", [128, 2 * NE + 16], F32)

        B.dma("sp", ebase1[:], ebase1_d, writes=["ebase1"])

        B.dma("sp", padbase[:], padbase_d, writes=["padbase"])
        B.dma("sp", triuf[:], triu_d, writes=["triuf"])
        B.op("dve", lambda e: e.tensor_copy(triub[:], triuf[:]), reads=["triuf"], writes=["triub"])
        B.op("dve", lambda e: e.memset(onesb[:], 1.0), writes=["onesb"])
        B.dma("pool", woa[0:64, :, :], w_out_d[0:512, :].rearrange("(h p) n -> p h n", p=64), writes=["woa"])
        B.dma("pool", wol[:], w_out_d[512:1024, :].rearrange("(c p) n -> p c n", p=128), writes=["wol"])

        def load_expert(e_):
            s = e_ % 2
            B.dma("pool", wgu[s][:, :, 0:256], wg_d[e_].rearrange("(c p) f -> p c f", p=128), writes=[("wg", s)], nobarrier=True)
            B.dma("pool", wgu[s][:, :, 256:512], wu_d[e_].rearrange("(c p) f -> p c f", p=128), writes=[("wu", s)], nobarrier=True)
            B.dma("pool", wdn[s][:], wd_d[e_].rearrange("(c p) n -> p c n", p=128), writes=[("wd", s)], nobarrier=True)

        def p3_block(j, part):
            if part == 1:
                return p3_back(j)
            xb = x3[j % 2]
            if j == 0:
                B.dma("sp", x3[0][:], xs[NOWN:NOWN + 128, :], writes=[("x3", 0)])
            if j + 1 < 16:
                B.dma("sp", x3[(j + 1) % 2][:], xs[NOWN + (j + 1) * 128:NOWN + (j + 2) * 128, :], writes=[("x3", (j + 1) % 2)])
            x1b = x1t[j % 2]
            for half in range(2):
                ib = nextpb()
                for h in range(NH):
                    B.op("pe", lambda e, h=h, half=half, ib=ib: e.matmul(pb[ib][:, :], lhsT=attT[0:64, h, j * 128:(j + 1) * 128],
                                                                      rhs=woa[0:64, h, half * 512:(half + 1) * 512], start=(h == 0), stop=False),
                         reads=[("attT", h, j // 4), "woa"], writes=[("pb", ib)])
                for cc in range(4):
                    B.op("pe", lambda e, cc=cc, half=half, ib=ib: e.matmul(pb[ib][:, :], lhsT=lruo[:, cc, j * 128:(j + 1) * 128],
                                                                        rhs=wol[:, cc, half * 512:(half + 1) * 512], start=False, stop=(cc == 3)),
                         reads=[("lruo", j // 4), "wol"], writes=[("pb", ib)])
                B.op("dve", lambda e, half=half, ib=ib: e.tensor_tensor(out=x1b[:, half * 512:(half + 1) * 512], in0=pb[ib][:, :],
                                                                     in1=xb[:, half * 512:(half + 1) * 512], op=ALU.add),
                     reads=[("pb", ib), ("x3", j % 2)], writes=[("x1t", j % 2, half)])
            x1r = [("x1t", j % 2, 0), ("x1t", j % 2, 1)]
            B.dma("sp", x1_d[j * 128:(j + 1) * 128, :], x1b[:], reads=x1r, writes=[("x1d", j)], semkey=("x1t_out", j % 2))
            B.op("act", lambda e: e.activation(out=xq[:], in_=x1b[:], func=AF.Square, accum_out=ss4[:, 0:1]),
                 reads=x1r, writes=["xq", ("ss4", 0)])
            B.op("dve", lambda e: e.tensor_scalar(out=rstd4[:, 0:1], in0=ss4[:, 0:1], scalar1=1.0 / D, scalar2=1e-6, op0=ALU.mult, op1=ALU.add),
                 reads=[("ss4", 0)], writes=[("rstd4", 0)])
            B.op("pool", lambda e: e.tensor_tensor(out=rstd4[:, 0:1], in0=rstd4[:, 0:1], in1=mhalf[:], op=ALU.pow),
                 reads=[("rstd4", 0), "mhalf"], writes=[("rstd4", 0)])
            B.op("act", lambda e: e.activation(out=xq[:], in_=x1b[:], func=AF.Copy, scale=rstd4[:, 0:1]),
                 reads=x1r + [("rstd4", 0)], writes=["xq"])
            B.op("dve", lambda e: e.tensor_tensor(out=h2tok[:, j, :], in0=xq[:], in1=g2bc[:], op=ALU.mult), reads=["xq", "g2bc"], writes=[("h2tok", j)])
        def p3_back(j):
            ip = nextpt()
            ptv = pt[ip][:].rearrange("p (a b) -> p a b", a=8)
            for c in range(8):
                B.op("pe", lambda e, c=c: e.transpose(ptv[:, c, :], h2tok[:, j, c * 128:(c + 1) * 128], identb[:]),
                     reads=[("h2tok", j), "identb"], writes=[("pt", ip)])
            hT_ = h2Tb[j % 2]
            B.op("act", lambda e: e.activation(out=hT_[:], in_=ptv, func=AF.Copy), reads=[("pt", ip)], writes=[("h2Tb", j % 2)])
            ib = nextpb()
            for dc in range(8):
                B.op("pe", lambda e, dc=dc, ib=ib: e.matmul(pb[ib][:, 0:36], lhsT=hT_[:, dc, :], rhs=wr[:, dc, :],
                                                            start=(dc == 0), stop=(dc == 7)), reads=[("h2Tb", j % 2), "wr"], writes=[("pb", ib)])
            B.op("dve", lambda e, ib=ib: e.tensor_tensor(out=rt[:, 0:36], in0=pb[ib][:, 0:36], in1=rbbc[:], op=ALU.add),
                 reads=[("pb", ib), "rbbc"], writes=["rt"])
            R = dict(reads=["rt"], writes=["rt"])
            B.op("dve", lambda e: e.tensor_reduce(out=rt[:, 36:37], in_=rt[:, 0:4], axis=AX.X, op=ALU.max), **R)
            B.op("dve", lambda e: e.tensor_scalar(out=rt[:, 37:38], in0=rt[:, 36:37], scalar1=-1.0, scalar2=None, op0=ALU.mult), **R)
            B.op("act", lambda e: e.activation(out=rt[:, 38:42], in_=rt[:, 0:4], func=AF.Exp, bias=rt[:, 37:38], accum_out=rt[:, 42:43]), **R)
            B.op("dve", lambda e: e.reciprocal(rt[:, 42:43], rt[:, 42:43]), **R)
            B.op("dve", lambda e: e.tensor_scalar(out=rt[:, 38:42], in0=rt[:, 0:4], scalar1=rt[:, 36:37], scalar2=None, op0=ALU.is_equal), **R)
            B.op("dve", lambda e: e.tensor_scalar(out=rt[:, 44:52], in0=rt[:, 4:12], scalar1=rt[:, 38:39], scalar2=None, op0=ALU.mult), **R)
            for g in range(1, 4):
                B.op("dve", lambda e, g=g: e.scalar_tensor_tensor(out=rt[:, 44:52], in0=rt[:, 4 + 8 * g:12 + 8 * g], scalar=rt[:, 38 + g:39 + g],
                                                                  in1=rt[:, 44:52], op0=ALU.mult, op1=ALU.add), **R)
            B.op("dve", lambda e: e.max(out=rt[:, 52:60], in_=rt[:, 44:52]), **R)
            B.op("dve", lambda e: e.tensor_scalar(out=rt[:, 60:61], in0=rt[:, 52:53], scalar1=-1.0, scalar2=None, op0=ALU.mult), **R)
            B.op("act", lambda e: e.activation(out=rt[:, 4:12], in_=rt[:, 44:52], func=AF.Exp, bias=rt[:, 60:61]), **R)
            B.op("dve", lambda e: e.tensor_scalar(out=rt[:, 12:20], in0=rt[:, 44:52], scalar1=rt[:, 53:54], scalar2=None, op0=ALU.is_ge), **R)
            B.op("dve", lambda e: e.tensor_tensor(out=rt[:, 4:12], in0=rt[:, 4:12], in1=rt[:, 12:20], op=ALU.mult), **R)
            B.op("dve", lambda e: e.tensor_reduce(out=rt[:, 61:62], in_=rt[:, 4:12], axis=AX.X, op=ALU.add), **R)
            B.op("dve", lambda e: e.reciprocal(rt[:, 61:62], rt[:, 61:62]), **R)
            B.op("dve", lambda e: e.tensor_tensor(out=rt[:, 61:62], in0=rt[:, 61:62], in1=rt[:, 42:43], op=ALU.mult), **R)
            for g in range(4):
                B.op("dve", lambda e, g=g: e.tensor_scalar(out=comb[:, j, 8 * g:8 * g + 8], in0=rt[:, 4:12], scalar1=rt[:, 38 + g:39 + g],
                                                           scalar2=rt[:, 61:62], op0=ALU.mult, op1=ALU.mult),
                     reads=["rt"], writes=[("comb", j)])
                B.op("dve", lambda e, g=g: e.tensor_scalar(out=ohb[:, j, 8 * g:8 * g + 8], in0=rt[:, 12:20], scalar1=rt[:, 38 + g:39 + g],
                                                           scalar2=None, op0=ALU.mult),
                     reads=["rt"], writes=[("ohb", j)])

        for j in range(17):
            if j < 16:
                p3_block(j, 0)
            if j >= 1:
                p3_block(j - 1, 1)
            if j == 0:
                load_expert(0)
                load_expert(1)

        ohr = [("ohb", j) for j in range(16)]
        combr = [("comb", j) for j in range(16)]
        ar.seek(OFF_P3)
        pre = ar.alloc([128, 16, NE], F32)
        sva = ar.alloc([128, 16, NE], F32)
        msk = ar.alloc([128, 16, NE], F32)
        ebase16 = ar.alloc([128, 16, NE], F32)
        top8a = ar.alloc([128, 16, 8], F32)
        B.dma("sp", ebase16[:], eb16_d.rearrange("p (a b) -> p a b", a=16), writes=["ebase16", "woa", "wol"])
        ibt = nextpb()
        ibk = nextpb()
        totv = pb[ibt][:, :].rearrange("p (a b) -> p a b", a=16)
        bkv = pb[ibk][:, :].rearrange("p (a b) -> p a b", a=16)
        for j in range(16):
            B.op("pe", lambda e, j=j: e.matmul(totv[:, j, :], lhsT=onesb[:], rhs=ohb[:, j, :], start=True, stop=True),
                 reads=[("ohb", j), "onesb"], writes=[("pb", ibt)])
        for j in range(16):
            B.op("pe", lambda e, j=j: e.matmul(bkv[:, j, :], lhsT=triub[:], rhs=ohb[:, j, :], start=True, stop=True),
                 reads=[("ohb", j), "triub"], writes=[("pb", ibk)])
        PRE = dict(reads=["pre"], writes=["pre"])
        B.op("dve", lambda e: e.memset(pre[:, 0, :], 0.0), reads=["pre"], writes=["pre", "woa", "wol"])
        for j in range(1, 16):
            B.op("dve", lambda e, j=j: e.tensor_tensor(out=pre[:, j, :], in0=totv[:, j - 1, :], in1=pre[:, j - 1, :], op=ALU.add),
                 reads=["pre", ("pb", ibt)], writes=["pre"])
        B.op("dve", lambda e: e.tensor_tensor(out=cntsb[:], in0=totv[:, 15, :], in1=pre[:, 15, :], op=ALU.add),
             reads=["pre", ("pb", ibt)], writes=["cntsb"])
        for k in range(16):
            B.op("dve", lambda e, k=k: e.tensor_scalar(out=flf[0:1, :, k], in0=cntsb[0:1, :], scalar1=128.0 * k, scalar2=None, op0=ALU.is_gt),
                 reads=["cntsb"], writes=["flf"])
        B.op("dve", lambda e: e.tensor_copy(flags[:, 0:NE * 16], flf[:].rearrange("p a b -> p (a b)")), reads=["flf"], writes=["flags0"])
        B.op("dve", lambda e: e.tensor_scalar(out=flf2[0:1, :], in0=cntsb[0:1, :], scalar1=256.0, scalar2=None, op0=ALU.is_gt),
             reads=["cntsb"], writes=["flf2"])
        B.op("dve", lambda e: e.tensor_copy(flags[:, NE * 16:NE * 17], flf2[:]), reads=["flf2", "flags0"], writes=["flags1"])
        B.op("dve", lambda e: e.tensor_reduce(out=flf3[:], in_=flf2[:], axis=AX.X, op=ALU.max), reads=["flf2"], writes=["flf3"])
        B.op("dve", lambda e: e.tensor_copy(flags[:, NE * 17:NE * 17 + 1], flf3[:]), reads=["flf3", "flags1"], writes=["flags2"])
        B.op("dve", lambda e: e.tensor_tensor(out=flf4[:], in0=flf[0:1, :, 0], in1=flf[0:1, :, 1], op=ALU.subtract), reads=["flf"], writes=["flf4"])
        B.op("dve", lambda e: e.tensor_copy(flags[:, NE * 17 + 1:NE * 18 + 1], flf4[:]), reads=["flf4", "flags2"], writes=["flags"])
        B.flags_ap = flags
        B.flags_key = "flags"
        B.op("dve", lambda e: e.tensor_tensor(out=svt[:, 0:NE], in0=cntsb[:], in1=padbase[:], op=ALU.add), reads=["cntsb", "padbase"], writes=["svt"])
        B.op("dve", lambda e: e.tensor_copy(padi[:], svt[:, 0:NE]), reads=["svt"], writes=["padi"])
        B.op("dve", lambda e: e.tensor_tensor(out=sva[:], in0=bkv, in1=pre[:], op=ALU.add), reads=["pre", ("pb", ibk)], writes=["sva"])
        B.op("dve", lambda e: e.tensor_tensor(out=sva[:], in0=sva[:], in1=ebase16[:], op=ALU.add), reads=["sva", "ebase16"], writes=["sva"])
        B.op("dve", lambda e: e.tensor_tensor(out=sva[:], in0=sva[:], in1=ohb[:], op=ALU.mult), reads=["sva"] + ohr, writes=["sva"])
        for j in range(16):
            B.op("dve", lambda e, j=j: e.max(out=top8a[:, j, :], in_=sva[:, j, :]), reads=["sva"], writes=["top8a"])
        for q in range(2):
            B.op("dve", lambda e, q=q: e.tensor_tensor(out=msk[:], in0=sva[:], in1=top8a[:, :, q:q + 1].to_broadcast([128, 16, NE]), op=ALU.is_equal),
                 reads=["sva", "top8a"], writes=["msk"])
            B.op("dve", lambda e: e.tensor_tensor(out=msk[:], in0=msk[:], in1=comb[:], op=ALU.mult), reads=["msk"] + combr, writes=["msk"])
            B.op("dve", lambda e, q=q: e.tensor_reduce(out=cw[:, :, q], in_=msk[:], axis=AX.X, op=ALU.add), reads=["msk"], writes=[("cwq", q)])
        B.op("dve", lambda e: e.tensor_scalar(out=svt[:, 0:32].rearrange("p (a b) -> p a b", a=16), in0=top8a[:, :, 0:2], scalar1=-1.0, scalar2=None, op0=ALU.add),
             reads=["top8a", "svt"], writes=["svt"])
        B.op("dve", lambda e: e.tensor_copy(sidx[:], svt[:, 0:32].rearrange("p (a b) -> p a b", a=16)), reads=["svt"], writes=[("sidx", j) for j in range(16)])

        B.barrier()
        ar.seek(OFF_P3)
        xin = [ar.alloc([128, D], BF16) for _ in range(4)]
        xT = [ar.alloc([128, 8, 128], BF16) for _ in range(2)]
        sg = [ar.alloc([128, 256], F32) for _ in range(2)]
        hid = [ar.alloc([128, 256], BF16) for _ in range(2)]
        hidT = [ar.alloc([128, 2, 128], BF16) for _ in range(2)]
        ysb = [ar.alloc([128, D], BF16) for _ in range(2)]
        zt = ar.alloc([128, D], BF16)
        assert ar.off <= OFF_P3 + 24 * 1024, ar.off - OFF_P3
        B.op("dve", lambda e: e.memset(zt[:], 0.0), writes=["zt"])

        regcache = {}

        def bcreg(e):
            if "bc" not in regcache:
                regcache["bc"] = e.to_reg(NE * CAP - 1)
            return regcache["bc"]

        def ind_scatter(src_ap, idx_ap, reads, wkey, grp):
            def fn(e):
                return e.indirect_dma_start(out=xs_scr[:, :], out_offset=bass.IndirectOffsetOnAxis(ap=idx_ap, axis=0),
                                            in_=src_ap, in_offset=None, bounds_check=bcreg(e), oob_is_err=False)
            B.op("pool", fn, reads=reads, writes=[wkey], dma=True, semgroup=grp)

        tok_keys = []
        for j in range(16):
            for q in range(2):
                ind_scatter(h2tok[:, j, :], sidx[:, j, q:q + 1], [("h2tok", j), ("sidx", j)], ("sct", j, q), "scat")
                tok_keys.append(("sct", j, q))
        for e_ in range(NE):
            ind_scatter(zt[:, :], padi[:, e_:e_ + 1], ["zt", "padi"], ("scz", e_), "scz%d" % (e_ // 4))
        scat_keys = tok_keys + [("scz", e_) for e_ in range(NE)]

        ys_keys = []
        tcount = [0]

        def moe_tiles(e_, ks, flag):
            s = e_ % 2
            wkeys = [("wg", s), ("wu", s)]
            B.begin_cond(flag)
            T_ = []
            for k in ks:
                tno = tcount[0]
                tcount[0] += 1
                T_.append(dict(k=k, b2=tno % 2, igu=tno % 2, iy=2 + 2 * (tno % 2), row0=e_ * CAP + k * 128,
                               bx=(e_ % 2) * 2 + (k % 2)))
            for t in T_:
                B.dma("sp", xin[t["bx"]][:], xs_scr[t["row0"]:t["row0"] + 128, :],
                      reads=tok_keys + [("scz", e2) for e2 in range((e_ // 4) * 4, (e_ // 4) * 4 + 4)], writes=[("xin", t["bx"])])
            for t in T_:
                ip = nextpt()
                t["ip"] = ip
                t["ptv"] = pt[ip][:].rearrange("p (a b) -> p a b", a=8)
                for c in range(8):
                    B.op("pe", lambda e, c=c, t=t: e.transpose(t["ptv"][:, c, :], xin[t["bx"]][:, c * 128:(c + 1) * 128], identb[:]),
                         reads=[("xin", t["bx"]), "identb"], writes=[("pt", ip)])
                B.op("dve", lambda e, t=t: e.tensor_copy(xT[t["b2"]][:], t["ptv"]), reads=[("pt", ip)], writes=[("xT", t["b2"])])
            for t in T_:
                for dc in range(8):
                    B.op("pe", lambda e, dc=dc, t=t: e.matmul(pb[t["igu"]][:, :], lhsT=xT[t["b2"]][:, dc, :], rhs=wgu[s][:, dc, :],
                                                              start=(dc == 0), stop=(dc == 7)),
                         reads=[("xT", t["b2"])] + wkeys, writes=[("pb", t["igu"])])
                B.op("act", lambda e, t=t: e.activation(out=sg[t["b2"]][:], in_=pb[t["igu"]][:, 0:256], func=AF.Silu),
                     reads=[("pb", t["igu"])], writes=[("sg", t["b2"])])
                B.op("dve", lambda e, t=t: e.tensor_tensor(out=hid[t["b2"]][:], in0=pb[t["igu"]][:, 256:512], in1=sg[t["b2"]][:], op=ALU.mult),
                     reads=[("pb", t["igu"]), ("sg", t["b2"])], writes=[("hid", t["b2"])])
            for t in T_:
                ip2 = nextpt()
                ptv2 = pt[ip2][:, 0:256].rearrange("p (a b) -> p a b", a=2)
                for fc_ in range(2):
                    B.op("pe", lambda e, fc_=fc_, t=t, ptv2=ptv2: e.transpose(ptv2[:, fc_, :], hid[t["b2"]][:, fc_ * 128:(fc_ + 1) * 128], identb[:]),
                         reads=[("hid", t["b2"]), "identb"], writes=[("pt", ip2)])
                B.op("dve", lambda e, t=t, ptv2=ptv2: e.tensor_copy(hidT[t["b2"]][:], ptv2), reads=[("pt", ip2)], writes=[("hidT", t["b2"])])
            for t in T_:
                iy, b2 = t["iy"], t["b2"]
                for half in range(2):
                    for fc_ in range(2):
                        B.op("pe", lambda e, fc_=fc_, half=half, iy=iy, b2=b2: e.matmul(pb[iy + half][:, :], lhsT=hidT[b2][:, fc_, :],
                                                                                      rhs=wdn[s][:, fc_, half * 512:(half + 1) * 512],
                                                                                      start=(fc_ == 0), stop=(fc_ == 1)),
                             reads=[("hidT", b2), ("wd", s)], writes=[("pb", iy + half)])
                B.op("act", lambda e, iy=iy, b2=b2: e.activation(out=ysb[b2][:, 0:512], in_=pb[iy][:, :], func=AF.Copy),
                     reads=[("pb", iy)], writes=[("ysb", b2, 0)])
                B.op("dve", lambda e, iy=iy, b2=b2: e.tensor_copy(ysb[b2][:, 512:1024], pb[iy + 1][:, :]),
                     reads=[("pb", iy + 1)], writes=[("ysb", b2, 1)])
                ykey = ("ysd", e_, t["k"], flag)
                B.dma("act", ys_scr[t["row0"]:t["row0"] + 128, :], ysb[b2][:], reads=[("ysb", b2, 0), ("ysb", b2, 1)], writes=[ykey],
                      semkey=("ys_out", b2))
                ys_keys.append(ykey)
            B.end_cond()

        def moe_tile(e_, k):
            moe_tiles(e_, [k], e_ * 16 + k)

        for e_ in range(NE):
            moe_tiles(e_, [0, 1], e_ * 16 + 1)
            moe_tiles(e_, [0], NE * 17 + 1 + e_)
            if e_ + 2 < NE:
                load_expert(e_ + 2)

        wst = zt.bitcast(F32)
        B.begin_cond(NE * 17)
        for e_ in range(NE):
            s = e_ % 2
            B.begin_cond(NE * 16 + e_)
            for dc in range(8):
                B.dma("sp", wst[:, 0:256], wg_d[e_, dc * 128:(dc + 1) * 128, :], reads=scat_keys, writes=["zt"], semkey="wst")
                B.op("dve", lambda e, dc=dc, s=s: e.tensor_copy(wgu[s][:, dc, 0:256], wst[:, 0:256]), reads=["zt"], writes=[("wg", s)])
                B.dma("sp", wst[:, 256:512], wu_d[e_, dc * 128:(dc + 1) * 128, :], reads=scat_keys, writes=["zt2"], semkey="wst2")
                B.op("dve", lambda e, dc=dc, s=s: e.tensor_copy(wgu[s][:, dc, 256:512], wst[:, 256:512]), reads=["zt2"], writes=[("wu", s)])
            for fc_ in range(2):
                for half in range(2):
                    B.dma("sp", wst[:, :], wd_d[e_, fc_ * 128:(fc_ + 1) * 128, half * 512:(half + 1) * 512], reads=scat_keys,
                          writes=["zt", "zt2"], semkey="wst")
                    B.op("dve", lambda e, fc_=fc_, half=half, s=s: e.tensor_copy(wdn[s][:, fc_, half * 512:(half + 1) * 512], wst[:, :]),
                         reads=["zt", "zt2"], writes=[("wd", s)])
            for k in range(2, 16):
                moe_tile(e_, k)
            B.end_cond()
        B.end_cond()

        B.barrier()
        ar.seek(OFF_P + 32 * 1024)
        rg = [ar.alloc([128, D], BF16) for _ in range(8)]
        B.dma("sp", g2bc[:], g3bc_d, reads=[], writes=["g2bc"], semkey="g3load")
        def ind_gather(dst_ap, idx_ap, reads, wkey):
            def fn(e):
                return e.indirect_dma_start(out=dst_ap, out_offset=None, in_=ys_scr[:, :],
                                            in_offset=bass.IndirectOffsetOnAxis(ap=idx_ap, axis=0),
                                            bounds_check=bcreg(e), oob_is_err=False)
            B.op("pool", fn, reads=reads, writes=[wkey], dma=True)

        for j in range(16):
            xb = x3[j % 2]
            ob = x1t[j % 2]
            r1 = rg[(j % 4) * 2]
            r2 = rg[(j % 4) * 2 + 1]
            ind_gather(r1[:, :], sidx[:, j, 0:1], ys_keys + [("sidx", j)], ("rg", (j % 4) * 2))
            ind_gather(r2[:, :], sidx[:, j, 1:2], ys_keys + [("sidx", j)], ("rg", (j % 4) * 2 + 1))
            if j == 0:
                B.dma("sp", x3[0][:], x1_d[0:128, :], reads=[("x1d", 0)], writes=[("x3", 0)])
            if j + 1 < 16:
                B.dma("sp", x3[(j + 1) % 2][:], x1_d[(j + 1) * 128:(j + 2) * 128, :], reads=[("x1d", j + 1)], writes=[("x3", (j + 1) % 2)])
            B.op("dve", lambda e, xb=xb, r1=r1, j=j: e.scalar_tensor_tensor(out=xb[:], in0=r1[:], scalar=cw[:, j, 0:1], in1=xb[:],
                                                                        op0=ALU.mult, op1=ALU.add),
                 reads=[("x3", j % 2), ("rg", (j % 4) * 2), ("cwq", 0)], writes=[("x3", j % 2)])
            B.op("dve", lambda e, xb=xb, r2=r2, j=j: e.scalar_tensor_tensor(out=xb[:], in0=r2[:], scalar=cw[:, j, 1:2], in1=xb[:],
                                                                        op0=ALU.mult, op1=ALU.add),
                 reads=[("x3", j % 2), ("rg", (j % 4) * 2 + 1), ("cwq", 1)], writes=[("x3", j % 2)])
            pj = j % 2
            obk = [("x1t", pj, 0), ("x1t", pj, 1)]
            B.op("act", lambda e, xb=xb, ob=ob, pj=pj: e.activation(out=ob[:], in_=xb[:], func=AF.Square, accum_out=ss4[:, pj:pj + 1]),
                 reads=[("x3", pj)], writes=obk + [("ss4", pj)])
            B.op("dve", lambda e, pj=pj: e.tensor_scalar(out=rstd4[:, pj:pj + 1], in0=ss4[:, pj:pj + 1], scalar1=1.0 / D, scalar2=1e-6, op0=ALU.mult, op1=ALU.add),
                 reads=[("ss4", pj)], writes=[("rstd4", pj)])
            B.op("pool", lambda e, pj=pj: e.tensor_tensor(out=rstd4[:, pj:pj + 1], in0=rstd4[:, pj:pj + 1], in1=mhalf[:], op=ALU.pow),
                 reads=[("rstd4", pj), "mhalf"], writes=[("rstd4", pj)])
            B.op("act", lambda e, xb=xb, ob=ob, pj=pj: e.activation(out=ob[:], in_=xb[:], func=AF.Copy, scale=rstd4[:, pj:pj + 1]),
                 reads=[("x3", pj), ("rstd4", pj)], writes=obk)
            B.op("dve", lambda e, ob=ob: e.tensor_tensor(out=ob[:], in0=ob[:], in1=g3bc[:], op=ALU.mult), reads=obk + ["g2bc"], writes=obk)
            B.dma("sp", out_d[j * 128:(j + 1) * 128, :], ob[:], reads=[("x1t", j % 2, 0), ("x1t", j % 2, 1)], writes=[("outd", j)],
                  semkey=("ob_out", j % 2))

        if os.environ.get("MK_NOPS"):
            B.ops = B.ops[:int(os.environ["MK_NOPS"])]
        B.emit(st)
        _NC_CACHE["B"] = B
    return nc


_NC_CACHE = {}


def _layout_inputs(inp):
    f = lambda a: np.ascontiguousarray(np.asarray(a, dtype=np.float32))
    x = f(inp["x"])
    common = {}
    common["ident"] = np.eye(128, dtype=np.float32)
    common["onesd"] = np.ones((NH, 3, 512), ml_dtypes.bfloat16)
    ee = np.arange(NE, dtype=np.float32)[None, :] * CAP
    common["ebase1"] = f(np.broadcast_to(ee + 1.0, (128, NE)))
    common["ebase16"] = f(np.broadcast_to((ee + 1.0)[:, None, :], (128, 16, NE)).reshape(128, 16 * NE))
    common["padbase"] = f(ee + np.arange(128, dtype=np.float32)[:, None])
    common["triu"] = (np.arange(128)[:, None] < np.arange(128)[None, :]).astype(np.float32)
    k = np.arange(128)
    common["maskT"] = np.where(k[:, None] <= k[None, :], 0.0, NEG).astype(np.float32)
    common["gcol"] = f(inp["mix_norm"][0].reshape(8, 128).T)
    common["w_in"] = f(inp["w_in"][0])
    common["bfcol"] = f(inp["b_forget"][0].reshape(8, 1))
    cw = f(inp["conv_w"][0])
    cd = np.zeros((128, 16, 128), np.float32)
    for tap in range(4):
        for cc in range(4):
            cd[k, tap * 4 + cc, k] = cw[tap, cc * 128:(cc + 1) * 128]
    common["cdiag"] = cd
    common["convb"] = f(inp["conv_b"][0].reshape(4, 128).T)
    for nm, key in (("bda", "w_a"), ("bdx", "w_x")):
        w = f(inp[key][0])
        bd = np.zeros((128, 4, 128), np.float32)
        for cc in range(4):
            bd[0:64, cc, 0:64] = w[2 * cc]
            bd[64:128, cc, 64:128] = w[2 * cc + 1]
        common[nm] = bd
    common["bacol"] = f(inp["b_a"][0].reshape(4, 128).T)
    common["bxcol"] = f(inp["b_x"][0].reshape(4, 128).T)
    common["lamcol"] = f(inp["lru_lambda"][0].reshape(4, 128).T)
    common["w_out"] = f(inp["w_out"][0])
    common["g2bc"] = f(np.broadcast_to(inp["ffn_norm"][0][None, :], (128, D)))
    common["g3bc"] = f(np.broadcast_to(np.asarray(inp["final_norm"])[None, :], (128, D)))
    wi = np.asarray(inp["w_inner"][0], dtype=np.float32)
    common["wr"] = f(np.concatenate([np.asarray(inp["w_group"][0], dtype=np.float32), wi.transpose(1, 0, 2).reshape(D, 32)], axis=1))
    rb = np.concatenate([np.asarray(inp["b_group"][0], dtype=np.float32), np.asarray(inp["b_inner"][0], dtype=np.float32).reshape(32)])
    common["rbbc"] = f(np.broadcast_to(rb[None, :], (128, 36)))
    common["w_gate"] = f(np.asarray(inp["w_gate"][0]).reshape(NE, D, FE))
    common["w_up"] = f(np.asarray(inp["w_up"][0]).reshape(NE, D, FE))
    common["w_down"] = f(np.asarray(inp["w_down"][0]).reshape(NE, FE, D))
    maps = []
    for c in range(8):
        b, half = c // 2, c % 2
        m = dict(common)
        if half == 1:
            m["xs"] = f(x[b])
            m["kmrow"] = np.zeros((1, NTOK), ml_dtypes.bfloat16)
        else:
            m["xs"] = f(np.concatenate([np.zeros((NOWN, D), np.float32), x[b, :NOWN]], axis=0))
            km = np.zeros((1, NTOK), np.float32)
            km[0, :NOWN] = NEG
            m["kmrow"] = km.astype(ml_dtypes.bfloat16)
        m["flag"] = np.full((128, 1), float(half), np.float32)
        maps.append(m)
    return maps


def kernel(**inputs):
    if "nc" not in _NC_CACHE:
        _NC_CACHE["nc"] = build_program()
    nc = _NC_CACHE["nc"]
    maps = _layout_inputs(inputs)
    res = run_bass_kernel_spmd(nc, maps, core_ids=list(range(8)))
    out = np.zeros((4, SEQ, D), np.float32)
    for c in range(8):
        b, half = c // 2, c % 2
        out[b, half * NOWN:(half + 1) * NOWN] = res.results[c]["out"]
    return out
```
